# Optimizing a Trainium2 kernel written in Bass

```python
import math
import jax, jax.numpy as jnp
from jax import lax
import numpy as np

D_MODEL = 1024
BATCH = 8
SEQ = 8192
DEPTH = 1

GRID_W = 64
EPS = 1e-6
ATTN_HEADS = 8
ATTN_KV_HEADS = 2
GQA_GROUP = ATTN_HEADS // ATTN_KV_HEADS
HEAD_DIM = 64
ROPE_THETA = 10000.0
ROPE_AXIS_DIM = HEAD_DIM // 2
Q_BLOCK = 128
HGRN_HEADS = 4
HGRN_DK = 128
HGRN_DV = 128
HGRN_CHUNK = 32
HGRN_SCALE = HGRN_DK ** -0.5
N_GROUPS = 4
EXPERTS_PER_GROUP = 8
N_EXPERTS = N_GROUPS * EXPERTS_PER_GROUP
TOP_K = 2
D_EXPERT = 512

ATTN_Q_W = ATTN_HEADS * HEAD_DIM
ATTN_KV_W = ATTN_KV_HEADS * HEAD_DIM
HGRN_K_W = HGRN_HEADS * HGRN_DK
HGRN_V_W = HGRN_HEADS * HGRN_DV
IN_SPLITS = (ATTN_Q_W, ATTN_KV_W, ATTN_KV_W, HGRN_K_W, HGRN_K_W, HGRN_K_W, HGRN_V_W, HGRN_V_W, D_MODEL, D_MODEL)
D_IN = sum(IN_SPLITS)
SPLIT_POINTS = tuple(int(s) for s in np.cumsum(IN_SPLITS)[:-1])

kernel_name = "hybrid_gqa_hgrn2_hiermoe_encoder"


def rmsnorm(x, g):
    xf = x.astype(jnp.float32)
    y = xf * lax.rsqrt(jnp.mean(xf * xf, axis=-1, keepdims=True) + EPS)
    return (y * g.astype(jnp.float32)).astype(x.dtype)


def axial_rope_tables(seq):
    rows = seq // GRID_W
    row_ids = jnp.repeat(jnp.arange(rows), GRID_W).astype(jnp.float32)
    col_ids = jnp.tile(jnp.arange(GRID_W), rows).astype(jnp.float32)
    inv_freq = ROPE_THETA ** (-jnp.arange(0, ROPE_AXIS_DIM, 2, dtype=jnp.float32) / ROPE_AXIS_DIM)
    ang = jnp.concatenate([row_ids[:, None] * inv_freq, col_ids[:, None] * inv_freq], axis=-1)
    return jnp.cos(ang), jnp.sin(ang)


def apply_rope(x, cos, sin):
    xr = x.astype(jnp.float32).reshape(x.shape[:-1] + (HEAD_DIM // 2, 2))
    a, b = xr[..., 0], xr[..., 1]
    out = jnp.stack([a * cos - b * sin, a * sin + b * cos], axis=-1)
    return out.reshape(x.shape).astype(x.dtype)


def bidir_gqa(q, k, v, q_norm, k_norm):
    B, S, _ = q.shape
    q = rmsnorm(q.reshape(B, S, ATTN_KV_HEADS, GQA_GROUP, HEAD_DIM), q_norm)
    k = rmsnorm(k.reshape(B, S, ATTN_KV_HEADS, HEAD_DIM), k_norm)
    v = v.reshape(B, S, ATTN_KV_HEADS, HEAD_DIM)
    cos, sin = axial_rope_tables(S)
    q = apply_rope(q, cos[:, None, None, :], sin[:, None, None, :])
    k = apply_rope(k, cos[:, None, :], sin[:, None, :])
    nb = S // Q_BLOCK
    qb = q.reshape(B, nb, Q_BLOCK, ATTN_KV_HEADS, GQA_GROUP, HEAD_DIM).transpose(1, 0, 3, 4, 2, 5)
    kt = k.transpose(0, 2, 1, 3)
    vt = v.transpose(0, 2, 1, 3)
    scale = HEAD_DIM ** -0.5

    def one_block(qblk):
        s = jnp.einsum('bhgqd,bhkd->bhgqk', qblk, kt).astype(jnp.float32) * scale
        p = jax.nn.softmax(s, axis=-1).astype(vt.dtype)
        return jnp.einsum('bhgqk,bhkd->bhgqd', p, vt)

    o = lax.map(one_block, qb)
    return o.transpose(1, 0, 4, 2, 3, 5).reshape(B, S, ATTN_Q_W)


def gla_chunkwise(q, k, v, log_f):
    B, H, S, dk = q.shape
    dv = v.shape[-1]
    nc = S // HGRN_CHUNK
    C = HGRN_CHUNK
    q = q.reshape(B, H, nc, C, dk)
    k = k.reshape(B, H, nc, C, dk)
    v = v.reshape(B, H, nc, C, dv)
    b = jnp.cumsum(log_f.reshape(B, H, nc, C, dk), axis=3)
    b_last = b[:, :, :, C - 1:, :]
    b_ref = b[:, :, :, C // 2:C // 2 + 1, :]
    a = jnp.einsum('bhnck,bhnsk->bhncs', q * jnp.exp(b - b_ref), k * jnp.exp(b_ref - b))
    mask = jnp.tril(jnp.ones((C, C), dtype=bool))
    o_intra = jnp.einsum('bhncs,bhnsv->bhncv', jnp.where(mask, a, 0.0), v)
    q_inter = q * jnp.exp(b)
    k_state = k * jnp.exp(b_last - b)
    decay = jnp.exp(b_last[:, :, :, 0, :])
    xs = (jnp.moveaxis(q_inter, 2, 0), jnp.moveaxis(k_state, 2, 0), jnp.moveaxis(v, 2, 0), jnp.moveaxis(decay, 2, 0))

    def step(state, inp):
        qi, ks, vv, d = inp
        o = jnp.einsum('bhck,bhkv->bhcv', qi, state)
        state = d[..., None] * state + jnp.einsum('bhck,bhcv->bhkv', ks, vv)
        return state, o

    s0 = jnp.zeros((B, H, dk, dv), jnp.float32)
    _, o_inter = lax.scan(step, s0, xs)
    o = o_intra + jnp.moveaxis(o_inter, 0, 2)
    return o.reshape(B, H, S, dv)


def hgrn2_bidir(hq, f_fwd, f_bwd, i_in, out_gate, lb_f, lb_b, hgrn_norm):
    B, S, _ = hq.shape

    def heads(t, d):
        return t.reshape(B, S, HGRN_HEADS, d).transpose(0, 2, 1, 3).astype(jnp.float32)

    q = heads(jax.nn.silu(hq.astype(jnp.float32)) * HGRN_SCALE, HGRN_DK)
    v = heads(i_in, HGRN_DV)

    def direction(f_logits, lb, flip):
        z = f_logits.astype(jnp.float32)
        f = lb + (1.0 - lb) * jax.nn.sigmoid(z)
        k = heads((1.0 - lb) * jax.nn.sigmoid(-z), HGRN_DK)
        log_f = heads(jnp.log(f), HGRN_DK)
        if flip:
            o = gla_chunkwise(jnp.flip(q, 2), jnp.flip(k, 2), jnp.flip(v, 2), jnp.flip(log_f, 2))
            return jnp.flip(o, 2)
        return gla_chunkwise(q, k, v, log_f)

    o = direction(f_fwd, lb_f, False) + direction(f_bwd, lb_b, True)
    o = rmsnorm(o.transpose(0, 2, 1, 3), hgrn_norm)
    o = o * jax.nn.silu(out_gate.reshape(B, S, HGRN_HEADS, HGRN_DV).astype(jnp.float32))
    return o.reshape(B, S, HGRN_V_W).astype(hq.dtype)


def hier_moe(h, w_rg, b_rg, w_re, b_re, w_gate, w_up, w_down):
    B, S, D = h.shape
    t = h.reshape(B * S, D)
    g_prob = jax.nn.softmax((t @ w_rg + b_rg).astype(jnp.float32), axis=-1)
    p_g, g_idx = lax.top_k(g_prob, 1)
    e_logits = (t @ w_re + b_re).astype(jnp.float32).reshape(-1, N_GROUPS, EXPERTS_PER_GROUP)
    e_sel = jnp.take_along_axis(e_logits, g_idx[:, :, None], axis=1)[:, 0]
    p_e, e_idx = lax.top_k(jax.nn.softmax(e_sel, axis=-1), TOP_K)
    p_e = p_e / jnp.sum(p_e, axis=-1, keepdims=True)
    expert_ids = g_idx * EXPERTS_PER_GROUP + e_idx
    combine = p_g * p_e
    dense_w = jnp.sum(jax.nn.one_hot(expert_ids, N_EXPERTS, dtype=jnp.float32) * combine[..., None], axis=1)
    dense_w = dense_w.astype(t.dtype)
    y = jnp.zeros((B * S, D), jnp.float32)
    for e in range(N_EXPERTS):
        hid = jax.nn.silu(t @ w_gate[e]) * (t @ w_up[e])
        y = y + ((hid * dense_w[:, e:e + 1]) @ w_down[e]).astype(jnp.float32)
    return y.reshape(B, S, D).astype(h.dtype)


def setup_inputs(seed: int = 0) -> dict:
    key = jax.random.key(seed)
    ks = jax.random.split(key, 20)
    f32 = jnp.float32
    L = DEPTH

    def nrm(k, shape, scale):
        return jax.random.normal(k, shape, f32) * scale

    return {
        "x": nrm(ks[0], (BATCH, SEQ, D_MODEL), 1.0),
        "g_mix": 1.0 + nrm(ks[1], (L, D_MODEL), 0.02),
        "w_in": nrm(ks[2], (L, D_MODEL, D_IN), D_MODEL ** -0.5),
        "q_norm": 1.0 + nrm(ks[3], (L, HEAD_DIM), 0.02),
        "k_norm": 1.0 + nrm(ks[4], (L, HEAD_DIM), 0.02),
        "hgrn_norm": 1.0 + nrm(ks[5], (L, HGRN_DV), 0.02),
        "lb_fwd": nrm(ks[6], (L + 1, HGRN_K_W), 0.5),
        "lb_bwd": nrm(ks[7], (L + 1, HGRN_K_W), 0.5),
        "w_attn_branch": nrm(ks[8], (L, ATTN_Q_W, D_MODEL), ATTN_Q_W ** -0.5),
        "w_hgrn_branch": nrm(ks[9], (L, HGRN_V_W, D_MODEL), HGRN_V_W ** -0.5),
        "w_out": nrm(ks[10], (L, D_MODEL, D_MODEL), D_MODEL ** -0.5),
        "g_ffn": 1.0 + nrm(ks[11], (L, D_MODEL), 0.02),
        "w_router_group": nrm(ks[12], (L, D_MODEL, N_GROUPS), D_MODEL ** -0.5),
        "b_router_group": nrm(ks[13], (L, N_GROUPS), 0.01),
        "w_router_expert": nrm(ks[14], (L, D_MODEL, N_EXPERTS), D_MODEL ** -0.5),
        "b_router_expert": nrm(ks[15], (L, N_EXPERTS), 0.01),
        "w_exp_gate": nrm(ks[16], (L, N_EXPERTS, D_MODEL, D_EXPERT), D_MODEL ** -0.5),
        "w_exp_up": nrm(ks[17], (L, N_EXPERTS, D_MODEL, D_EXPERT), D_MODEL ** -0.5),
        "w_exp_down": nrm(ks[18], (L, N_EXPERTS, D_EXPERT, D_MODEL), D_EXPERT ** -0.5),
    }


def reference(x, g_mix, w_in, q_norm, k_norm, hgrn_norm, lb_fwd, lb_bwd, w_attn_branch, w_hgrn_branch,
              w_out, g_ffn, w_router_group, b_router_group, w_router_expert, b_router_expert,
              w_exp_gate, w_exp_up, w_exp_down):
    lbs_f = jnp.cumsum(jax.nn.softmax(lb_fwd.astype(jnp.float32), axis=0), axis=0)
    lbs_b = jnp.cumsum(jax.nn.softmax(lb_bwd.astype(jnp.float32), axis=0), axis=0)
    for l in range(DEPTH):
        h = rmsnorm(x, g_mix[l])
        proj = h @ w_in[l]
        aq, ak, av, hq, hf_f, hf_b, hi, hg, gate_a, gate_b = jnp.split(proj, SPLIT_POINTS, axis=-1)
        attn_o = bidir_gqa(aq, ak, av, q_norm[l], k_norm[l])
        hgrn_o = hgrn2_bidir(hq, hf_f, hf_b, hi, hg, lbs_f[l], lbs_b[l], hgrn_norm[l])
        merged = (jax.nn.sigmoid(gate_a) * (attn_o @ w_attn_branch[l])
                  + jax.nn.sigmoid(gate_b) * (hgrn_o @ w_hgrn_branch[l]))
        x = x + merged @ w_out[l]
        h = rmsnorm(x, g_ffn[l])
        x = x + hier_moe(h, w_router_group[l], b_router_group[l], w_router_expert[l], b_router_expert[l],
                         w_exp_gate[l], w_exp_up[l], w_exp_down[l])
    return x
```

```python
import numpy as np
import ml_dtypes
from contextlib import ExitStack
import concourse.bass as bass
import concourse.mybir as mybir
from concourse.bass_utils import run_bass_kernel_spmd

F32 = mybir.dt.float32
BF16 = mybir.dt.bfloat16
I32 = mybir.dt.int32
ALU = mybir.AluOpType
AF = mybir.ActivationFunctionType
AX = mybir.AxisListType

D = 1024
EPS = 1e-6
HGRN_SCALE = 128 ** -0.5
EPOCH = 30000
BIG = 1.0e30
TROWS = 512
OPT = {'spb_act': True, 'hla': True}


class Ctx:
    def __init__(self, nc):
        self.nc = nc
        self.E = {'pe': nc.tensor, 'act': nc.scalar, 'dve': nc.vector, 'pool': nc.gpsimd, 'sp': nc.sync}
        self.cnt = {e: 0 for e in self.E}
        self.known = {e: {} for e in self.E}
        self.esem = {e: [] for e in self.E}
        self.st = {}
        self.dsem = {}
        self.dcount = {}
        self.free_dsems = []
        self.nsem = 0
        self.all_dsems = []

    def newsem(self):
        self.nsem += 1
        return self.nc.alloc_semaphore(f"sm{self.nsem}")

    def _need(self, eng, tok):
        _, sem, val = tok
        k = self.known[eng]
        if k.get(id(sem), 0) >= val:
            return
        self.E[eng].wait_ge(sem, val)
        k[id(sem)] = val

    def _deps(self, eng, r, w, selfsync):
        deps = []
        for key in r:
            s = self.st.get(key)
            if s and s['w']:
                deps.append(s['w'])
        for key in w:
            s = self.st.get(key)
            if s:
                if s['w']:
                    deps.append(s['w'])
                deps.extend(s['r'].values())
        for tok in deps:
            if tok[0] == eng and not selfsync:
                continue
            self._need(eng, tok)

    def _mark(self, tok, r, w):
        for key in r:
            self.st.setdefault(key, {'w': None, 'r': {}})['r'][id(tok[1])] = tok
        for key in w:
            self.st[key] = {'w': tok, 'r': {}}

    def op(self, eng, fn, r=(), w=(), selfsync=True):
        self._deps(eng, r, w, selfsync)
        ins = fn()
        self.cnt[eng] += 1
        n = self.cnt[eng]
        idx = (n - 1) // EPOCH
        while len(self.esem[eng]) <= idx:
            self.esem[eng].append(self.newsem())
        sem = self.esem[eng][idx]
        val = (n - 1) % EPOCH + 1
        ins.then_inc(sem, 1)
        tok = (eng, sem, val)
        self._mark(tok, r, w)
        return tok

    def _dsem_for(self, key):
        if key not in self.dsem:
            if self.free_dsems:
                sem = self.free_dsems.pop()
            else:
                sem = self.newsem()
                self.all_dsems.append(sem)
                self.dcount[id(sem)] = 0
            self.dsem[key] = sem
        return self.dsem[key]

    def dma(self, q, fn, r=(), w=(), semkey=None):
        self._deps(q, r, w, True)
        key = semkey or (w[0] if w else r[0])
        sem = self._dsem_for(key)
        ins = fn()
        ins.then_inc(sem, 16)
        self.dcount[id(sem)] += 1
        tok = ('dma', sem, 16 * self.dcount[id(sem)])
        self._mark(tok, r, w)
        return tok

    def finalize_group(self, semkey, keys):
        sem = self.dsem[semkey]
        tok = ('dma', sem, 16 * self.dcount[id(sem)])
        for key in keys:
            self.st[key] = {'w': tok, 'r': {}}

    def barrier(self):
        toks = []
        for e in self.E:
            n = self.cnt[e]
            if n > 0:
                idx = (n - 1) // EPOCH
                toks.append((e, self.esem[e][idx], (n - 1) % EPOCH + 1))
        for sem in self.all_dsems:
            c = self.dcount[id(sem)]
            if c > 0:
                toks.append(('dma', sem, 16 * c))
        for e in self.E:
            for tok in toks:
                if tok[0] == e:
                    continue
                self._need(e, tok)
        self.st = {}
        self.free_dsems = list(self.all_dsems)
        self.dsem = {}


def build(S, debug=False):
    NB = S // 512
    NT = S // 128
    NCH = S // 32
    NTILES = (2 * S) // TROWS + 32
    NROWS = NTILES * TROWS
    nc = bass.Bass("TRN2", target_bir_lowering=False)
    cx = Ctx(nc)
    es = ExitStack()

    def din(name, shape, dt=F32):
        return nc.dram_tensor(name, list(shape), dt, kind="ExternalInput").ap()

    def dscr(name, shape, dt):
        return nc.dram_tensor(name, list(shape), dt, kind="ExternalOutput" if debug else "Internal").ap()

    xT_d = din("xT", [D, S])
    x_d = din("x", [S, D])
    w1_d = din("w1", [128, 8, 2944])
    w3_d = din("w3", [128, 8, 2560])
    wa_d = din("wa", [128, 4, 1024])
    wb_d = din("wb", [128, 4, 1024])
    wo_d = din("wo", [128, 8, 1024])
    wr_d = din("wr", [128, 8, 36])
    wg_d = din("wg", [32 * 128 * 2, 2048])
    wu_d = din("wu", [32 * 128 * 2, 2048])
    wd_d = din("wd", [32 * 128 * 2, 2048])
    cos_d = din("cosT", [128, S])
    sin_d = din("sinT", [128, S])
    vec_d = din("vecs", [128, 64])
    lbf_d = din("lbf", [128, 2, 2, 4])
    gffn_bc_d = din("gffn_bc", [128, 1024])
    rbias_d = din("rbias", [128, 36])
    cm_d = din("cmats", [5, 128, 128])
    msk_d = din("masks", [2, 128, 128])
    seg_d = din("segmask", [128, 512])
    misc_d = din("misc", [128, 64 + 128])

    out_d = nc.dram_tensor("out", [S, D], F32, kind="ExternalOutput").ap()

    QT = dscr("QT", [4, 128, S], BF16)
    KT = dscr("KT", [2, 128, S], BF16)
    Vd = dscr("Vd", [S, 128], BF16)
    Vi = dscr("Vi", [S, 512], BF16)
    QD = dscr("QD", [2, 4, 128, S], BF16)
    KD = dscr("KD", [2, 4, 128, S], BF16)
    KS = dscr("KS", [2, 4, S, 128], BF16)
    COLS = dscr("COLS", [2, 4, 128, 3, NCH], F32)
    OD = dscr("OD", [2, 4, 128, S], BF16)
    AO = dscr("AO", [4, 128, S], BF16)
    X2 = dscr("X2", [S, D], F32)
    H2 = dscr("H2", [S, D], BF16)
    XS = dscr("XS", [NROWS, D], BF16)
    YS = dscr("YS", [NROWS, D], BF16)

    uniq = [0]

    def sbuf(stack, name, shape, dt):
        uniq[0] += 1
        return stack.enter_context(nc.sbuf_tensor(f"{name}_u{uniq[0]}", list(shape), dt))

    def psum(stack, name, shape, dt):
        uniq[0] += 1
        return stack.enter_context(nc.psum_tensor(f"{name}_u{uniq[0]}", list(shape), dt))

    def mm(out, lhsT, rhs, start, stop, r, w):
        return cx.op('pe', lambda: nc.tensor.matmul(out, lhsT=lhsT, rhs=rhs, start=start, stop=stop), r, w, selfsync=False)

    def tr(out, in_, ident, r, w):
        return cx.op('pe', lambda: nc.tensor.transpose(out, in_, ident), r, w, selfsync=False)

    def act(out, in_, func, r, w, scale=1.0, bias=0.0, accum_out=None):
        if accum_out is None:
            return cx.op('act', lambda: nc.scalar.activation(out=out, in_=in_, func=func, bias=bias, scale=scale), r, w)
        return cx.op('act', lambda: nc.scalar.activation(out=out, in_=in_, func=func, bias=bias, scale=scale, accum_out=accum_out), r, w)

    def tt(eng, out, in0, in1, op, r, w):
        e = nc.vector if eng == 'dve' else nc.gpsimd
        return cx.op(eng, lambda: e.tensor_tensor(out=out, in0=in0, in1=in1, op=op), r, w)

    def ts(eng, out, in0, s1, s2, op0, op1, r, w):
        e = nc.vector if eng == 'dve' else nc.gpsimd
        if s2 is None:
            return cx.op(eng, lambda: e.tensor_scalar(out=out, in0=in0, scalar1=s1, scalar2=None, op0=op0), r, w)
        return cx.op(eng, lambda: e.tensor_scalar(out=out, in0=in0, scalar1=s1, scalar2=s2, op0=op0, op1=op1), r, w)

    def stt(out, in0, scalar, in1, op0, op1, r, w):
        return cx.op('dve', lambda: nc.vector.scalar_tensor_tensor(out=out, in0=in0, scalar=scalar, in1=in1, op0=op0, op1=op1), r, w)

    def recip(out, in_, r, w):
        return cx.op('dve', lambda: nc.vector.reciprocal(out=out, in_=in_), r, w)

    def cp(eng, out, in_, r, w):
        if eng == 'act':
            return cx.op('act', lambda: nc.scalar.copy(out=out, in_=in_), r, w)
        e = nc.vector if eng == 'dve' else nc.gpsimd
        return cx.op(eng, lambda: e.tensor_copy(out=out, in_=in_), r, w)

    def red(out, in_, op, r, w):
        return cx.op('dve', lambda: nc.vector.tensor_reduce(out=out, in_=in_, axis=AX.X, op=op), r, w)

    def ld(q, out, in_, r, w, semkey=None):
        e = cx.E[q]
        return cx.dma(q, lambda: e.dma_start(out=out, in_=in_), r, w, semkey)

    def load_cast(dst, src, n, key):
        o = 0
        while o < n:
            m = min(2048, n - o)
            ld('pool', dst[:, o:o + m], src[:, o:o + m], (), (key,), semkey='const')
            o += m

    cm = sbuf(es, "cm", [128, 5, 128], BF16)
    cmf = sbuf(es, "cmf", [128, 5, 128], F32)
    vecs = sbuf(es, "vecs_sb", [128, 64], F32)
    misc = sbuf(es, "misc_sb", [128, 192], F32)
    M12 = sbuf(es, "M12", [128, NT, 2, 32], F32)
    RK = sbuf(es, "RK", [128, NT, 2], F32)
    C12 = sbuf(es, "C12", [128, NT, 2], F32)
    Macc = sbuf(es, "Macc", [128, 32], F32)
    WI = sbuf(es, "WI", [128, NTILES, 2], I32)
    DI = sbuf(es, "DI", [128, NT, 2], I32)
    lbt = sbuf(es, "lbt", [128, 2, 2, 4], F32)
    lbc = sbuf(es, "lbc", [128, 8], F32)
    oml = sbuf(es, "oml", [128, 8], F32)

    for i in range(5):
        ld('sp', cmf[:, i, :], cm_d[i], (), ('cst',), semkey='const')
    ld('sp', vecs[:], vec_d, (), ('cst',), semkey='const')
    ld('sp', misc[:], misc_d, (), ('cst',), semkey='const')
    ld('sp', lbt[:], lbf_d, (), ('cst',), semkey='const')
    cx.finalize_group('const', ['cst'])
    cp('dve', cm[:], cmf[:], ('cst',), ('cm',))
    cx.op('pool', lambda: nc.gpsimd.memset(Macc[:], 0.0), (), ('Macc',))
    tt('dve', lbc[:].rearrange("p (d h) -> p d h", d=2), lbt[:, :, 1, :], lbt[:, :, 0, :], ALU.subtract, ('cst',), ('lbc',))
    act(lbc[:], lbc[:], AF.Exp, ('lbc',), ('lbc',))
    ts('dve', lbc[:], lbc[:], 1.0, None, ALU.add, None, ('lbc',), ('lbc',))
    recip(lbc[:], lbc[:], ('lbc',), ('lbc',))
    ts('dve', oml[:], lbc[:], -1.0, 1.0, ALU.mult, ALU.add, ('lbc',), ('oml',))

    ident = cm[:, 0, :]
    ones_bf = cm[:, 1, :]
    blk64 = cm[:, 2, :]
    swapm = cm[:, 3, :]
    ones_f = cmf[:, 1, :]
    ustrict_f = cmf[:, 4, :]

    def emit_norm(blk, xTs, sq, hT, rt, rstd, ps_ss, gcol0):
        t0 = blk * 512
        for c in range(8):
            ld('sp' if c % 2 == 0 else 'act', xTs[:, c, :], xT_d[c * 128:(c + 1) * 128, t0:t0 + 512], (), (f'xT{c}',))
        for c in range(8):
            act(sq[:, c % 2, :], xTs[:, c, :], AF.Square, (f'xT{c}',), (f'sq{c % 2}',))
            mm(ps_ss[:], ones_bf, sq[:, c % 2, :], c == 0, c == 7, (f'sq{c % 2}', 'cm'), ('ps_ss',))
        act(rt[:], ps_ss[:], AF.Sqrt, ('ps_ss',), ('rt',), scale=1.0 / D, bias=EPS)
        recip(rstd[:], rt[:], ('rt',), ('rstd',))
        for c in range(8):
            stt(hT[:, c, :], xTs[:, c, :], vecs[:, gcol0 + c:gcol0 + c + 1], rstd[:], ALU.mult, ALU.mult,
                (f'xT{c}', 'rstd', 'cst'), (f'hT{c}',))

    with ExitStack() as ps1:
        W1 = sbuf(ps1, "W1", [128, 8, 2944], BF16)
        for c in range(8):
            load_cast(W1[:, c, :], w1_d[:, c, :], 2944, 'W1')
        cx.finalize_group('const', ['W1'])
        xTs = sbuf(ps1, "xTs", [128, 8, 512], F32)
        sq = sbuf(ps1, "sq", [128, 2, 512], BF16)
        hT = sbuf(ps1, "hT", [128, 8, 512], BF16)
        rt = sbuf(ps1, "rt", [128, 512], F32)
        rstd = sbuf(ps1, "rstd", [128, 512], F32)
        cos_sb = sbuf(ps1, "cos_sb", [128, 512], F32)
        sin_sb = sbuf(ps1, "sin_sb", [128, 512], F32)
        seg = sbuf(ps1, "seg", [128, 512], F32)
        msk = None
        QTst = sbuf(ps1, "QTst", [128, 6, 512], BF16)
        QDst = sbuf(ps1, "QDst", [128, 8, 512], BF16)
        KDst = sbuf(ps1, "KDst", [128, 8, 512], BF16)
        KSst = sbuf(ps1, "KSst", [128, 8, 4, 128], BF16)
        VIst = sbuf(ps1, "VIst", [128, 4, 640], BF16)
        CLst = sbuf(ps1, "CLst", [128, 8, 3, 16], F32)
        sqh = sbuf(ps1, "sqh", [128, 4, 512], BF16)
        sqq = sbuf(ps1, "sqq", [128, 2, 512], BF16)
        tq = sbuf(ps1, "tq", [128, 512], F32)
        rq = sbuf(ps1, "rq", [128, 512], F32)
        qn = sbuf(ps1, "qn", [128, 2, 512], BF16)
        t1 = sbuf(ps1, "t1", [128, 512], F32)
        t2 = sbuf(ps1, "t2", [128, 512], F32)
        TA = [sbuf(ps1, f"TA{i}", [128, 512], F32) for i in range(2)]
        TB = [sbuf(ps1, f"TB{i}", [128, 512], F32) for i in range(2)]
        TC = [sbuf(ps1, f"TC{i}", [128, 512], F32) for i in range(2)]
        TD = [sbuf(ps1, f"TD{i}", [128, 512], F32) for i in range(2)]
        TE = [sbuf(ps1, f"TE{i}", [128, 512], F32) for i in range(2)]
        cl = [sbuf(ps1, f"cl{i}", [128, 16], F32) for i in range(2)]
        ksT = [sbuf(ps1, f"ksT{i}", [128, 512], BF16) for i in range(2)]
        ps_ss = psum(ps1, "ps_ss", [128, 512], F32)
        pp = [psum(ps1, f"pp{i}", [128, 512], F32) for i in range(3)]
        ps_sum = psum(ps1, "ps_sum", [128, 512], F32)
        ps_swap = psum(ps1, "ps_swap", [128, 512], F32)
        psT = [psum(ps1, f"psT{i}", [128, 1024], BF16) for i in range(2)]
        ld('sp', seg[:], seg_d, (), ('seg',))
        pi = 0
        it = 0
        dq = []

        def defer(n, fn):
            dq.append([n, fn])

        def tick():
            for e in dq:
                e[0] -= 1
            ready = [e for e in dq if e[0] <= 0]
            for e in ready:
                dq.remove(e)
            for e in ready:
                e[1]()

        def f_chain(j, P, pk, s, t0, blk):
            steps = []
            dh = j - 10
            d = dh // 4
            hh = dh % 4
            A, B, C, Dd, Eb = TA[s], TB[s], TC[s], TD[s], TE[s]
            kA, kB, kC, kD, kE = f'TA{s}', f'TB{s}', f'TC{s}', f'TD{s}', f'TE{s}'
            C3 = C[:].rearrange("p (n c) -> p n c", c=32)
            D3 = Dd[:].rearrange("p (n c) -> p n c", c=32)
            ref = 16 if d == 0 else 15
            last = 31 if d == 0 else 0
            clk = f'cl{s}'
            ck = f'CLst{dh}'
            kst = ksT[s]
            steps.append(lambda: act(A[:], P[:], AF.Exp, (pk,), (kA,), scale=-1.0))
            steps.append(lambda: ts('pool', A[:], A[:], 1.0, None, ALU.add, None, (kA,), (kA,)))
            steps.append(lambda: recip(A[:], A[:], (kA,), (kA,)))
            steps.append(lambda: ts('dve', A[:], A[:], oml[:, dh:dh + 1], lbc[:, dh:dh + 1], ALU.mult, ALU.add, (kA, 'oml', 'lbc'), (kA,)))
            steps.append(lambda: act(B[:], A[:], AF.Ln, (kA,), (kB,)))
            steps.append(lambda: cx.op('dve', lambda: nc.vector.tensor_tensor_scan(out=C[:], data0=seg[:], data1=B[:], initial=0.0,
                                                                                  op0=ALU.mult, op1=ALU.add), (kB, 'seg'), (kC,)))
            if d == 1:
                steps.append(lambda: tt('dve', D3, C3[:, :, 31:32].to_broadcast([128, 16, 32]), C3, ALU.subtract, (kC,), (kD,)))
                steps.append(lambda: tt('dve', C[:], Dd[:], B[:], ALU.add, (kD, kB), (kC,)))
            steps.append(lambda: tt('dve', D3, C3, C3[:, :, ref:ref + 1].to_broadcast([128, 16, 32]), ALU.subtract, (kC,), (kD,)))
            steps.append(lambda: tt('dve', cl[s][:].rearrange("p (n o) -> p n o", o=1), C3[:, :, last:last + 1], C3[:, :, ref:ref + 1], ALU.subtract, (kC,), (clk,)))
            steps.append(lambda: act(B[:], Dd[:], AF.Exp, (kD,), (kB,)))
            steps.append(lambda: act(Eb[:], Dd[:], AF.Exp, (kD,), (kE,), scale=-1.0))

            def cols_():
                act(CLst[:, dh, 0, :], cl[s][:], AF.Exp, (clk,), (ck,))
                act(CLst[:, dh, 1, :].rearrange("p (n o) -> p n o", o=1), C3[:, :, ref:ref + 1], AF.Exp, (kC,), (ck,))
                act(CLst[:, dh, 2, :].rearrange("p (n o) -> p n o", o=1), C3[:, :, last:last + 1], AF.Exp, (kC,), (ck,))
                ld('sp', COLS[d, hh][:, :, blk * 16:(blk + 1) * 16], CLst[:, dh, :, :], (ck,), ())
            steps.append(cols_)
            steps.append(lambda: stt(QDst[:, dh, :], sqh[:, hh, :], HGRN_SCALE, B[:], ALU.mult, ALU.mult, (f'sqh{hh}', kB), (f'QDst{dh}',)))
            steps.append(lambda: ts('pool', A[:], A[:], -1.0, 1.0, ALU.mult, ALU.add, (kA,), (kA,)))
            steps.append(lambda: tt('dve', KDst[:, dh, :], A[:], Eb[:], ALU.mult, (kA, kE), (f'KDst{dh}',)))

            def stores_():
                ld('sp', QD[d, hh][:, t0:t0 + 512], QDst[:, dh, :], (f'QDst{dh}',), ())
                ld('act', KD[d, hh][:, t0:t0 + 512], KDst[:, dh, :], (f'KDst{dh}',), ())
            steps.append(stores_)
            steps.append(lambda: tt('dve', kst[:].rearrange("p (n c) -> p n c", c=32), KDst[:, dh, :].rearrange("p (n c) -> p n c", c=32),
                                    CLst[:, dh, 0, :].rearrange("p (n o) -> p n o", o=1).to_broadcast([128, 16, 32]), ALU.mult,
                                    (f'KDst{dh}', ck), (f'ksT{s}',)))

            def stage_t():
                pT = psT[s]
                for t4 in range(4):
                    tr(pT[:, t4 * 128:(t4 + 1) * 128], kst[:, t4 * 128:(t4 + 1) * 128], ident, (f'ksT{s}', 'cm'), (f'psT{s}',))
                cp('act', KSst[:, dh, :, :], pT[:, 0:512].rearrange("p (t k) -> p t k", k=128), (f'psT{s}',), (f'KSst{dh}',))
                ld('sp', KS[d, hh][t0:t0 + 512, :].rearrange("(t p) k -> p t k", p=128), KSst[:, dh, :, :], (f'KSst{dh}',), ())
            steps.append(lambda: defer(2, stage_t))
            return steps

        pend_chain = None
        for blk in range(NB):
            t0 = blk * 512
            emit_norm(blk, xTs, sq, hT, rt, rstd, ps_ss, 0)
            ld('sp', cos_sb[:], cos_d[:, t0:t0 + 512], (), ('cos',))
            ld('act', sin_sb[:], sin_d[:, t0:t0 + 512], (), ('sin',))
            hr = tuple(f'hT{c}' for c in range(8))
            for t4 in range(4):
                P = pp[pi]; pk = f'pp{pi}'; pi = (pi + 1) % 3
                for c in range(8):
                    mm(P[:], hT[:, c, t4 * 128:(t4 + 1) * 128], W1[:, c, 2304 + 128:2304 + 640], c == 0, c == 7, hr + ('W1',), (pk,))
                for c in range(8):
                    mm(ps_sum[:, 0:128], hT[:, c, t4 * 128:(t4 + 1) * 128], W1[:, c, 2304:2304 + 128], c == 0, c == 7, hr + ('W1',), ('ps_sum',))
                tick()
                cp('act', VIst[:, t4, 128:640], P[:], (pk,), (f'VIst{t4}',))
                cp('dve', VIst[:, t4, 0:128], ps_sum[:, 0:128], ('ps_sum',), (f'VIst{t4}',))
                ld('sp', Vi[t0 + t4 * 128:t0 + (t4 + 1) * 128, :], VIst[:, t4, 128:640], (f'VIst{t4}',), ())
                ld('sp', Vd[t0 + t4 * 128:t0 + (t4 + 1) * 128, :], VIst[:, t4, 0:128], (f'VIst{t4}',), ())
            for j in range(18):
                P = pp[pi]; pk = f'pp{pi}'; pi = (pi + 1) % 3
                for c in range(8):
                    mm(P[:], W1[:, c, j * 128:(j + 1) * 128], hT[:, c, :], c == 0, c == 7, hr + ('W1',), (pk,))
                tick()
                if j < 6:
                    q2 = j % 2
                    act(sqq[:, q2, :], P[:], AF.Square, (pk,), (f'sqq{q2}',))

                    def stage_a(j=j, P=P, pk=pk, q2=q2):
                        mm(ps_sum[:], blk64, sqq[:, q2, :], True, True, (f'sqq{q2}', 'cm'), ('ps_sum',))
                        act(tq[:], ps_sum[:], AF.Sqrt, ('ps_sum',), ('tq',), scale=1.0 / 64, bias=EPS)
                        recip(rq[:], tq[:], ('tq',), ('rq',))
                        gc = 16 if j < 4 else 17
                        stt(qn[:, q2, :], P[:], vecs[:, gc:gc + 1], rq[:], ALU.mult, ALU.mult, (pk, 'rq', 'cst'), (f'qn{q2}',))

                    def stage_b(j=j, q2=q2, t0=t0):
                        mm(ps_swap[:], swapm, qn[:, q2, :], True, True, (f'qn{q2}', 'cm'), ('ps_swap',))
                        tt('pool', t1[:], qn[:, q2, :], cos_sb[:], ALU.mult, (f'qn{q2}', 'cos'), ('t1',))
                        tt('dve', t2[:], ps_swap[:], sin_sb[:], ALU.mult, ('ps_swap', 'sin'), ('t2',))
                        tt('pool', QTst[:, j, :], t1[:], t2[:], ALU.add, ('t1', 't2'), (f'QTst{j}',))
                        dst = QT[j][:, t0:t0 + 512] if j < 4 else KT[j - 4][:, t0:t0 + 512]
                        ld('act', dst, QTst[:, j, :], (f'QTst{j}',), ())

                    defer(1, stage_a)
                    defer(2, stage_b)
                elif j < 10:
                    hh = j - 6
                    act(sqh[:, hh, :], P[:], AF.Silu, (pk,), (f'sqh{hh}',))
                else:
                    s_ = it % 2
                    it += 1
                    chain = f_chain(j, P, pk, s_, t0, blk)
                    if pend_chain is None:
                        pend_chain = chain
                    else:
                        a_, b_ = pend_chain, chain
                        pend_chain = None
                        for k in range(max(len(a_), len(b_))):
                            if k < len(a_):
                                a_[k]()
                            if k < len(b_):
                                b_[k]()
        while dq:
            tick()
        cx.barrier()

    with ExitStack() as ps2:
        KTs = sbuf(ps2, "KTs", [128, 2, 2, S], BF16)
        Vs = sbuf(ps2, "Vs", [128, NT, 128], BF16)
        Vp0 = sbuf(ps2, "Vp0", [128, NT, 2, 65], BF16)
        Vp1 = sbuf(ps2, "Vp1", [128, NT, 2, 128], BF16)
        Qs = [sbuf(ps2, f"Qs{i}", [128, 512], BF16) for i in range(2)]
        Pt = [sbuf(ps2, f"Pt{i}", [128, 512], BF16) for i in range(4)]
        rs = sbuf(ps2, "rs", [128, 512], F32)
        rb = sbuf(ps2, "rb", [128, 512], F32)
        AOst = [sbuf(ps2, f"AOst{i}", [128, 512], BF16) for i in range(2)]
        pss = [psum(ps2, f"pss{i}", [128, 512], F32) for i in range(3)]
        po = [psum(ps2, f"po{i}", [128, 512], F32) for i in range(2)]
        pb = psum(ps2, "pb", [128, 512], F32)
        cx.op('pool', lambda: nc.gpsimd.memset(KTs[64:128, :, 0, :], 0.0), (), ('KTz0',))
        cx.op('pool', lambda: nc.gpsimd.memset(KTs[0:64, :, 1, :], 0.0), (), ('KTz1',))
        for g in range(2):
            ld('sp', KTs[0:64, g, 0, :], KT[g][0:64, :], (), ('KTs',), semkey='const')
            ld('act', KTs[64:128, g, 1, :], KT[g][64:128, :], (), ('KTs',), semkey='const')
        ld('act', Vs[:], Vd.rearrange("(t p) f -> p t f", p=128), (), ('Vs',), semkey='const')
        cx.finalize_group('const', ['KTs', 'Vs'])
        cx.op('pool', lambda: nc.gpsimd.memset(Vp0[:], 1.0), (), ('Vp0',))
        cx.op('pool', lambda: nc.gpsimd.memset(Vp1[:], 0.0), (), ('Vp1',))
        cx.op('pool', lambda: nc.gpsimd.memset(Vp1[:, :, :, 0:1], 1.0), ('Vp1',), ('Vp1',))
        for g in range(2):
            cp('dve', Vp0[:, :, g, 0:64], Vs[:, :, g * 64:(g + 1) * 64], ('Vs', 'Vp0'), ('Vp0',))
            cp('dve', Vp1[:, :, g, 64:128], Vs[:, :, g * 64:(g + 1) * 64], ('Vs', 'Vp1'), ('Vp1',))
        heads = [(qb, c, hl) for qb in range(NB) for c in range(4) for hl in range(2)]
        NH = len(heads)
        NI = NH * NT
        LA = 2

        def qload(ci):
            qb, c = ci // 4, ci % 4
            ld('sp', Qs[ci % 2][:], QT[c][:, qb * 512:(qb + 1) * 512], (), (f'Qs{ci % 2}',))

        def emit_S(i):
            hi, kc = i // NT, i % NT
            qb, c, hl = heads[hi]
            ci = qb * 4 + c
            if kc == 0 and hl == 0 and ci + 1 < NB * 4:
                qload(ci + 1)
            hp = hl * 64
            g = c // 2
            mm(pss[i % 3][:], KTs[:, g, hl, kc * 128:(kc + 1) * 128], Qs[ci % 2][:, :], True, True,
               ('KTs', 'KTz0', 'KTz1', f'Qs{ci % 2}'), (f'pss{i % 3}',))

        def emit_exp(i):
            act(Pt[i % 4][:], pss[i % 3][:], AF.Exp, (f'pss{i % 3}',), (f'Pt{i % 4}',), scale=0.125)

        def emit_PV(i):
            hi, kc = i // NT, i % NT
            qb, c, hl = heads[hi]
            g = c // 2
            O = po[hi % 2]; ok = f'po{hi % 2}'
            if hl == 0:
                mm(O[0:65, :], Vp0[:, kc, g, :], Pt[i % 4][:], kc == 0, kc == NT - 1, ('Vp0', f'Pt{i % 4}'), (ok,))
            else:
                mm(O[:, :], Vp1[:, kc, g, :], Pt[i % 4][:], kc == 0, kc == NT - 1, ('Vp1', f'Pt{i % 4}'), (ok,))

        def epilogue(hi):
            qb, c, hl = heads[hi]
            O = po[hi % 2]; ok = f'po{hi % 2}'
            ao = AOst[c % 2]; aok = f'AOst{c % 2}'
            if hl == 0:
                recip(rs[64:65, :], O[64:65, :], (ok,), ('rs',))
                mm(pb[0:64, :], cmf[64:65, 1, 0:64], rs[64:65, :], True, True, ('rs', 'cst'), ('pb',))
                cp('act', rb[0:64, :], pb[0:64, :], ('pb',), ('rb',))
                tt('dve', ao[0:64, :], O[0:64, :], rb[0:64, :], ALU.mult, (ok, 'rb'), (aok,))
            else:
                recip(rs[0:1, :], O[0:1, :], (ok,), ('rs',))
                mm(pb[:, :], cmf[0:1, 1, :], rs[0:1, :], True, True, ('rs', 'cst'), ('pb',))
                cp('act', rb[64:128, :], pb[64:128, :], ('pb',), ('rb',))
                tt('dve', ao[64:128, :], O[64:128, :], rb[64:128, :], ALU.mult, (ok, 'rb'), (aok,))
                ld('act', AO[c][:, qb * 512:(qb + 1) * 512], ao[:], (aok,), ())

        qload(0)
        epi_at = min(6, NT - 1)
        for i in range(-LA, NI):
            if i + LA < NI:
                emit_S(i + LA)
            if i >= 0:
                emit_exp(i)
                emit_PV(i)
                hi, kc = i // NT, i % NT
                if kc == epi_at and hi > 0:
                    epilogue(hi - 1)
        epilogue(NH - 1)
        cx.barrier()

    with ExitStack() as ps3:
        msk = sbuf(ps3, "msk", [128, 2, 128], F32)
        ld('sp', msk[:, 0, :], msk_d[0], (), ('msk',), semkey='const')
        ld('sp', msk[:, 1, :], msk_d[1], (), ('msk',), semkey='const')
        cols = sbuf(ps3, "cols", [128, 8, 3, NCH], F32)
        for d in range(2):
            for hh in range(4):
                ld('act', cols[:, d * 4 + hh, :, :], COLS[d, hh], (), ('cols',), semkey='const')
        cx.finalize_group('const', ['msk', 'cols'])
        for d in range(2):
            with ExitStack() as psd:
                qd = [[sbuf(psd, f"qd{hh}_{i}", [128, 512], BF16) for i in range(2)] for hh in range(4)]
                kd = [[sbuf(psd, f"kd{hh}_{i}", [128, 512], BF16) for i in range(2)] for hh in range(4)]
                ks32 = [[sbuf(psd, f"ks128{hh}_{i}", [128, 4, 128], BF16) for i in range(2)] for hh in range(4)]
                v32 = [[sbuf(psd, f"v32z{hh}_{i}", [128, 16, 128], BF16) for i in range(2)] for hh in range(4)]
                for hh in range(4):
                    for i in range(2):
                        cx.op('pool', lambda: nc.gpsimd.memset(v32[hh][i][:], 0.0), (), (f'v32{hh}_{i}',))
                v128 = [[sbuf(psd, f"v128{hh}_{i}", [128, 4, 128], BF16) for i in range(2)] for hh in range(4)]
                state = [sbuf(psd, f"state{hh}", [128, 128], F32) for hh in range(4)]
                Spb = [sbuf(psd, f"Spb{hh}", [128, 128], BF16) for hh in range(4)]
                Am = [[sbuf(psd, f"Am{hh}_{i}", [128, 128], BF16) for i in range(2)] for hh in range(4)]
                ost = [[sbuf(psd, f"ost{hh}_{i}", [128, 512], BF16) for i in range(2)] for hh in range(4)]
                poh = [psum(psd, f"poh{hh}", [128, 512], F32) for hh in range(4)]
                pA = [psum(psd, f"pA{i}", [128, 512], F32) for i in range(2)]
                pP = [psum(psd, f"pP{i}", [128, 512], F32) for i in range(2)]
                for hh in range(4):
                    cx.op('pool', lambda: nc.gpsimd.memset(state[hh][:], 0.0), (), (f'state{hh}',))
                    cx.op('pool', lambda: nc.gpsimd.memset(Spb[hh][:], 0.0), (), (f'Spb{hh}',))
                blocks = list(range(NB)) if d == 0 else list(range(NB - 1, -1, -1))
                ai = 0
                ppi = 0
                for bi, blk in enumerate(blocks):
                    t0 = blk * 512
                    b2 = bi % 2
                    for hh in range(4):
                        sfx = f'{hh}_{b2}'
                        ld('sp', qd[hh][b2][:], QD[d, hh][:, t0:t0 + 512], (), ('qd' + sfx,))
                        ld('act', kd[hh][b2][:], KD[d, hh][:, t0:t0 + 512], (), ('kd' + sfx,))
                        ld('sp', ks32[hh][b2][:], KS[d, hh][t0:t0 + 512, :].rearrange("(t p) k -> p t k", p=128), (), ('ks32' + sfx,))
                        for cc in range(4):
                            ld('act' if cc % 2 == 0 else 'sp', v32[hh][b2][cc * 32:(cc + 1) * 32, cc::4, :],
                               Vi[t0:t0 + 512, hh * 128:(hh + 1) * 128].rearrange("(t cc c) k -> cc c t k", cc=4, c=32)[cc], (), ('v32' + sfx,))
                        ld('sp', v128[hh][b2][:], Vi[t0:t0 + 512, hh * 128:(hh + 1) * 128].rearrange("(t p) k -> p t k", p=128), (), ('v128' + sfx,))
                    tiles = list(range(4)) if d == 0 else list(range(3, -1, -1))
                    for ti, t4 in enumerate(tiles):
                        for hh in range(4):
                            sfx = f'{hh}_{b2}'
                            tc = slice(t4 * 128, (t4 + 1) * 128)
                            A = pA[ai]; ak = f'pA{ai}'; ai = (ai + 1) % 2
                            mm(A[:, 0:128], kd[hh][b2][:, tc], qd[hh][b2][:, tc], True, True, ('kd' + sfx, 'qd' + sfx), (ak,))
                            am = Am[hh][ti % 2]; amk = f'Am{hh}_{ti % 2}'
                            tt('dve', am[:], A[:, 0:128], msk[:, d, :], ALU.mult, (ak, 'msk'), (amk,))
                            ok = f'poh{hh}'
                            mm(poh[hh][:, tc], v128[hh][b2][:, t4, :], am[:], True, False, ('v128' + sfx, amk), (ok,))
                        chunks = list(range(4)) if d == 0 else list(range(3, -1, -1))
                        for ci, cc in enumerate(chunks):
                            for hh in range(4):
                                sfx = f'{hh}_{b2}'
                                ok = f'poh{hh}'
                                n_loc = t4 * 4 + cc
                                n_glob = blk * 16 + n_loc
                                cs = slice(n_loc * 32, (n_loc + 1) * 32)
                                mm(poh[hh][:, cs], Spb[hh][:], qd[hh][b2][:, cs], False, ci == 3, (f'Spb{hh}', 'qd' + sfx), (ok,))
                                Pp = pP[ppi]; pk = f'pP{ppi}'; ppi = (ppi + 1) % 2
                                mm(Pp[:, 0:128], ks32[hh][b2][:, t4, :], v32[hh][b2][:, n_loc, :], True, True, ('ks32' + sfx, 'v32' + sfx), (pk,))
                                stt(state[hh][:], state[hh][:], cols[:, d * 4 + hh, 2, n_glob:n_glob + 1], Pp[:, 0:128], ALU.mult, ALU.add,
                                    (f'state{hh}', 'cols', pk), (f'state{hh}',))
                                n_next = n_glob + 1 if d == 0 else n_glob - 1
                                if 0 <= n_next < NCH:
                                    ts('dve', Spb[hh][:], state[hh][:], cols[:, d * 4 + hh, 1, n_next:n_next + 1], None, ALU.mult, None,
                                       (f'state{hh}', 'cols'), (f'Spb{hh}',))
                    for hh in range(4):
                        osk = f'ost{hh}_{b2}'
                        cp('act', ost[hh][b2][:], poh[hh][:], (f'poh{hh}',), (osk,))
                        ld('sp', OD[d, hh][:, t0:t0 + 512], ost[hh][b2][:], (osk,), ())
            cx.barrier()

    with ExitStack() as ps4:
        W3 = sbuf(ps4, "W3", [128, 8, 2560], BF16)
        Wa = sbuf(ps4, "Wa", [128, 4, 1024], BF16)
        Wb = sbuf(ps4, "Wb", [128, 4, 1024], BF16)
        Wo = sbuf(ps4, "Wo", [128, 8, 1024], BF16)
        Wr = sbuf(ps4, "Wr", [128, 8, 36], F32)
        gbc = sbuf(ps4, "gbc", [128, 1024], F32)
        rbias = sbuf(ps4, "rbias", [128, 36], F32)
        for c in range(8):
            load_cast(W3[:, c, :], w3_d[:, c, :], 2560, 'W3')
            load_cast(Wo[:, c, :], wo_d[:, c, :], 1024, 'Wo')
        for c in range(4):
            load_cast(Wa[:, c, :], wa_d[:, c, :], 1024, 'Wa')
            load_cast(Wb[:, c, :], wb_d[:, c, :], 1024, 'Wb')
        ld('sp', Wr[:], wr_d, (), ('Wr',), semkey='const')
        ld('sp', gbc[:], gffn_bc_d, (), ('gbc',), semkey='const')
        ld('sp', rbias[:], rbias_d, (), ('rbias',), semkey='const')
        cx.finalize_group('const', ['W3', 'Wo', 'Wa', 'Wb', 'Wr', 'gbc', 'rbias'])
        for c in range(8):
            ts('pool', Wr[:, c, :], Wr[:, c, :], vecs[:, 8 + c:9 + c], None, ALU.mult, None, ('Wr', 'cst'), ('Wr',))
        xTs = sbuf(ps4, "xTs3", [128, 8, 512], F32)
        sq = sbuf(ps4, "sq3", [128, 2, 512], BF16)
        hT = sbuf(ps4, "hT3", [128, 8, 512], BF16)
        rt = sbuf(ps4, "rt3", [128, 512], F32)
        rstd = sbuf(ps4, "rstd3", [128, 512], F32)
        AOs = sbuf(ps4, "AOs", [128, 4, 512], BF16)
        ODs = sbuf(ps4, "ODs", [128, 2, 4, 512], BF16)
        osum = sbuf(ps4, "osum", [128, 1, 512], F32)
        osq = sbuf(ps4, "osq", [128, 512], BF16)
        ort = sbuf(ps4, "ort", [128, 512], F32)
        orr = sbuf(ps4, "orr", [128, 512], F32)
        ho = sbuf(ps4, "ho", [128, 512], F32)
        sog = sbuf(ps4, "sog", [128, 512], BF16)
        HO = sbuf(ps4, "HO", [128, 4, 512], BF16)
        sga = sbuf(ps4, "sga", [128, 512], BF16)
        sgb = sbuf(ps4, "sgb", [128, 512], BF16)
        m1 = sbuf(ps4, "m1", [128, 512], F32)
        m2 = sbuf(ps4, "m2", [128, 512], F32)
        mg = sbuf(ps4, "mg", [128, 8, 512], BF16)
        x2T = xTs
        xtok = [sbuf(ps4, "xtok0", [128, 1024], F32)] * 2
        x2tok = [sbuf(ps4, "x2tok0", [128, 1024], F32)] * 2
        junk = sbuf(ps4, "junk", [128, 1024], BF16)
        h2 = [sbuf(ps4, f"h2{i}", [128, 1024], BF16) for i in range(2)]
        sm = sbuf(ps4, "sm", [128, 64], F32)
        L_all = sbuf(ps4, "L_all", [128, NT, 36], F32)
        smr = [sbuf(ps4, f"smr{i}", [128, 16], F32) for i in range(4)]
        G1r = [sbuf(ps4, f"G1r{i}", [128, 4], F32) for i in range(4)]
        G2r = [sbuf(ps4, f"G2r{i}", [128, 4], F32) for i in range(4)]
        R1r = [sbuf(ps4, f"R1r{i}", [128, 32], F32) for i in range(4)]
        R2r = [sbuf(ps4, f"R2r{i}", [128, 32], F32) for i in range(4)]
        R1 = sbuf(ps4, "R1", [128, 32], F32)
        R2 = sbuf(ps4, "R2", [128, 32], F32)
        R3 = sbuf(ps4, "R3", [128, 32], F32)
        G1 = sbuf(ps4, "G1", [128, 4], F32)
        G2 = sbuf(ps4, "G2", [128, 4], F32)
        cum = sbuf(ps4, "cum", [128, 32], F32)
        p_ss = psum(ps4, "p_ss", [128, 512], F32)
        p_a = psum(ps4, "p_a", [128, 512], F32)
        p_b = psum(ps4, "p_b", [128, 512], F32)
        p_g = [psum(ps4, f"p_g{i}", [128, 512], F32) for i in range(2)]
        p_x = [psum(ps4, f"p_x{i}", [128, 512], F32) for i in range(2)]
        p_l = psum(ps4, "p_l", [128, 512], F32)
        gi = 0
        xi = 0
        for blk in range(NB):
            t0 = blk * 512
            emit_norm(blk, xTs, sq, hT, rt, rstd, p_ss, 0)
            hr = tuple(f'hT{c}' for c in range(8))
            for c in range(4):
                ld('sp', AOs[:, c, :], AO[c][:, t0:t0 + 512], (), (f'AOs{c}',))
                for d in range(2):
                    ld('act', ODs[:, d, c, :], OD[d, c][:, t0:t0 + 512], (), (f'ODs{d}{c}',))
            for hh in range(4):
                tt('pool', osum[:, 0, :], ODs[:, 0, hh, :], ODs[:, 1, hh, :], ALU.add, (f'ODs0{hh}', f'ODs1{hh}'), ('osum',))
                act(osq[:], osum[:, 0, :], AF.Square, ('osum',), ('osq',))
                mm(p_ss[:], ones_bf, osq[:], True, True, ('osq', 'cm'), ('ps_ss',))
                act(ort[:], p_ss[:], AF.Sqrt, ('ps_ss',), ('ort',), scale=1.0 / 128, bias=EPS)
                recip(orr[:], ort[:], ('ort',), ('orr',))
                stt(ho[:], osum[:, 0, :], vecs[:, 18:19], orr[:], ALU.mult, ALU.mult, ('osum', 'orr', 'cst'), ('ho',))
                G = p_g[gi]; gk = f'p_g{gi}'; gi = (gi + 1) % 2
                for c in range(8):
                    mm(G[:], W3[:, c, hh * 128:(hh + 1) * 128], hT[:, c, :], c == 0, c == 7, hr + ('W3',), (gk,))
                act(sog[:], G[:], AF.Silu, (gk,), ('sog',))
                tt('dve', HO[:, hh, :], ho[:], sog[:], ALU.mult, ('ho', 'sog'), (f'HO{hh}',))
            for oc in range(8):
                for c in range(4):
                    mm(p_a[:], Wa[:, c, oc * 128:(oc + 1) * 128], AOs[:, c, :], c == 0, c == 3, (f'AOs{c}', 'Wa'), ('p_a',))
                G = p_g[gi]; gk = f'p_g{gi}'; gi = (gi + 1) % 2
                for c in range(8):
                    mm(G[:], W3[:, c, 512 + oc * 128:512 + (oc + 1) * 128], hT[:, c, :], c == 0, c == 7, hr + ('W3',), (gk,))
                act(sga[:], G[:], AF.Sigmoid, (gk,), ('sga',))
                tt('dve', m1[:], p_a[:], sga[:], ALU.mult, ('p_a', 'sga'), ('m1',))
                for c in range(4):
                    mm(p_b[:], Wb[:, c, oc * 128:(oc + 1) * 128], HO[:, c, :], c == 0, c == 3, (f'HO{c}', 'Wb'), ('p_b',))
                G = p_g[gi]; gk = f'p_g{gi}'; gi = (gi + 1) % 2
                for c in range(8):
                    mm(G[:], W3[:, c, 1536 + oc * 128:1536 + (oc + 1) * 128], hT[:, c, :], c == 0, c == 7, hr + ('W3',), (gk,))
                act(sgb[:], G[:], AF.Sigmoid, (gk,), ('sgb',))
                tt('dve', m2[:], p_b[:], sgb[:], ALU.mult, ('p_b', 'sgb'), ('m2',))
                tt('pool', mg[:, oc, :], m1[:], m2[:], ALU.add, ('m1', 'm2'), (f'mg{oc}',))
            mr = tuple(f'mg{c}' for c in range(8))
            for oc in range(8):
                X = p_x[xi]; xk = f'p_x{xi}'; xi = (xi + 1) % 2
                for c in range(8):
                    mm(X[:], Wo[:, c, oc * 128:(oc + 1) * 128], mg[:, c, :], c == 0, c == 7, mr + ('Wo',), (xk,))
                tt('dve', x2T[:, oc, :], X[:], xTs[:, oc, :], ALU.add, (xk, f'xT{oc}'), (f'xT{oc}',))
            x2r = tuple(f'xT{c}' for c in range(8))
            for t4 in range(4):
                tile_i = blk * 4 + t4
                b2 = 0
                r0 = t0 + t4 * 128
                ld('sp', xtok[b2][:], x_d[r0:r0 + 128, :], (), (f'xtok{b2}',))
                for half in range(2):
                    X = p_x[xi]; xk = f'p_x{xi}'; xi = (xi + 1) % 2
                    for c in range(8):
                        mm(X[:], mg[:, c, t4 * 128:(t4 + 1) * 128], Wo[:, c, half * 512:(half + 1) * 512], c == 0, c == 7, mr + ('Wo',), (xk,))
                    tt('dve', x2tok[b2][:, half * 512:(half + 1) * 512], X[:], xtok[b2][:, half * 512:(half + 1) * 512], ALU.add,
                       (xk, f'xtok{b2}'), (f'x2tok{b2}',))
                ld('act', X2[r0:r0 + 128, :], x2tok[b2][:], (f'x2tok{b2}',), ())
                act(junk[:], x2tok[b2][:], AF.Square, (f'x2tok{b2}',), ('junk',))
                red(sm[:, 0:1], junk[:], ALU.add, ('junk',), ('ssq',))
                act(sm[:, 1:2], sm[:, 0:1], AF.Sqrt, ('ssq',), ('rt2',), scale=1.0 / D, bias=EPS)
                recip(sm[:, 2:3], sm[:, 1:2], ('rt2',), ('r2',))
                stt(h2[b2][:], x2tok[b2][:], sm[:, 2:3], gbc[:], ALU.mult, ALU.mult, (f'x2tok{b2}', 'r2', 'gbc'), (f'h2{b2}',))
                ld('sp', H2[r0:r0 + 128, :], h2[b2][:], (f'h2{b2}',), ())
                for c in range(8):
                    mm(p_l[:, 0:36], x2T[:, c, t4 * 128:(t4 + 1) * 128], Wr[:, c, :], c == 0, c == 7, x2r + ('Wr',), ('p_l',))
                stt(L_all[:, tile_i, :], p_l[:, 0:36], sm[:, 2:3], rbias[:], ALU.mult, ALU.add, ('p_l', 'r2', 'rbias'), (f'L{tile_i}',))
        cx.barrier()

        def route_chain(tile_i, sl):
            st_ = []
            L = L_all[:, tile_i, :]
            smx = smr[sl]
            g1, g2, r1, r2_ = G1r[sl], G2r[sl], R1r[sl], R2r[sl]
            p = f'_{sl}'
            k1 = M12[:, tile_i, 0, :]
            k2 = M12[:, tile_i, 1, :]
            st_.append(lambda: red(smx[:, 3:4], L[:, 0:4], ALU.max, (), ('gmax' + p,)))
            st_.append(lambda: ts('dve', g1[:], L[:, 0:4], smx[:, 3:4], None, ALU.is_ge, None, ('gmax' + p,), ('G1' + p,)))
            st_.append(lambda: ts('dve', smx[:, 4:5], smx[:, 3:4], -1.0, None, ALU.mult, None, ('gmax' + p,), ('ngmax' + p,)))
            st_.append(lambda: act(g2[:], L[:, 0:4], AF.Exp, ('ngmax' + p,), ('G2' + p,), bias=smx[:, 4:5]))
            st_.append(lambda: red(smx[:, 5:6], g2[:], ALU.add, ('G2' + p,), ('gsum' + p,)))
            st_.append(lambda: recip(smx[:, 6:7], smx[:, 5:6], ('gsum' + p,), ('pg' + p,)))
            st_.append(lambda: ts('dve', g1[:], g1[:], -1.0, BIG, ALU.add, ALU.mult, ('G1' + p,), ('G1' + p,)))
            st_.append(lambda: tt('dve', r1[:].rearrange("p (g e) -> p g e", e=8), L[:, 4:36].rearrange("p (g e) -> p g e", e=8),
                                  g1[:].rearrange("p (g o) -> p g o", o=1).to_broadcast([128, 4, 8]), ALU.add, ('G1' + p,), ('R1' + p,)))
            st_.append(lambda: red(smx[:, 7:8], r1[:], ALU.max, ('R1' + p,), ('mx1' + p,)))
            st_.append(lambda: ts('dve', k1, r1[:], smx[:, 7:8], None, ALU.is_ge, None, ('R1' + p, 'mx1' + p), ('k1' + p,)))
            st_.append(lambda: stt(r2_[:], k1, -BIG, r1[:], ALU.mult, ALU.add, ('k1' + p, 'R1' + p), ('R2' + p,)))
            st_.append(lambda: red(smx[:, 8:9], r2_[:], ALU.max, ('R2' + p,), ('mx2' + p,)))
            st_.append(lambda: ts('dve', k2, r2_[:], smx[:, 8:9], None, ALU.is_ge, None, ('R2' + p, 'mx2' + p), ('k2' + p,)))
            st_.append(lambda: tt('dve', smx[:, 9:10], smx[:, 8:9], smx[:, 7:8], ALU.subtract, ('mx1' + p, 'mx2' + p), ('dm' + p,)))
            st_.append(lambda: act(smx[:, 10:11], smx[:, 9:10], AF.Exp, ('dm' + p,), ('edm' + p,)))
            st_.append(lambda: ts('dve', smx[:, 10:11], smx[:, 10:11], 1.0, None, ALU.add, None, ('edm' + p,), ('edm' + p,)))
            st_.append(lambda: recip(smx[:, 11:12], smx[:, 10:11], ('edm' + p,), ('p1' + p,)))
            st_.append(lambda: tt('dve', C12[:, tile_i, 0:1], smx[:, 11:12], smx[:, 6:7], ALU.mult, ('p1' + p, 'pg' + p), ('c1' + p,)))
            st_.append(lambda: tt('dve', C12[:, tile_i, 1:2], smx[:, 6:7], C12[:, tile_i, 0:1], ALU.subtract, ('pg' + p, 'c1' + p), ('c2' + p,)))
            return st_

        NSL = 4
        for t0_ in range(0, NT, NSL):
            chains = [route_chain(t0_ + sl, sl) for sl in range(min(NSL, NT - t0_))]
            for k in range(max(len(c) for c in chains)):
                for c in chains:
                    if k < len(c):
                        c[k]()
        cx.barrier()
        for tile_i in range(NT):
            k1 = M12[:, tile_i, 0, :]
            k2 = M12[:, tile_i, 1, :]
            tt('dve', R3[:], k1, k2, ALU.add, (), ('R3',))
            mm(p_l[:, 64:96], ustrict_f, R3[:], True, False, ('R3', 'cst'), ('p_l2',))
            mm(p_l[:, 64:96], ones_f, Macc[:], False, True, ('Macc', 'cst'), ('p_l2',))
            cp('act', cum[:], p_l[:, 64:96], ('p_l2',), ('cum',))
            tt('pool', Macc[:], Macc[:], R3[:], ALU.add, ('Macc', 'R3'), ('Macc',))
            rkk = f'RK_{tile_i}'
            tt('dve', R1[:], k1, cum[:], ALU.mult, ('cum',), ('R1',))
            red(RK[:, tile_i, 0:1], R1[:], ALU.add, ('R1',), (rkk + 'a',))
            tt('dve', R2[:], k2, cum[:], ALU.mult, ('cum',), ('R2',))
            red(RK[:, tile_i, 1:2], R2[:], ALU.add, ('R2',), (rkk + 'b',))
        cx.barrier()

    with ExitStack() as ps5:
        nf = sbuf(ps5, "nf", [128, 32], F32)
        ni = sbuf(ps5, "ni", [128, 32], I32)
        npad = sbuf(ps5, "npad", [128, 32], F32)
        endc = sbuf(ps5, "endc", [128, 32], F32)
        base = sbuf(ps5, "base", [128, 32], F32)
        onesr = sbuf(ps5, "onesr", [128, 32], F32)
        cmpb = sbuf(ps5, "cmpb", [128, NTILES, 32], F32)
        te = sbuf(ps5, "te", [128, NTILES], F32)
        wif = sbuf(ps5, "wif", [128, NTILES, 2], F32)
        Dtmp = sbuf(ps5, "Dtmp", [128, 32], F32)
        Df = sbuf(ps5, "Df", [128, NT, 2], F32)
        h2l = [sbuf(ps5, f"h2l{i}", [128, 1024], BF16) for i in range(2)]
        p_t = psum(ps5, "p_t", [128, 512], F32)
        mm(p_t[:, 0:32], ones_f, Macc[:], True, True, ('Macc', 'cst'), ('p_t',))
        ts('dve', nf[:], p_t[:, 0:32], 511.0, 1.0 / 512, ALU.add, ALU.mult, ('p_t',), ('nf',))
        ts('dve', nf[:], nf[:], -0.499, None, ALU.add, None, ('nf',), ('nf',))
        cp('dve', ni[:], nf[:], ('nf',), ('ni',))
        cp('dve', npad[:], ni[:], ('ni',), ('npad',))
        ts('dve', npad[:], npad[:], 512.0, None, ALU.mult, None, ('npad',), ('npad',))
        cx.op('pool', lambda: nc.gpsimd.memset(onesr[:], 1.0), (), ('onesr',))
        cx.op('dve', lambda: nc.vector.tensor_tensor_scan(out=endc[:], data0=onesr[:], data1=npad[:], initial=0.0,
                                                         op0=ALU.mult, op1=ALU.add), ('onesr', 'npad'), ('endc',))
        tt('dve', base[:], endc[:], npad[:], ALU.subtract, ('endc', 'npad'), ('base',))
        tt('dve', cmpb[:], endc[:].rearrange("p (o e) -> p o e", o=1).to_broadcast([128, NTILES, 32]),
           misc[:, 64:64 + NTILES].rearrange("p (t o) -> p t o", o=1).to_broadcast([128, NTILES, 32]), ALU.is_le, ('endc', 'cst'), ('cmpb',))
        red(te[:].rearrange("p (t o) -> p t o", o=1), cmpb[:], ALU.add, ('cmpb',), ('te',))
        ts('dve', te[:], te[:], 31.0, None, ALU.min, None, ('te',), ('te',))
        for half in range(2):
            ts('dve', wif[:, :, half], te[:], 256.0, float(half), ALU.mult, ALU.add, ('te',), ('wif',))
        sm5 = sbuf(ps5, "sm5", [128, 1], F32)
        ts('dve', sm5[:], misc[:, 32:33], 2.0, None, ALU.mult, None, ('cst',), ('sm5',))
        ts('dve', wif[:], wif[:], sm5[:, 0:1], None, ALU.add, None, ('wif', 'sm5'), ('wif',))
        cp('dve', WI[:], wif[:], ('wif',), ('WI',))
        for ti in range(NT):
            for k in range(2):
                tt('dve', Dtmp[:], M12[:, ti, k, :], base[:], ALU.mult, ('base',), ('Dtmp',))
                red(Df[:, ti, k:k + 1], Dtmp[:], ALU.add, ('Dtmp',), ('Df',))
        tt('dve', Df[:], Df[:], RK[:], ALU.add, ('Df',), ('Df',))
        cp('dve', DI[:], Df[:], ('Df',), ('DI',))
        for ti in range(NT):
            b2 = ti % 2
            ld('sp', h2l[b2][:], H2[ti * 128:(ti + 1) * 128, :], (), (f'h2l{b2}',))
            for k in range(2):
                cx.dma('pool', lambda: nc.gpsimd.indirect_dma_start(
                    out=XS[:, :], out_offset=bass.IndirectOffsetOnAxis(ap=DI[:, ti, k:k + 1], axis=0),
                    in_=h2l[b2][:, :], in_offset=None), (f'h2l{b2}', 'DI'), (), semkey=f'sc{b2}')
        cx.barrier()

    with ExitStack() as ps6:
        Wg = [sbuf(ps6, f"Wg{i}", [128, 8, 512], BF16) for i in range(2)]
        Wu = [sbuf(ps6, f"Wu{i}", [128, 8, 512], BF16) for i in range(2)]
        Wd = [sbuf(ps6, f"Wd{i}", [128, 4, 1024], BF16) for i in range(2)]
        Xr = [sbuf(ps6, f"Xr{i}", [128, 4, 1024], BF16) for i in range(2)]
        XT = sbuf(ps6, "XTm", [128, 8, 512], BF16)
        sg = [sbuf(ps6, f"sg{i}", [128, 512], F32) for i in range(2)]
        hid = sbuf(ps6, "hid", [128, 4, 512], BF16)
        Yst = [sbuf(ps6, f"Yst{i}", [128, 1024], BF16) for i in range(2)]
        pTm = [psum(ps6, f"pTm{i}", [128, 1024], BF16) for i in range(2)]
        pg_ = [psum(ps6, f"pgm{i}", [128, 512], F32) for i in range(2)]
        pu_ = [psum(ps6, f"pum{i}", [128, 512], F32) for i in range(2)]
        py_ = [psum(ps6, f"pym{i}", [128, 512], F32) for i in range(2)]
        ti2 = 0
        gi = 0
        yi = 0
        ysi = 0
        for t in range(NTILES):
            b2 = t % 2
            for half in range(2):
                for (Wt, src, nm, ncc) in ((Wg[b2], wg_d, 'Wg', 4), (Wu[b2], wu_d, 'Wu', 4), (Wd[b2], wd_d, 'Wd', 2)):
                    dst = Wt[:, half * ncc:(half + 1) * ncc, :].rearrange("p c n -> p (c n)")
                    cx.dma('pool', lambda dst=dst, src=src: nc.gpsimd.indirect_dma_start(
                        out=dst, out_offset=None, in_=src[:, :],
                        in_offset=bass.IndirectOffsetOnAxis(ap=WI[:, t, half:half + 1], axis=0)), ('WI',), (f'{nm}{b2}',), semkey=f'{nm}{b2}')
            ld('sp', Xr[b2][:], XS[t * 512:(t + 1) * 512, :].rearrange("(s p) d -> p s d", p=128), (), (f'Xr{b2}',))
            for s in range(4):
                T = pTm[ti2]; tk = f'pTm{ti2}'; ti2 = (ti2 + 1) % 2
                for c in range(8):
                    tr(T[:, c * 128:(c + 1) * 128], Xr[b2][:, s, c * 128:(c + 1) * 128], ident, (f'Xr{b2}', 'cm'), (tk,))
                cp('dve' if s % 2 == 0 else 'act', XT[:, :, s * 128:(s + 1) * 128], T[:].rearrange("p (c k) -> p c k", k=128), (tk,), (f'XT{s}',))
            xr = tuple(f'XT{s}' for s in range(4))
            for hc in range(4):
                Gp = pg_[gi]; gk = f'pgm{gi}'
                Up = pu_[gi]; uk = f'pum{gi}'
                sgt = sg[gi]; sk = f'sg{gi}'
                gi = (gi + 1) % 2
                for c in range(8):
                    mm(Gp[:], Wg[b2][:, c, hc * 128:(hc + 1) * 128], XT[:, c, :], c == 0, c == 7, xr + (f'Wg{b2}',), (gk,))
                for c in range(8):
                    mm(Up[:], Wu[b2][:, c, hc * 128:(hc + 1) * 128], XT[:, c, :], c == 0, c == 7, xr + (f'Wu{b2}',), (uk,))
                act(sgt[:], Gp[:], AF.Silu, (gk,), (sk,))
                tt('dve', hid[:, hc, :], Up[:], sgt[:], ALU.mult, (uk, sk), (f'hid{hc}',))
            hr4 = tuple(f'hid{c}' for c in range(4))
            for s in range(4):
                ys = Yst[ysi]; ysk = f'Yst{ysi}'; ysi = (ysi + 1) % 2
                for half in range(2):
                    Y = py_[yi]; yk = f'pym{yi}'; yi = (yi + 1) % 2
                    for c in range(4):
                        mm(Y[:], hid[:, c, s * 128:(s + 1) * 128], Wd[b2][:, c, half * 512:(half + 1) * 512], c == 0, c == 3, hr4 + (f'Wd{b2}',), (yk,))
                    cp('act' if half == 0 else 'dve', ys[:, half * 512:(half + 1) * 512], Y[:], (yk,), (ysk,))
                ld('act', YS[t * 512 + s * 128:t * 512 + (s + 1) * 128, :], ys[:], (ysk,), ())
        cx.barrier()

    with ExitStack() as ps7:
        x2l = [sbuf(ps7, f"x2l{i}", [128, 1024], F32) for i in range(2)]
        y1 = [sbuf(ps7, f"y1{i}", [128, 1024], BF16) for i in range(2)]
        y2 = [sbuf(ps7, f"y2{i}", [128, 1024], BF16) for i in range(2)]
        acc = [sbuf(ps7, f"acc{i}", [128, 1024], F32) for i in range(2)]
        for ti in range(NT):
            b2 = ti % 2
            ld('sp', x2l[b2][:], X2[ti * 128:(ti + 1) * 128, :], (), (f'x2l{b2}',))
            for k, yt, nm in ((0, y1[b2], 'y1'), (1, y2[b2], 'y2')):
                cx.dma('pool', lambda yt=yt, k=k: nc.gpsimd.indirect_dma_start(
                    out=yt[:, :], out_offset=None, in_=YS[:, :],
                    in_offset=bass.IndirectOffsetOnAxis(ap=DI[:, ti, k:k + 1], axis=0)), ('DI',), (f'{nm}{b2}',))
            stt(acc[b2][:], y1[b2][:], C12[:, ti, 0:1], x2l[b2][:], ALU.mult, ALU.add, (f'y1{b2}', f'x2l{b2}'), (f'acc{b2}',))
            stt(acc[b2][:], y2[b2][:], C12[:, ti, 1:2], acc[b2][:], ALU.mult, ALU.add, (f'y2{b2}', f'acc{b2}'), (f'acc{b2}',))
            ld('sp', out_d[ti * 128:(ti + 1) * 128, :], acc[b2][:], (f'acc{b2}',), ())
        cx.barrier()
    es.close()
    return nc


def _consts(S):
    NTILES = (2 * S) // TROWS + 32
    ident = np.eye(128, dtype=np.float32)
    ones = np.ones((128, 128), np.float32)
    blk = np.zeros((128, 128), np.float32)
    blk[:64, :64] = 1
    blk[64:, 64:] = 1
    swap = np.zeros((128, 128), np.float32)
    for h in range(2):
        for i in range(32):
            swap[h * 64 + 32 + i, h * 64 + i] = 1
            swap[h * 64 + i, h * 64 + 32 + i] = 1
    ust = np.triu(np.ones((128, 128), np.float32), 1)
    cm = np.stack([ident, ones, blk, swap, ust]).astype(np.float32)
    s_idx = np.arange(128)[:, None]
    c_idx = np.arange(128)[None, :]
    same = (s_idx // 32) == (c_idx // 32)
    mf = (same & (s_idx <= c_idx)).astype(np.float32)
    mb = (same & (s_idx >= c_idx)).astype(np.float32)
    masks = np.stack([mf, mb])
    seg = np.ones((128, 512), np.float32)
    seg[:, ::32] = 0
    misc = np.zeros((128, 192), np.float32)
    misc[:, 0:32] = np.arange(32)[None, :]
    misc[:, 32] = np.arange(128)
    misc[:, 64:64 + NTILES] = (np.arange(NTILES) * TROWS)[None, :]
    rows = S // 64
    row_ids = np.repeat(np.arange(rows), 64).astype(np.float32)
    col_ids = np.tile(np.arange(64), rows).astype(np.float32)
    inv_freq = (10000.0 ** (-np.arange(0, 32, 2, dtype=np.float32) / 32)).astype(np.float32)
    ang = np.concatenate([row_ids[:, None] * inv_freq, col_ids[:, None] * inv_freq], axis=-1).astype(np.float32)
    cos = np.cos(ang).T.astype(np.float32)
    sin = np.sin(ang).T.astype(np.float32)
    cosT = np.tile(cos, (4, 1))
    sinT = np.tile(np.concatenate([-sin, sin], axis=0), (2, 1))
    return dict(cmats=cm, masks=masks, segmask=seg, misc=misc, cosT=np.ascontiguousarray(cosT), sinT=np.ascontiguousarray(sinT))


def _fm(w, nchunk):
    return np.ascontiguousarray(w.reshape(nchunk, 128, -1).transpose(1, 0, 2))


def _prep_shared(inp):
    w_in = inp["w_in"][0]
    offs = np.cumsum([0, 512, 128, 128, 512, 512, 512, 512, 512, 1024, 1024])
    aq, ak, av, hq, hff, hfb, hi, hg, ga, gb = [w_in[:, offs[i]:offs[i + 1]] for i in range(10)]
    deint = np.concatenate([np.arange(0, 64, 2), np.arange(1, 64, 2)])
    qperm = np.concatenate([h * 64 + deint for h in range(8)])
    aqp = aq[:, qperm]
    k0 = ak[:, 0:64][:, deint]
    k1 = ak[:, 64:128][:, deint]
    w1 = np.concatenate([aqp, k0, k0, k1, k1, hq, hff, hfb, av, hi], axis=1)
    w3 = np.concatenate([hg, ga, gb], axis=1)
    vec = np.zeros((128, 64), np.float32)
    vec[:, 0:8] = inp["g_mix"][0].reshape(8, 128).T
    vec[:, 8:16] = inp["g_ffn"][0].reshape(8, 128).T
    vec[:, 16] = np.tile(inp["q_norm"][0][deint], 2)
    vec[:, 17] = np.tile(inp["k_norm"][0][deint], 2)
    vec[:, 18] = inp["hgrn_norm"][0]
    lbf = np.stack([inp["lb_fwd"].reshape(2, 4, 128), inp["lb_bwd"].reshape(2, 4, 128)])
    lbf = np.ascontiguousarray(lbf.transpose(3, 0, 1, 2)).astype(np.float32)
    wr = np.concatenate([inp["w_router_group"][0], inp["w_router_expert"][0]], axis=1)
    rb = np.concatenate([inp["b_router_group"][0], inp["b_router_expert"][0]])[None, :].repeat(128, 0)

    def exp_rows(w, nchunk):
        n = w.shape[-1]
        a = w.reshape(32, 2, nchunk // 2, 128, n).transpose(0, 3, 1, 2, 4)
        return np.ascontiguousarray(a.reshape(32 * 128 * 2, (nchunk // 2) * n))

    return dict(
        w1=_fm(w1, 8), w3=_fm(w3, 8), wa=_fm(inp["w_attn_branch"][0], 4), wb=_fm(inp["w_hgrn_branch"][0], 4),
        wo=_fm(inp["w_out"][0], 8), wr=_fm(wr, 8).astype(np.float32),
        wg=exp_rows(inp["w_exp_gate"][0], 8), wu=exp_rows(inp["w_exp_up"][0], 8), wd=exp_rows(inp["w_exp_down"][0], 4),
        vecs=vec, lbf=lbf, gffn_bc=np.ascontiguousarray(inp["g_ffn"][0][None, :].repeat(128, 0)).astype(np.float32),
        rbias=np.ascontiguousarray(rb).astype(np.float32),
    )


def run(inp, cores=None, debug=False):
    x = np.asarray(inp["x"], np.float32)
    B, S, _ = x.shape
    inp = {k: np.asarray(v, np.float32) for k, v in inp.items()}
    shared = _prep_shared(inp)
    shared.update(_consts(S))
    nc = build(S, debug=debug)
    in_maps = []
    for b in range(B):
        m = dict(shared)
        m["x"] = np.ascontiguousarray(x[b])
        m["xT"] = np.ascontiguousarray(x[b].T)
        in_maps.append(m)
    res = run_bass_kernel_spmd(nc, in_maps, core_ids=list(range(B)) if cores is None else cores)
    if debug:
        return res.results
    return np.stack([r["out"] for r in res.results]).astype(np.float32)


def kernel(**inputs):
    return run(inputs)
```

```python
import numpy as np
import ml_dtypes
from contextlib import ExitStack
import concourse.bass as bass
import concourse.mybir as mybir
from concourse.bass_utils import run_bass_kernel_spmd

F32 = mybir.dt.float32
BF16 = mybir.dt.bfloat16
I32 = mybir.dt.int32
ALU = mybir.AluOpType
AF = mybir.ActivationFunctionType
AX = mybir.AxisListType

D = 1024
EPS = 1e-6
HGRN_SCALE = 128 ** -0.5
EPOCH = 30000
BIG = 1.0e30
TROWS = 512
OPT = {'spb_act': True, 'hla': True}


class Ctx:
    def __init__(self, nc):
        self.nc = nc
        self.E = {'pe': nc.tensor, 'act': nc.scalar, 'dve': nc.vector, 'pool': nc.gpsimd, 'sp': nc.sync}
        self.cnt = {e: 0 for e in self.E}
        self.known = {e: {} for e in self.E}
        self.esem = {e: [] for e in self.E}
        self.st = {}
        self.dsem = {}
        self.dcount = {}
        self.free_dsems = []
        self.nsem = 0
        self.all_dsems = []

    def newsem(self):
        self.nsem += 1
        return self.nc.alloc_semaphore(f"sm{self.nsem}")

    def _need(self, eng, tok):
        _, sem, val = tok
        k = self.known[eng]
        if k.get(id(sem), 0) >= val:
            return
        self.E[eng].wait_ge(sem, val)
        k[id(sem)] = val

    def _deps(self, eng, r, w, selfsync):
        deps = []
        for key in r:
            s = self.st.get(key)
            if s and s['w']:
                deps.append(s['w'])
        for key in w:
            s = self.st.get(key)
            if s:
                if s['w']:
                    deps.append(s['w'])
                deps.extend(s['r'].values())
        for tok in deps:
            if tok[0] == eng and not selfsync:
                continue
            self._need(eng, tok)

    def _mark(self, tok, r, w):
        for key in r:
            self.st.setdefault(key, {'w': None, 'r': {}})['r'][id(tok[1])] = tok
        for key in w:
            self.st[key] = {'w': tok, 'r': {}}

    def op(self, eng, fn, r=(), w=(), selfsync=True):
        self._deps(eng, r, w, selfsync)
        ins = fn()
        self.cnt[eng] += 1
        n = self.cnt[eng]
        idx = (n - 1) // EPOCH
        while len(self.esem[eng]) <= idx:
            self.esem[eng].append(self.newsem())
        sem = self.esem[eng][idx]
        val = (n - 1) % EPOCH + 1
        ins.then_inc(sem, 1)
        tok = (eng, sem, val)
        self._mark(tok, r, w)
        return tok

    def _dsem_for(self, key):
        if key not in self.dsem:
            if self.free_dsems:
                sem = self.free_dsems.pop()
            else:
                sem = self.newsem()
                self.all_dsems.append(sem)
                self.dcount[id(sem)] = 0
            self.dsem[key] = sem
        return self.dsem[key]

    def dma(self, q, fn, r=(), w=(), semkey=None):
        self._deps(q, r, w, True)
        key = semkey or (w[0] if w else r[0])
        sem = self._dsem_for(key)
        ins = fn()
        ins.then_inc(sem, 16)
        self.dcount[id(sem)] += 1
        tok = ('dma', sem, 16 * self.dcount[id(sem)])
        self._mark(tok, r, w)
        return tok

    def finalize_group(self, semkey, keys):
        sem = self.dsem[semkey]
        tok = ('dma', sem, 16 * self.dcount[id(sem)])
        for key in keys:
            self.st[key] = {'w': tok, 'r': {}}

    def barrier(self):
        toks = []
        for e in self.E:
            n = self.cnt[e]
            if n > 0:
                idx = (n - 1) // EPOCH
                toks.append((e, self.esem[e][idx], (n - 1) % EPOCH + 1))
        for sem in self.all_dsems:
            c = self.dcount[id(sem)]
            if c > 0:
                toks.append(('dma', sem, 16 * c))
        for e in self.E:
            for tok in toks:
                if tok[0] == e:
                    continue
                self._need(e, tok)
        self.st = {}
        self.free_dsems = list(self.all_dsems)
        self.dsem = {}


def build(S, debug=False):
    NB = S // 512
    NT = S // 128
    NCH = S // 32
    NTILES = (2 * S) // TROWS + 32
    NROWS = NTILES * TROWS
    nc = bass.Bass("TRN2", target_bir_lowering=False)
    cx = Ctx(nc)
    es = ExitStack()

    def din(name, shape, dt=F32):
        return nc.dram_tensor(name, list(shape), dt, kind="ExternalInput").ap()

    def dscr(name, shape, dt):
        return nc.dram_tensor(name, list(shape), dt, kind="ExternalOutput" if debug else "Internal").ap()

    xT_d = din("xT", [D, S])
    x_d = din("x", [S, D])
    w1_d = din("w1", [128, 8, 2944])
    w3_d = din("w3", [128, 8, 2560])
    wa_d = din("wa", [128, 4, 1024])
    wb_d = din("wb", [128, 4, 1024])
    wo_d = din("wo", [128, 8, 1024])
    wr_d = din("wr", [128, 8, 36])
    wg_d = din("wg", [32 * 128 * 2, 2048])
    wu_d = din("wu", [32 * 128 * 2, 2048])
    wd_d = din("wd", [32 * 128 * 2, 2048])
    cos_d = din("cosT", [128, S])
    sin_d = din("sinT", [128, S])
    vec_d = din("vecs", [128, 64])
    lbf_d = din("lbf", [128, 2, 2, 4])
    gffn_bc_d = din("gffn_bc", [128, 1024])
    rbias_d = din("rbias", [128, 36])
    cm_d = din("cmats", [5, 128, 128])
    msk_d = din("masks", [2, 128, 128])
    seg_d = din("segmask", [128, 512])
    misc_d = din("misc", [128, 64 + 128])

    out_d = nc.dram_tensor("out", [S, D], F32, kind="ExternalOutput").ap()

    QT = dscr("QT", [4, 128, S], BF16)
    KT = dscr("KT", [2, 128, S], BF16)
    Vd = dscr("Vd", [S, 128], BF16)
    Vi = dscr("Vi", [S, 512], BF16)
    QD = dscr("QD", [2, 4, 128, S], BF16)
    KD = dscr("KD", [2, 4, 128, S], BF16)
    KS = dscr("KS", [2, 4, S, 128], BF16)
    COLS = dscr("COLS", [2, 4, 128, 3, NCH], F32)
    OD = dscr("OD", [2, 4, 128, S], BF16)
    AO = dscr("AO", [4, 128, S], BF16)
    X2 = dscr("X2", [S, D], F32)
    H2 = dscr("H2", [S, D], BF16)
    XS = dscr("XS", [NROWS, D], BF16)
    YS = dscr("YS", [NROWS, D], BF16)

    uniq = [0]

    def sbuf(stack, name, shape, dt):
        uniq[0] += 1
        return stack.enter_context(nc.sbuf_tensor(f"{name}_u{uniq[0]}", list(shape), dt))

    def psum(stack, name, shape, dt):
        uniq[0] += 1
        return stack.enter_context(nc.psum_tensor(f"{name}_u{uniq[0]}", list(shape), dt))

    def mm(out, lhsT, rhs, start, stop, r, w):
        return cx.op('pe', lambda: nc.tensor.matmul(out, lhsT=lhsT, rhs=rhs, start=start, stop=stop), r, w, selfsync=False)

    def tr(out, in_, ident, r, w):
        return cx.op('pe', lambda: nc.tensor.transpose(out, in_, ident), r, w, selfsync=False)

    def act(out, in_, func, r, w, scale=1.0, bias=0.0, accum_out=None):
        if accum_out is None:
            return cx.op('act', lambda: nc.scalar.activation(out=out, in_=in_, func=func, bias=bias, scale=scale), r, w)
        return cx.op('act', lambda: nc.scalar.activation(out=out, in_=in_, func=func, bias=bias, scale=scale, accum_out=accum_out), r, w)

    def tt(eng, out, in0, in1, op, r, w):
        e = nc.vector if eng == 'dve' else nc.gpsimd
        return cx.op(eng, lambda: e.tensor_tensor(out=out, in0=in0, in1=in1, op=op), r, w)

    def ts(eng, out, in0, s1, s2, op0, op1, r, w):
        e = nc.vector if eng == 'dve' else nc.gpsimd
        if s2 is None:
            return cx.op(eng, lambda: e.tensor_scalar(out=out, in0=in0, scalar1=s1, scalar2=None, op0=op0), r, w)
        return cx.op(eng, lambda: e.tensor_scalar(out=out, in0=in0, scalar1=s1, scalar2=s2, op0=op0, op1=op1), r, w)

    def stt(out, in0, scalar, in1, op0, op1, r, w):
        return cx.op('dve', lambda: nc.vector.scalar_tensor_tensor(out=out, in0=in0, scalar=scalar, in1=in1, op0=op0, op1=op1), r, w)

    def recip(out, in_, r, w):
        return cx.op('dve', lambda: nc.vector.reciprocal(out=out, in_=in_), r, w)

    def cp(eng, out, in_, r, w):
        if eng == 'act':
            return cx.op('act', lambda: nc.scalar.copy(out=out, in_=in_), r, w)
        e = nc.vector if eng == 'dve' else nc.gpsimd
        return cx.op(eng, lambda: e.tensor_copy(out=out, in_=in_), r, w)

    def red(out, in_, op, r, w):
        return cx.op('dve', lambda: nc.vector.tensor_reduce(out=out, in_=in_, axis=AX.X, op=op), r, w)

    def ld(q, out, in_, r, w, semkey=None):
        e = cx.E[q]
        return cx.dma(q, lambda: e.dma_start(out=out, in_=in_), r, w, semkey)

    def load_cast(dst, src, n, key):
        o = 0
        while o < n:
            m = min(2048, n - o)
            ld('pool', dst[:, o:o + m], src[:, o:o + m], (), (key,), semkey='const')
            o += m

    cm = sbuf(es, "cm", [128, 5, 128], BF16)
    cmf = sbuf(es, "cmf", [128, 5, 128], F32)
    vecs = sbuf(es, "vecs_sb", [128, 64], F32)
    misc = sbuf(es, "misc_sb", [128, 192], F32)
    M12 = sbuf(es, "M12", [128, NT, 2, 32], F32)
    RK = sbuf(es, "RK", [128, NT, 2], F32)
    C12 = sbuf(es, "C12", [128, NT, 2], F32)
    Macc = sbuf(es, "Macc", [128, 32], F32)
    WI = sbuf(es, "WI", [128, NTILES, 2], I32)
    DI = sbuf(es, "DI", [128, NT, 2], I32)
    lbt = sbuf(es, "lbt", [128, 2, 2, 4], F32)
    lbc = sbuf(es, "lbc", [128, 8], F32)
    oml = sbuf(es, "oml", [128, 8], F32)

    for i in range(5):
        ld('sp', cmf[:, i, :], cm_d[i], (), ('cst',), semkey='const')
    ld('sp', vecs[:], vec_d, (), ('cst',), semkey='const')
    ld('sp', misc[:], misc_d, (), ('cst',), semkey='const')
    ld('sp', lbt[:], lbf_d, (), ('cst',), semkey='const')
    cx.finalize_group('const', ['cst'])
    cp('dve', cm[:], cmf[:], ('cst',), ('cm',))
    cx.op('pool', lambda: nc.gpsimd.memset(Macc[:], 0.0), (), ('Macc',))
    tt('dve', lbc[:].rearrange("p (d h) -> p d h", d=2), lbt[:, :, 1, :], lbt[:, :, 0, :], ALU.subtract, ('cst',), ('lbc',))
    act(lbc[:], lbc[:], AF.Exp, ('lbc',), ('lbc',))
    ts('dve', lbc[:], lbc[:], 1.0, None, ALU.add, None, ('lbc',), ('lbc',))
    recip(lbc[:], lbc[:], ('lbc',), ('lbc',))
    ts('dve', oml[:], lbc[:], -1.0, 1.0, ALU.mult, ALU.add, ('lbc',), ('oml',))

    ident = cm[:, 0, :]
    ones_bf = cm[:, 1, :]
    blk64 = cm[:, 2, :]
    swapm = cm[:, 3, :]
    ones_f = cmf[:, 1, :]
    ustrict_f = cmf[:, 4, :]

    def emit_norm(blk, xTs, sq, hT, rt, rstd, ps_ss, gcol0):
        t0 = blk * 512
        for c in range(8):
            ld('sp' if c % 2 == 0 else 'act', xTs[:, c, :], xT_d[c * 128:(c + 1) * 128, t0:t0 + 512], (), (f'xT{c}',))
        for c in range(8):
            act(sq[:, c % 2, :], xTs[:, c, :], AF.Square, (f'xT{c}',), (f'sq{c % 2}',))
            mm(ps_ss[:], ones_bf, sq[:, c % 2, :], c == 0, c == 7, (f'sq{c % 2}', 'cm'), ('ps_ss',))
        act(rt[:], ps_ss[:], AF.Sqrt, ('ps_ss',), ('rt',), scale=1.0 / D, bias=EPS)
        recip(rstd[:], rt[:], ('rt',), ('rstd',))
        for c in range(8):
            stt(hT[:, c, :], xTs[:, c, :], vecs[:, gcol0 + c:gcol0 + c + 1], rstd[:], ALU.mult, ALU.mult,
                (f'xT{c}', 'rstd', 'cst'), (f'hT{c}',))

    with ExitStack() as ps1:
        W1 = sbuf(ps1, "W1", [128, 8, 2944], BF16)
        for c in range(8):
            load_cast(W1[:, c, :], w1_d[:, c, :], 2944, 'W1')
        cx.finalize_group('const', ['W1'])
        xTs = sbuf(ps1, "xTs", [128, 8, 512], F32)
        sq = sbuf(ps1, "sq", [128, 2, 512], BF16)
        hT = sbuf(ps1, "hT", [128, 8, 512], BF16)
        rt = sbuf(ps1, "rt", [128, 512], F32)
        rstd = sbuf(ps1, "rstd", [128, 512], F32)
        cos_sb = sbuf(ps1, "cos_sb", [128, 512], F32)
        sin_sb = sbuf(ps1, "sin_sb", [128, 512], F32)
        seg = sbuf(ps1, "seg", [128, 512], F32)
        msk = None
        QTst = sbuf(ps1, "QTst", [128, 6, 512], BF16)
        QDst = sbuf(ps1, "QDst", [128, 8, 512], BF16)
        KDst = sbuf(ps1, "KDst", [128, 8, 512], BF16)
        KSst = sbuf(ps1, "KSst", [128, 8, 4, 128], BF16)
        VIst = sbuf(ps1, "VIst", [128, 4, 640], BF16)
        CLst = sbuf(ps1, "CLst", [128, 8, 3, 16], F32)
        sqh = sbuf(ps1, "sqh", [128, 4, 512], BF16)
        sqq = sbuf(ps1, "sqq", [128, 2, 512], BF16)
        tq = sbuf(ps1, "tq", [128, 512], F32)
        rq = sbuf(ps1, "rq", [128, 512], F32)
        qn = sbuf(ps1, "qn", [128, 2, 512], BF16)
        t1 = sbuf(ps1, "t1", [128, 512], F32)
        t2 = sbuf(ps1, "t2", [128, 512], F32)
        TA = [sbuf(ps1, f"TA{i}", [128, 512], F32) for i in range(2)]
        TB = [sbuf(ps1, f"TB{i}", [128, 512], F32) for i in range(2)]
        TC = [sbuf(ps1, f"TC{i}", [128, 512], F32) for i in range(2)]
        TD = [sbuf(ps1, f"TD{i}", [128, 512], F32) for i in range(2)]
        TE = [sbuf(ps1, f"TE{i}", [128, 512], F32) for i in range(2)]
        cl = [sbuf(ps1, f"cl{i}", [128, 16], F32) for i in range(2)]
        ksT = [sbuf(ps1, f"ksT{i}", [128, 512], BF16) for i in range(2)]
        ps_ss = psum(ps1, "ps_ss", [128, 512], F32)
        pp = [psum(ps1, f"pp{i}", [128, 512], F32) for i in range(3)]
        ps_sum = psum(ps1, "ps_sum", [128, 512], F32)
        ps_swap = psum(ps1, "ps_swap", [128, 512], F32)
        psT = [psum(ps1, f"psT{i}", [128, 1024], BF16) for i in range(2)]
        ld('sp', seg[:], seg_d, (), ('seg',))
        pi = 0
        it = 0
        dq = []

        def defer(n, fn):
            dq.append([n, fn])

        def tick():
            for e in dq:
                e[0] -= 1
            ready = [e for e in dq if e[0] <= 0]
            for e in ready:
                dq.remove(e)
            for e in ready:
                e[1]()

        def f_chain(j, P, pk, s, t0, blk):
            steps = []
            dh = j - 10
            d = dh // 4
            hh = dh % 4
            A, B, C, Dd, Eb = TA[s], TB[s], TC[s], TD[s], TE[s]
            kA, kB, kC, kD, kE = f'TA{s}', f'TB{s}', f'TC{s}', f'TD{s}', f'TE{s}'
            C3 = C[:].rearrange("p (n c) -> p n c", c=32)
            D3 = Dd[:].rearrange("p (n c) -> p n c", c=32)
            ref = 16 if d == 0 else 15
            last = 31 if d == 0 else 0
            clk = f'cl{s}'
            ck = f'CLst{dh}'
            kst = ksT[s]
            steps.append(lambda: act(A[:], P[:], AF.Exp, (pk,), (kA,), scale=-1.0))
            steps.append(lambda: ts('pool', A[:], A[:], 1.0, None, ALU.add, None, (kA,), (kA,)))
            steps.append(lambda: recip(A[:], A[:], (kA,), (kA,)))
            steps.append(lambda: ts('dve', A[:], A[:], oml[:, dh:dh + 1], lbc[:, dh:dh + 1], ALU.mult, ALU.add, (kA, 'oml', 'lbc'), (kA,)))
            steps.append(lambda: act(B[:], A[:], AF.Ln, (kA,), (kB,)))
            steps.append(lambda: cx.op('dve', lambda: nc.vector.tensor_tensor_scan(out=C[:], data0=seg[:], data1=B[:], initial=0.0,
                                                                                  op0=ALU.mult, op1=ALU.add), (kB, 'seg'), (kC,)))
            if d == 1:
                steps.append(lambda: tt('dve', D3, C3[:, :, 31:32].to_broadcast([128, 16, 32]), C3, ALU.subtract, (kC,), (kD,)))
                steps.append(lambda: tt('dve', C[:], Dd[:], B[:], ALU.add, (kD, kB), (kC,)))
            steps.append(lambda: tt('dve', D3, C3, C3[:, :, ref:ref + 1].to_broadcast([128, 16, 32]), ALU.subtract, (kC,), (kD,)))
            steps.append(lambda: tt('dve', cl[s][:].rearrange("p (n o) -> p n o", o=1), C3[:, :, last:last + 1], C3[:, :, ref:ref + 1], ALU.subtract, (kC,), (clk,)))
            steps.append(lambda: act(B[:], Dd[:], AF.Exp, (kD,), (kB,)))
            steps.append(lambda: act(Eb[:], Dd[:], AF.Exp, (kD,), (kE,), scale=-1.0))

            def cols_():
                act(CLst[:, dh, 0, :], cl[s][:], AF.Exp, (clk,), (ck,))
                act(CLst[:, dh, 1, :].rearrange("p (n o) -> p n o", o=1), C3[:, :, ref:ref + 1], AF.Exp, (kC,), (ck,))
                act(CLst[:, dh, 2, :].rearrange("p (n o) -> p n o", o=1), C3[:, :, last:last + 1], AF.Exp, (kC,), (ck,))
                ld('sp', COLS[d, hh][:, :, blk * 16:(blk + 1) * 16], CLst[:, dh, :, :], (ck,), ())
            steps.append(cols_)
            steps.append(lambda: stt(QDst[:, dh, :], sqh[:, hh, :], HGRN_SCALE, B[:], ALU.mult, ALU.mult, (f'sqh{hh}', kB), (f'QDst{dh}',)))
            steps.append(lambda: ts('pool', A[:], A[:], -1.0, 1.0, ALU.mult, ALU.add, (kA,), (kA,)))
            steps.append(lambda: tt('dve', KDst[:, dh, :], A[:], Eb[:], ALU.mult, (kA, kE), (f'KDst{dh}',)))

            def stores_():
                ld('sp', QD[d, hh][:, t0:t0 + 512], QDst[:, dh, :], (f'QDst{dh}',), ())
                ld('act', KD[d, hh][:, t0:t0 + 512], KDst[:, dh, :], (f'KDst{dh}',), ())
            steps.append(stores_)
            steps.append(lambda: tt('dve', kst[:].rearrange("p (n c) -> p n c", c=32), KDst[:, dh, :].rearrange("p (n c) -> p n c", c=32),
                                    CLst[:, dh, 0, :].rearrange("p (n o) -> p n o", o=1).to_broadcast([128, 16, 32]), ALU.mult,
                                    (f'KDst{dh}', ck), (f'ksT{s}',)))

            def stage_t():
                pT = psT[s]
                for t4 in range(4):
                    tr(pT[:, t4 * 128:(t4 + 1) * 128], kst[:, t4 * 128:(t4 + 1) * 128], ident, (f'ksT{s}', 'cm'), (f'psT{s}',))
                cp('act', KSst[:, dh, :, :], pT[:, 0:512].rearrange("p (t k) -> p t k", k=128), (f'psT{s}',), (f'KSst{dh}',))
                ld('sp', KS[d, hh][t0:t0 + 512, :].rearrange("(t p) k -> p t k", p=128), KSst[:, dh, :, :], (f'KSst{dh}',), ())
            steps.append(lambda: defer(2, stage_t))
            return steps

        pend_chain = None
        for blk in range(NB):
            t0 = blk * 512
            emit_norm(blk, xTs, sq, hT, rt, rstd, ps_ss, 0)
            ld('sp', cos_sb[:], cos_d[:, t0:t0 + 512], (), ('cos',))
            ld('act', sin_sb[:], sin_d[:, t0:t0 + 512], (), ('sin',))
            hr = tuple(f'hT{c}' for c in range(8))
            for t4 in range(4):
                P = pp[pi]; pk = f'pp{pi}'; pi = (pi + 1) % 3
                for c in range(8):
                    mm(P[:], hT[:, c, t4 * 128:(t4 + 1) * 128], W1[:, c, 2304 + 128:2304 + 640], c == 0, c == 7, hr + ('W1',), (pk,))
                for c in range(8):
                    mm(ps_sum[:, 0:128], hT[:, c, t4 * 128:(t4 + 1) * 128], W1[:, c, 2304:2304 + 128], c == 0, c == 7, hr + ('W1',), ('ps_sum',))
                tick()
                cp('act', VIst[:, t4, 128:640], P[:], (pk,), (f'VIst{t4}',))
                cp('dve', VIst[:, t4, 0:128], ps_sum[:, 0:128], ('ps_sum',), (f'VIst{t4}',))
                ld('sp', Vi[t0 + t4 * 128:t0 + (t4 + 1) * 128, :], VIst[:, t4, 128:640], (f'VIst{t4}',), ())
                ld('sp', Vd[t0 + t4 * 128:t0 + (t4 + 1) * 128, :], VIst[:, t4, 0:128], (f'VIst{t4}',), ())
            for j in range(18):
                P = pp[pi]; pk = f'pp{pi}'; pi = (pi + 1) % 3
                for c in range(8):
                    mm(P[:], W1[:, c, j * 128:(j + 1) * 128], hT[:, c, :], c == 0, c == 7, hr + ('W1',), (pk,))
                tick()
                if j < 6:
                    q2 = j % 2
                    act(sqq[:, q2, :], P[:], AF.Square, (pk,), (f'sqq{q2}',))

                    def stage_a(j=j, P=P, pk=pk, q2=q2):
                        mm(ps_sum[:], blk64, sqq[:, q2, :], True, True, (f'sqq{q2}', 'cm'), ('ps_sum',))
                        act(tq[:], ps_sum[:], AF.Sqrt, ('ps_sum',), ('tq',), scale=1.0 / 64, bias=EPS)
                        recip(rq[:], tq[:], ('tq',), ('rq',))
                        gc = 16 if j < 4 else 17
                        stt(qn[:, q2, :], P[:], vecs[:, gc:gc + 1], rq[:], ALU.mult, ALU.mult, (pk, 'rq', 'cst'), (f'qn{q2}',))

                    def stage_b(j=j, q2=q2, t0=t0):
                        mm(ps_swap[:], swapm, qn[:, q2, :], True, True, (f'qn{q2}', 'cm'), ('ps_swap',))
                        tt('pool', t1[:], qn[:, q2, :], cos_sb[:], ALU.mult, (f'qn{q2}', 'cos'), ('t1',))
                        tt('dve', t2[:], ps_swap[:], sin_sb[:], ALU.mult, ('ps_swap', 'sin'), ('t2',))
                        tt('pool', QTst[:, j, :], t1[:], t2[:], ALU.add, ('t1', 't2'), (f'QTst{j}',))
                        dst = QT[j][:, t0:t0 + 512] if j < 4 else KT[j - 4][:, t0:t0 + 512]
                        ld('act', dst, QTst[:, j, :], (f'QTst{j}',), ())

                    defer(1, stage_a)
                    defer(2, stage_b)
                elif j < 10:
                    hh = j - 6
                    act(sqh[:, hh, :], P[:], AF.Silu, (pk,), (f'sqh{hh}',))
                else:
                    s_ = it % 2
                    it += 1
                    chain = f_chain(j, P, pk, s_, t0, blk)
                    if pend_chain is None:
                        pend_chain = chain
                    else:
                        a_, b_ = pend_chain, chain
                        pend_chain = None
                        for k in range(max(len(a_), len(b_))):
                            if k < len(a_):
                                a_[k]()
                            if k < len(b_):
                                b_[k]()
        while dq:
            tick()
        cx.barrier()

    with ExitStack() as ps2:
        KTs = sbuf(ps2, "KTs", [128, 2, 2, S], BF16)
        Vs = sbuf(ps2, "Vs", [128, NT, 128], BF16)
        Vp0 = sbuf(ps2, "Vp0", [128, NT, 2, 65], BF16)
        Vp1 = sbuf(ps2, "Vp1", [128, NT, 2, 128], BF16)
        Qs = [sbuf(ps2, f"Qs{i}", [128, 512], BF16) for i in range(2)]
        Pt = [sbuf(ps2, f"Pt{i}", [128, 512], BF16) for i in range(4)]
        rs = sbuf(ps2, "rs", [128, 512], F32)
        rb = sbuf(ps2, "rb", [128, 512], F32)
        AOst = [sbuf(ps2, f"AOst{i}", [128, 512], BF16) for i in range(2)]
        pss = [psum(ps2, f"pss{i}", [128, 512], F32) for i in range(3)]
        po = [psum(ps2, f"po{i}", [128, 512], F32) for i in range(2)]
        pb = psum(ps2, "pb", [128, 512], F32)
        cx.op('pool', lambda: nc.gpsimd.memset(KTs[64:128, :, 0, :], 0.0), (), ('KTz0',))
        cx.op('pool', lambda: nc.gpsimd.memset(KTs[0:64, :, 1, :], 0.0), (), ('KTz1',))
        for g in range(2):
            ld('sp', KTs[0:64, g, 0, :], KT[g][0:64, :], (), ('KTs',), semkey='const')
            ld('act', KTs[64:128, g, 1, :], KT[g][64:128, :], (), ('KTs',), semkey='const')
        ld('act', Vs[:], Vd.rearrange("(t p) f -> p t f", p=128), (), ('Vs',), semkey='const')
        cx.finalize_group('const', ['KTs', 'Vs'])
        cx.op('pool', lambda: nc.gpsimd.memset(Vp0[:], 1.0), (), ('Vp0',))
        cx.op('pool', lambda: nc.gpsimd.memset(Vp1[:], 0.0), (), ('Vp1',))
        cx.op('pool', lambda: nc.gpsimd.memset(Vp1[:, :, :, 0:1], 1.0), ('Vp1',), ('Vp1',))
        for g in range(2):
            cp('dve', Vp0[:, :, g, 0:64], Vs[:, :, g * 64:(g + 1) * 64], ('Vs', 'Vp0'), ('Vp0',))
            cp('dve', Vp1[:, :, g, 64:128], Vs[:, :, g * 64:(g + 1) * 64], ('Vs', 'Vp1'), ('Vp1',))
        heads = [(qb, c, hl) for qb in range(NB) for c in range(4) for hl in range(2)]
        NH = len(heads)
        NI = NH * NT
        LA = 2

        def qload(ci):
            qb, c = ci // 4, ci % 4
            ld('sp', Qs[ci % 2][:], QT[c][:, qb * 512:(qb + 1) * 512], (), (f'Qs{ci % 2}',))

        def emit_S(i):
            hi, kc = i // NT, i % NT
            qb, c, hl = heads[hi]
            ci = qb * 4 + c
            if kc == 0 and hl == 0 and ci + 1 < NB * 4:
                qload(ci + 1)
            hp = hl * 64
            g = c // 2
            mm(pss[i % 3][:], KTs[:, g, hl, kc * 128:(kc + 1) * 128], Qs[ci % 2][:, :], True, True,
               ('KTs', 'KTz0', 'KTz1', f'Qs{ci % 2}'), (f'pss{i % 3}',))

        def emit_exp(i):
            act(Pt[i % 4][:], pss[i % 3][:], AF.Exp, (f'pss{i % 3}',), (f'Pt{i % 4}',), scale=0.125)

        def emit_PV(i):
            hi, kc = i // NT, i % NT
            qb, c, hl = heads[hi]
            g = c // 2
            O = po[hi % 2]; ok = f'po{hi % 2}'
            if hl == 0:
                mm(O[0:65, :], Vp0[:, kc, g, :], Pt[i % 4][:], kc == 0, kc == NT - 1, ('Vp0', f'Pt{i % 4}'), (ok,))
            else:
                mm(O[:, :], Vp1[:, kc, g, :], Pt[i % 4][:], kc == 0, kc == NT - 1, ('Vp1', f'Pt{i % 4}'), (ok,))

        def epilogue(hi):
            qb, c, hl = heads[hi]
            O = po[hi % 2]; ok = f'po{hi % 2}'
            ao = AOst[c % 2]; aok = f'AOst{c % 2}'
            if hl == 0:
                recip(rs[64:65, :], O[64:65, :], (ok,), ('rs',))
                mm(pb[0:64, :], cmf[64:65, 1, 0:64], rs[64:65, :], True, True, ('rs', 'cst'), ('pb',))
                cp('act', rb[0:64, :], pb[0:64, :], ('pb',), ('rb',))
                tt('dve', ao[0:64, :], O[0:64, :], rb[0:64, :], ALU.mult, (ok, 'rb'), (aok,))
            else:
                recip(rs[0:1, :], O[0:1, :], (ok,), ('rs',))
                mm(pb[:, :], cmf[0:1, 1, :], rs[0:1, :], True, True, ('rs', 'cst'), ('pb',))
                cp('act', rb[64:128, :], pb[64:128, :], ('pb',), ('rb',))
                tt('dve', ao[64:128, :], O[64:128, :], rb[64:128, :], ALU.mult, (ok, 'rb'), (aok,))
                ld('act', AO[c][:, qb * 512:(qb + 1) * 512], ao[:], (aok,), ())

        qload(0)
        epi_at = min(6, NT - 1)
        for i in range(-LA, NI):
            if i + LA < NI:
                emit_S(i + LA)
            if i >= 0:
                emit_exp(i)
                emit_PV(i)
                hi, kc = i // NT, i % NT
                if kc == epi_at and hi > 0:
                    epilogue(hi - 1)
        epilogue(NH - 1)
        cx.barrier()

    with ExitStack() as ps3:
        msk = sbuf(ps3, "msk", [128, 2, 128], F32)
        ld('sp', msk[:, 0, :], msk_d[0], (), ('msk',), semkey='const')
        ld('sp', msk[:, 1, :], msk_d[1], (), ('msk',), semkey='const')
        cols = sbuf(ps3, "cols", [128, 8, 3, NCH], F32)
        for d in range(2):
            for hh in range(4):
                ld('act', cols[:, d * 4 + hh, :, :], COLS[d, hh], (), ('cols',), semkey='const')
        cx.finalize_group('const', ['msk', 'cols'])
        for d in range(2):
            with ExitStack() as psd:
                qd = [[sbuf(psd, f"qd{hh}_{i}", [128, 512], BF16) for i in range(2)] for hh in range(4)]
                kd = [[sbuf(psd, f"kd{hh}_{i}", [128, 512], BF16) for i in range(2)] for hh in range(4)]
                ks32 = [[sbuf(psd, f"ks128{hh}_{i}", [128, 4, 128], BF16) for i in range(2)] for hh in range(4)]
                v32 = [[sbuf(psd, f"v32z{hh}_{i}", [128, 16, 128], BF16) for i in range(2)] for hh in range(4)]
                for hh in range(4):
                    for i in range(2):
                        cx.op('pool', lambda: nc.gpsimd.memset(v32[hh][i][:], 0.0), (), (f'v32{hh}_{i}',))
                v128 = [[sbuf(psd, f"v128{hh}_{i}", [128, 4, 128], BF16) for i in range(2)] for hh in range(4)]
                state = [sbuf(psd, f"state{hh}", [128, 128], F32) for hh in range(4)]
                Spb = [sbuf(psd, f"Spb{hh}", [128, 128], BF16) for hh in range(4)]
                Am = [[sbuf(psd, f"Am{hh}_{i}", [128, 128], BF16) for i in range(2)] for hh in range(4)]
                ost = [[sbuf(psd, f"ost{hh}_{i}", [128, 512], BF16) for i in range(2)] for hh in range(4)]
                poh = [psum(psd, f"poh{hh}", [128, 512], F32) for hh in range(4)]
                pA = [psum(psd, f"pA{i}", [128, 512], F32) for i in range(2)]
                pP = [psum(psd, f"pP{i}", [128, 512], F32) for i in range(2)]
                for hh in range(4):
                    cx.op('pool', lambda: nc.gpsimd.memset(state[hh][:], 0.0), (), (f'state{hh}',))
                    cx.op('pool', lambda: nc.gpsimd.memset(Spb[hh][:], 0.0), (), (f'Spb{hh}',))
                blocks = list(range(NB)) if d == 0 else list(range(NB - 1, -1, -1))
                ai = 0
                ppi = 0
                for bi, blk in enumerate(blocks):
                    t0 = blk * 512
                    b2 = bi % 2
                    for hh in range(4):
                        sfx = f'{hh}_{b2}'
                        ld('sp', qd[hh][b2][:], QD[d, hh][:, t0:t0 + 512], (), ('qd' + sfx,))
                        ld('act', kd[hh][b2][:], KD[d, hh][:, t0:t0 + 512], (), ('kd' + sfx,))
                        ld('sp', ks32[hh][b2][:], KS[d, hh][t0:t0 + 512, :].rearrange("(t p) k -> p t k", p=128), (), ('ks32' + sfx,))
                        for cc in range(4):
                            ld('act' if cc % 2 == 0 else 'sp', v32[hh][b2][cc * 32:(cc + 1) * 32, cc::4, :],
                               Vi[t0:t0 + 512, hh * 128:(hh + 1) * 128].rearrange("(t cc c) k -> cc c t k", cc=4, c=32)[cc], (), ('v32' + sfx,))
                        ld('sp', v128[hh][b2][:], Vi[t0:t0 + 512, hh * 128:(hh + 1) * 128].rearrange("(t p) k -> p t k", p=128), (), ('v128' + sfx,))
                    tiles = list(range(4)) if d == 0 else list(range(3, -1, -1))
                    for ti, t4 in enumerate(tiles):
                        for hh in range(4):
                            sfx = f'{hh}_{b2}'
                            tc = slice(t4 * 128, (t4 + 1) * 128)
                            A = pA[ai]; ak = f'pA{ai}'; ai = (ai + 1) % 2
                            mm(A[:, 0:128], kd[hh][b2][:, tc], qd[hh][b2][:, tc], True, True, ('kd' + sfx, 'qd' + sfx), (ak,))
                            am = Am[hh][ti % 2]; amk = f'Am{hh}_{ti % 2}'
                            tt('dve', am[:], A[:, 0:128], msk[:, d, :], ALU.mult, (ak, 'msk'), (amk,))
                            ok = f'poh{hh}'
                            mm(poh[hh][:, tc], v128[hh][b2][:, t4, :], am[:], True, False, ('v128' + sfx, amk), (ok,))
                        chunks = list(range(4)) if d == 0 else list(range(3, -1, -1))
                        for ci, cc in enumerate(chunks):
                            for hh in range(4):
                                sfx = f'{hh}_{b2}'
                                ok = f'poh{hh}'
                                n_loc = t4 * 4 + cc
                                n_glob = blk * 16 + n_loc
                                cs = slice(n_loc * 32, (n_loc + 1) * 32)
                                mm(poh[hh][:, cs], Spb[hh][:], qd[hh][b2][:, cs], False, ci == 3, (f'Spb{hh}', 'qd' + sfx), (ok,))
                                Pp = pP[ppi]; pk = f'pP{ppi}'; ppi = (ppi + 1) % 2
                                mm(Pp[:, 0:128], ks32[hh][b2][:, t4, :], v32[hh][b2][:, n_loc, :], True, True, ('ks32' + sfx, 'v32' + sfx), (pk,))
                                stt(state[hh][:], state[hh][:], cols[:, d * 4 + hh, 2, n_glob:n_glob + 1], Pp[:, 0:128], ALU.mult, ALU.add,
                                    (f'state{hh}', 'cols', pk), (f'state{hh}',))
                                n_next = n_glob + 1 if d == 0 else n_glob - 1
                                if 0 <= n_next < NCH:
                                    ts('dve', Spb[hh][:], state[hh][:], cols[:, d * 4 + hh, 1, n_next:n_next + 1], None, ALU.mult, None,
                                       (f'state{hh}', 'cols'), (f'Spb{hh}',))
                    for hh in range(4):
                        osk = f'ost{hh}_{b2}'
                        cp('act', ost[hh][b2][:], poh[hh][:], (f'poh{hh}',), (osk,))
                        ld('sp', OD[d, hh][:, t0:t0 + 512], ost[hh][b2][:], (osk,), ())
            cx.barrier()

    with ExitStack() as ps4:
        W3 = sbuf(ps4, "W3", [128, 8, 2560], BF16)
        Wa = sbuf(ps4, "Wa", [128, 4, 1024], BF16)
        Wb = sbuf(ps4, "Wb", [128, 4, 1024], BF16)
        Wo = sbuf(ps4, "Wo", [128, 8, 1024], BF16)
        Wr = sbuf(ps4, "Wr", [128, 8, 36], F32)
        gbc = sbuf(ps4, "gbc", [128, 1024], F32)
        rbias = sbuf(ps4, "rbias", [128, 36], F32)
        for c in range(8):
            load_cast(W3[:, c, :], w3_d[:, c, :], 2560, 'W3')
            load_cast(Wo[:, c, :], wo_d[:, c, :], 1024, 'Wo')
        for c in range(4):
            load_cast(Wa[:, c, :], wa_d[:, c, :], 1024, 'Wa')
            load_cast(Wb[:, c, :], wb_d[:, c, :], 1024, 'Wb')
        ld('sp', Wr[:], wr_d, (), ('Wr',), semkey='const')
        ld('sp', gbc[:], gffn_bc_d, (), ('gbc',), semkey='const')
        ld('sp', rbias[:], rbias_d, (), ('rbias',), semkey='const')
        cx.finalize_group('const', ['W3', 'Wo', 'Wa', 'Wb', 'Wr', 'gbc', 'rbias'])
        for c in range(8):
            ts('pool', Wr[:, c, :], Wr[:, c, :], vecs[:, 8 + c:9 + c], None, ALU.mult, None, ('Wr', 'cst'), ('Wr',))
        xTs = sbuf(ps4, "xTs3", [128, 8, 512], F32)
        sq = sbuf(ps4, "sq3", [128, 2, 512], BF16)
        hT = sbuf(ps4, "hT3", [128, 8, 512], BF16)
        rt = sbuf(ps4, "rt3", [128, 512], F32)
        rstd = sbuf(ps4, "rstd3", [128, 512], F32)
        AOs = sbuf(ps4, "AOs", [128, 4, 512], BF16)
        ODs = sbuf(ps4, "ODs", [128, 2, 4, 512], BF16)
        osum = sbuf(ps4, "osum", [128, 1, 512], F32)
        osq = sbuf(ps4, "osq", [128, 512], BF16)
        ort = sbuf(ps4, "ort", [128, 512], F32)
        orr = sbuf(ps4, "orr", [128, 512], F32)
        ho = sbuf(ps4, "ho", [128, 512], F32)
        sog = sbuf(ps4, "sog", [128, 512], BF16)
        HO = sbuf(ps4, "HO", [128, 4, 512], BF16)
        sga = sbuf(ps4, "sga", [128, 512], BF16)
        sgb = sbuf(ps4, "sgb", [128, 512], BF16)
        m1 = sbuf(ps4, "m1", [128, 512], F32)
        m2 = sbuf(ps4, "m2", [128, 512], F32)
        mg = sbuf(ps4, "mg", [128, 8, 512], BF16)
        x2T = xTs
        xtok = [sbuf(ps4, f"xtok{i}", [128, 1024], F32) for i in range(2)]
        x2tok = [sbuf(ps4, "x2tok0", [128, 1024], F32)] * 2
        junk = sbuf(ps4, "junk", [128, 1024], BF16)
        h2 = [sbuf(ps4, f"h2{i}", [128, 1024], BF16) for i in range(2)]
        sm = sbuf(ps4, "sm", [128, 64], F32)
        L_all = sbuf(ps4, "L_all", [128, NT, 36], F32)
        smr = [sbuf(ps4, f"smr{i}", [128, 16], F32) for i in range(4)]
        G1r = [sbuf(ps4, f"G1r{i}", [128, 4], F32) for i in range(4)]
        G2r = [sbuf(ps4, f"G2r{i}", [128, 4], F32) for i in range(4)]
        R1r = [sbuf(ps4, f"R1r{i}", [128, 32], F32) for i in range(4)]
        R2r = [sbuf(ps4, f"R2r{i}", [128, 32], F32) for i in range(4)]
        R1 = sbuf(ps4, "R1", [128, 32], F32)
        R2 = sbuf(ps4, "R2", [128, 32], F32)
        R3 = sbuf(ps4, "R3", [128, 32], F32)
        G1 = sbuf(ps4, "G1", [128, 4], F32)
        G2 = sbuf(ps4, "G2", [128, 4], F32)
        cum = sbuf(ps4, "cum", [128, 32], F32)
        p_ss = psum(ps4, "p_ss", [128, 512], F32)
        p_a = psum(ps4, "p_a", [128, 512], F32)
        p_b = psum(ps4, "p_b", [128, 512], F32)
        p_g = [psum(ps4, f"p_g{i}", [128, 512], F32) for i in range(2)]
        p_x = [psum(ps4, f"p_x{i}", [128, 512], F32) for i in range(2)]
        p_l = psum(ps4, "p_l", [128, 512], F32)
        gi = 0
        xi = 0

        def load_ao(b):
            tb = b * 512
            for c in range(4):
                ld('sp', AOs[:, c, :], AO[c][:, tb:tb + 512], (), (f'AOs{c}',))
                for d in range(2):
                    ld('act', ODs[:, d, c, :], OD[d, c][:, tb:tb + 512], (), (f'ODs{d}{c}',))

        def load_xtok(ti_):
            ld('sp', xtok[ti_ % 2][:], x_d[ti_ * 128:(ti_ + 1) * 128, :], (), (f'xtok{ti_ % 2}',))

        load_xtok(0)
        for blk in range(NB):
            t0 = blk * 512
            emit_norm(blk, xTs, sq, hT, rt, rstd, p_ss, 0)
            hr = tuple(f'hT{c}' for c in range(8))
            if blk == 0:
                load_ao(0)
            for hh in range(4):
                tt('pool', osum[:, 0, :], ODs[:, 0, hh, :], ODs[:, 1, hh, :], ALU.add, (f'ODs0{hh}', f'ODs1{hh}'), ('osum',))
                act(osq[:], osum[:, 0, :], AF.Square, ('osum',), ('osq',))
                mm(p_ss[:], ones_bf, osq[:], True, True, ('osq', 'cm'), ('ps_ss',))
                act(ort[:], p_ss[:], AF.Sqrt, ('ps_ss',), ('ort',), scale=1.0 / 128, bias=EPS)
                recip(orr[:], ort[:], ('ort',), ('orr',))
                stt(ho[:], osum[:, 0, :], vecs[:, 18:19], orr[:], ALU.mult, ALU.mult, ('osum', 'orr', 'cst'), ('ho',))
                G = p_g[gi]; gk = f'p_g{gi}'; gi = (gi + 1) % 2
                for c in range(8):
                    mm(G[:], W3[:, c, hh * 128:(hh + 1) * 128], hT[:, c, :], c == 0, c == 7, hr + ('W3',), (gk,))
                act(sog[:], G[:], AF.Silu, (gk,), ('sog',))
                tt('dve', HO[:, hh, :], ho[:], sog[:], ALU.mult, ('ho', 'sog'), (f'HO{hh}',))
            for oc in range(8):
                for c in range(4):
                    mm(p_a[:], Wa[:, c, oc * 128:(oc + 1) * 128], AOs[:, c, :], c == 0, c == 3, (f'AOs{c}', 'Wa'), ('p_a',))
                G = p_g[gi]; gk = f'p_g{gi}'; gi = (gi + 1) % 2
                for c in range(8):
                    mm(G[:], W3[:, c, 512 + oc * 128:512 + (oc + 1) * 128], hT[:, c, :], c == 0, c == 7, hr + ('W3',), (gk,))
                act(sga[:], G[:], AF.Sigmoid, (gk,), ('sga',))
                tt('dve', m1[:], p_a[:], sga[:], ALU.mult, ('p_a', 'sga'), ('m1',))
                for c in range(4):
                    mm(p_b[:], Wb[:, c, oc * 128:(oc + 1) * 128], HO[:, c, :], c == 0, c == 3, (f'HO{c}', 'Wb'), ('p_b',))
                G = p_g[gi]; gk = f'p_g{gi}'; gi = (gi + 1) % 2
                for c in range(8):
                    mm(G[:], W3[:, c, 1536 + oc * 128:1536 + (oc + 1) * 128], hT[:, c, :], c == 0, c == 7, hr + ('W3',), (gk,))
                act(sgb[:], G[:], AF.Sigmoid, (gk,), ('sgb',))
                tt('dve', m2[:], p_b[:], sgb[:], ALU.mult, ('p_b', 'sgb'), ('m2',))
                tt('pool', mg[:, oc, :], m1[:], m2[:], ALU.add, ('m1', 'm2'), (f'mg{oc}',))
            if blk + 1 < NB:
                load_ao(blk + 1)
            mr = tuple(f'mg{c}' for c in range(8))
            for oc in range(8):
                X = p_x[xi]; xk = f'p_x{xi}'; xi = (xi + 1) % 2
                for c in range(8):
                    mm(X[:], Wo[:, c, oc * 128:(oc + 1) * 128], mg[:, c, :], c == 0, c == 7, mr + ('Wo',), (xk,))
                tt('dve', x2T[:, oc, :], X[:], xTs[:, oc, :], ALU.add, (xk, f'xT{oc}'), (f'xT{oc}',))
            x2r = tuple(f'xT{c}' for c in range(8))
            for t4 in range(4):
                tile_i = blk * 4 + t4
                b2 = 0
                r0 = t0 + t4 * 128
                xb = tile_i % 2
                if tile_i + 1 < NT:
                    load_xtok(tile_i + 1)
                for half in range(2):
                    X = p_x[xi]; xk = f'p_x{xi}'; xi = (xi + 1) % 2
                    for c in range(8):
                        mm(X[:], mg[:, c, t4 * 128:(t4 + 1) * 128], Wo[:, c, half * 512:(half + 1) * 512], c == 0, c == 7, mr + ('Wo',), (xk,))
                    tt('dve', x2tok[b2][:, half * 512:(half + 1) * 512], X[:], xtok[xb][:, half * 512:(half + 1) * 512], ALU.add,
                       (xk, f'xtok{xb}'), (f'x2tok{b2}',))
                ld('act', X2[r0:r0 + 128, :], x2tok[b2][:], (f'x2tok{b2}',), ())
                act(junk[:], x2tok[b2][:], AF.Square, (f'x2tok{b2}',), ('junk',))
                red(sm[:, 0:1], junk[:], ALU.add, ('junk',), ('ssq',))
                act(sm[:, 1:2], sm[:, 0:1], AF.Sqrt, ('ssq',), ('rt2',), scale=1.0 / D, bias=EPS)
                recip(sm[:, 2:3], sm[:, 1:2], ('rt2',), ('r2',))
                stt(h2[b2][:], x2tok[b2][:], sm[:, 2:3], gbc[:], ALU.mult, ALU.mult, (f'x2tok{b2}', 'r2', 'gbc'), (f'h2{b2}',))
                ld('sp', H2[r0:r0 + 128, :], h2[b2][:], (f'h2{b2}',), ())
                for c in range(8):
                    mm(p_l[:, 0:36], x2T[:, c, t4 * 128:(t4 + 1) * 128], Wr[:, c, :], c == 0, c == 7, x2r + ('Wr',), ('p_l',))
                stt(L_all[:, tile_i, :], p_l[:, 0:36], sm[:, 2:3], rbias[:], ALU.mult, ALU.add, ('p_l', 'r2', 'rbias'), (f'L{tile_i}',))
        cx.barrier()

        def route_chain(tile_i, sl):
            st_ = []
            L = L_all[:, tile_i, :]
            smx = smr[sl]
            g1, g2, r1, r2_ = G1r[sl], G2r[sl], R1r[sl], R2r[sl]
            p = f'_{sl}'
            k1 = M12[:, tile_i, 0, :]
            k2 = M12[:, tile_i, 1, :]
            st_.append(lambda: red(smx[:, 3:4], L[:, 0:4], ALU.max, (), ('gmax' + p,)))
            st_.append(lambda: ts('dve', g1[:], L[:, 0:4], smx[:, 3:4], None, ALU.is_ge, None, ('gmax' + p,), ('G1' + p,)))
            st_.append(lambda: ts('dve', smx[:, 4:5], smx[:, 3:4], -1.0, None, ALU.mult, None, ('gmax' + p,), ('ngmax' + p,)))
            st_.append(lambda: act(g2[:], L[:, 0:4], AF.Exp, ('ngmax' + p,), ('G2' + p,), bias=smx[:, 4:5]))
            st_.append(lambda: red(smx[:, 5:6], g2[:], ALU.add, ('G2' + p,), ('gsum' + p,)))
            st_.append(lambda: recip(smx[:, 6:7], smx[:, 5:6], ('gsum' + p,), ('pg' + p,)))
            st_.append(lambda: ts('dve', g1[:], g1[:], -1.0, BIG, ALU.add, ALU.mult, ('G1' + p,), ('G1' + p,)))
            st_.append(lambda: tt('dve', r1[:].rearrange("p (g e) -> p g e", e=8), L[:, 4:36].rearrange("p (g e) -> p g e", e=8),
                                  g1[:].rearrange("p (g o) -> p g o", o=1).to_broadcast([128, 4, 8]), ALU.add, ('G1' + p,), ('R1' + p,)))
            st_.append(lambda: red(smx[:, 7:8], r1[:], ALU.max, ('R1' + p,), ('mx1' + p,)))
            st_.append(lambda: ts('dve', k1, r1[:], smx[:, 7:8], None, ALU.is_ge, None, ('R1' + p, 'mx1' + p), ('k1' + p,)))
            st_.append(lambda: stt(r2_[:], k1, -BIG, r1[:], ALU.mult, ALU.add, ('k1' + p, 'R1' + p), ('R2' + p,)))
            st_.append(lambda: red(smx[:, 8:9], r2_[:], ALU.max, ('R2' + p,), ('mx2' + p,)))
            st_.append(lambda: ts('dve', k2, r2_[:], smx[:, 8:9], None, ALU.is_ge, None, ('R2' + p, 'mx2' + p), ('k2' + p,)))
            st_.append(lambda: tt('dve', smx[:, 9:10], smx[:, 8:9], smx[:, 7:8], ALU.subtract, ('mx1' + p, 'mx2' + p), ('dm' + p,)))
            st_.append(lambda: act(smx[:, 10:11], smx[:, 9:10], AF.Exp, ('dm' + p,), ('edm' + p,)))
            st_.append(lambda: ts('dve', smx[:, 10:11], smx[:, 10:11], 1.0, None, ALU.add, None, ('edm' + p,), ('edm' + p,)))
            st_.append(lambda: recip(smx[:, 11:12], smx[:, 10:11], ('edm' + p,), ('p1' + p,)))
            st_.append(lambda: tt('dve', C12[:, tile_i, 0:1], smx[:, 11:12], smx[:, 6:7], ALU.mult, ('p1' + p, 'pg' + p), ('c1' + p,)))
            st_.append(lambda: tt('dve', C12[:, tile_i, 1:2], smx[:, 6:7], C12[:, tile_i, 0:1], ALU.subtract, ('pg' + p, 'c1' + p), ('c2' + p,)))
            return st_

        NSL = 4
        for t0_ in range(0, NT, NSL):
            chains = [route_chain(t0_ + sl, sl) for sl in range(min(NSL, NT - t0_))]
            for k in range(max(len(c) for c in chains)):
                for c in chains:
                    if k < len(c):
                        c[k]()
        cx.barrier()
        for tile_i in range(NT):
            k1 = M12[:, tile_i, 0, :]
            k2 = M12[:, tile_i, 1, :]
            tt('dve', R3[:], k1, k2, ALU.add, (), ('R3',))
            mm(p_l[:, 64:96], ustrict_f, R3[:], True, False, ('R3', 'cst'), ('p_l2',))
            mm(p_l[:, 64:96], ones_f, Macc[:], False, True, ('Macc', 'cst'), ('p_l2',))
            cp('act', cum[:], p_l[:, 64:96], ('p_l2',), ('cum',))
            tt('pool', Macc[:], Macc[:], R3[:], ALU.add, ('Macc', 'R3'), ('Macc',))
            rkk = f'RK_{tile_i}'
            tt('dve', R1[:], k1, cum[:], ALU.mult, ('cum',), ('R1',))
            red(RK[:, tile_i, 0:1], R1[:], ALU.add, ('R1',), (rkk + 'a',))
            tt('dve', R2[:], k2, cum[:], ALU.mult, ('cum',), ('R2',))
            red(RK[:, tile_i, 1:2], R2[:], ALU.add, ('R2',), (rkk + 'b',))
        cx.barrier()

    with ExitStack() as ps5:
        nf = sbuf(ps5, "nf", [128, 32], F32)
        ni = sbuf(ps5, "ni", [128, 32], I32)
        npad = sbuf(ps5, "npad", [128, 32], F32)
        endc = sbuf(ps5, "endc", [128, 32], F32)
        base = sbuf(ps5, "base", [128, 32], F32)
        onesr = sbuf(ps5, "onesr", [128, 32], F32)
        cmpb = sbuf(ps5, "cmpb", [128, NTILES, 32], F32)
        te = sbuf(ps5, "te", [128, NTILES], F32)
        wif = sbuf(ps5, "wif", [128, NTILES, 2], F32)
        Dtmp = sbuf(ps5, "Dtmp", [128, 32], F32)
        Df = sbuf(ps5, "Df", [128, NT, 2], F32)
        h2l = [sbuf(ps5, f"h2l{i}", [128, 1024], BF16) for i in range(2)]
        p_t = psum(ps5, "p_t", [128, 512], F32)
        mm(p_t[:, 0:32], ones_f, Macc[:], True, True, ('Macc', 'cst'), ('p_t',))
        ts('dve', nf[:], p_t[:, 0:32], 511.0, 1.0 / 512, ALU.add, ALU.mult, ('p_t',), ('nf',))
        ts('dve', nf[:], nf[:], -0.499, None, ALU.add, None, ('nf',), ('nf',))
        cp('dve', ni[:], nf[:], ('nf',), ('ni',))
        cp('dve', npad[:], ni[:], ('ni',), ('npad',))
        ts('dve', npad[:], npad[:], 512.0, None, ALU.mult, None, ('npad',), ('npad',))
        cx.op('pool', lambda: nc.gpsimd.memset(onesr[:], 1.0), (), ('onesr',))
        cx.op('dve', lambda: nc.vector.tensor_tensor_scan(out=endc[:], data0=onesr[:], data1=npad[:], initial=0.0,
                                                         op0=ALU.mult, op1=ALU.add), ('onesr', 'npad'), ('endc',))
        tt('dve', base[:], endc[:], npad[:], ALU.subtract, ('endc', 'npad'), ('base',))
        tt('dve', cmpb[:], endc[:].rearrange("p (o e) -> p o e", o=1).to_broadcast([128, NTILES, 32]),
           misc[:, 64:64 + NTILES].rearrange("p (t o) -> p t o", o=1).to_broadcast([128, NTILES, 32]), ALU.is_le, ('endc', 'cst'), ('cmpb',))
        red(te[:].rearrange("p (t o) -> p t o", o=1), cmpb[:], ALU.add, ('cmpb',), ('te',))
        ts('dve', te[:], te[:], 31.0, None, ALU.min, None, ('te',), ('te',))
        for half in range(2):
            ts('dve', wif[:, :, half], te[:], 256.0, float(half), ALU.mult, ALU.add, ('te',), ('wif',))
        sm5 = sbuf(ps5, "sm5", [128, 1], F32)
        ts('dve', sm5[:], misc[:, 32:33], 2.0, None, ALU.mult, None, ('cst',), ('sm5',))
        ts('dve', wif[:], wif[:], sm5[:, 0:1], None, ALU.add, None, ('wif', 'sm5'), ('wif',))
        cp('dve', WI[:], wif[:], ('wif',), ('WI',))
        for ti in range(NT):
            for k in range(2):
                tt('dve', Dtmp[:], M12[:, ti, k, :], base[:], ALU.mult, ('base',), ('Dtmp',))
                red(Df[:, ti, k:k + 1], Dtmp[:], ALU.add, ('Dtmp',), ('Df',))
        tt('dve', Df[:], Df[:], RK[:], ALU.add, ('Df',), ('Df',))
        cp('dve', DI[:], Df[:], ('Df',), ('DI',))
        for ti in range(NT):
            b2 = ti % 2
            ld('sp', h2l[b2][:], H2[ti * 128:(ti + 1) * 128, :], (), (f'h2l{b2}',))
            for k in range(2):
                cx.dma('pool', lambda: nc.gpsimd.indirect_dma_start(
                    out=XS[:, :], out_offset=bass.IndirectOffsetOnAxis(ap=DI[:, ti, k:k + 1], axis=0),
                    in_=h2l[b2][:, :], in_offset=None), (f'h2l{b2}', 'DI'), (), semkey=f'sc{b2}')
        cx.barrier()

    with ExitStack() as ps6:
        Wg = [sbuf(ps6, f"Wg{i}", [128, 8, 512], BF16) for i in range(2)]
        Wu = [sbuf(ps6, f"Wu{i}", [128, 8, 512], BF16) for i in range(2)]
        Wd = [sbuf(ps6, f"Wd{i}", [128, 4, 1024], BF16) for i in range(2)]
        Xr = [sbuf(ps6, f"Xr{i}", [128, 4, 1024], BF16) for i in range(2)]
        XT = sbuf(ps6, "XTm", [128, 8, 512], BF16)
        sg = [sbuf(ps6, f"sg{i}", [128, 512], F32) for i in range(2)]
        hid = sbuf(ps6, "hid", [128, 4, 512], BF16)
        Yst = [sbuf(ps6, f"Yst{i}", [128, 1024], BF16) for i in range(2)]
        pTm = [psum(ps6, f"pTm{i}", [128, 1024], BF16) for i in range(2)]
        pg_ = [psum(ps6, f"pgm{i}", [128, 512], F32) for i in range(2)]
        pu_ = [psum(ps6, f"pum{i}", [128, 512], F32) for i in range(2)]
        py_ = [psum(ps6, f"pym{i}", [128, 512], F32) for i in range(2)]
        ti2 = 0
        gi = 0
        yi = 0
        ysi = 0
        for t in range(NTILES):
            b2 = t % 2
            for half in range(2):
                for (Wt, src, nm, ncc) in ((Wg[b2], wg_d, 'Wg', 4), (Wu[b2], wu_d, 'Wu', 4), (Wd[b2], wd_d, 'Wd', 2)):
                    dst = Wt[:, half * ncc:(half + 1) * ncc, :].rearrange("p c n -> p (c n)")
                    cx.dma('pool', lambda dst=dst, src=src: nc.gpsimd.indirect_dma_start(
                        out=dst, out_offset=None, in_=src[:, :],
                        in_offset=bass.IndirectOffsetOnAxis(ap=WI[:, t, half:half + 1], axis=0)), ('WI',), (f'{nm}{b2}',), semkey=f'{nm}{b2}')
            ld('sp', Xr[b2][:], XS[t * 512:(t + 1) * 512, :].rearrange("(s p) d -> p s d", p=128), (), (f'Xr{b2}',))
            for s in range(4):
                T = pTm[ti2]; tk = f'pTm{ti2}'; ti2 = (ti2 + 1) % 2
                for c in range(8):
                    tr(T[:, c * 128:(c + 1) * 128], Xr[b2][:, s, c * 128:(c + 1) * 128], ident, (f'Xr{b2}', 'cm'), (tk,))
                cp('dve' if s % 2 == 0 else 'act', XT[:, :, s * 128:(s + 1) * 128], T[:].rearrange("p (c k) -> p c k", k=128), (tk,), (f'XT{s}',))
            xr = tuple(f'XT{s}' for s in range(4))
            for hc in range(4):
                Gp = pg_[gi]; gk = f'pgm{gi}'
                Up = pu_[gi]; uk = f'pum{gi}'
                sgt = sg[gi]; sk = f'sg{gi}'
                gi = (gi + 1) % 2
                for c in range(8):
                    mm(Gp[:], Wg[b2][:, c, hc * 128:(hc + 1) * 128], XT[:, c, :], c == 0, c == 7, xr + (f'Wg{b2}',), (gk,))
                for c in range(8):
                    mm(Up[:], Wu[b2][:, c, hc * 128:(hc + 1) * 128], XT[:, c, :], c == 0, c == 7, xr + (f'Wu{b2}',), (uk,))
                act(sgt[:], Gp[:], AF.Silu, (gk,), (sk,))
                tt('dve', hid[:, hc, :], Up[:], sgt[:], ALU.mult, (uk, sk), (f'hid{hc}',))
            hr4 = tuple(f'hid{c}' for c in range(4))
            for s in range(4):
                ys = Yst[ysi]; ysk = f'Yst{ysi}'; ysi = (ysi + 1) % 2
                for half in range(2):
                    Y = py_[yi]; yk = f'pym{yi}'; yi = (yi + 1) % 2
                    for c in range(4):
                        mm(Y[:], hid[:, c, s * 128:(s + 1) * 128], Wd[b2][:, c, half * 512:(half + 1) * 512], c == 0, c == 3, hr4 + (f'Wd{b2}',), (yk,))
                    cp('act' if half == 0 else 'dve', ys[:, half * 512:(half + 1) * 512], Y[:], (yk,), (ysk,))
                ld('act', YS[t * 512 + s * 128:t * 512 + (s + 1) * 128, :], ys[:], (ysk,), ())
        cx.barrier()

    with ExitStack() as ps7:
        x2l = [sbuf(ps7, f"x2l{i}", [128, 1024], F32) for i in range(2)]
        y1 = [sbuf(ps7, f"y1{i}", [128, 1024], BF16) for i in range(2)]
        y2 = [sbuf(ps7, f"y2{i}", [128, 1024], BF16) for i in range(2)]
        acc = [sbuf(ps7, f"acc{i}", [128, 1024], F32) for i in range(2)]
        for ti in range(NT):
            b2 = ti % 2
            ld('sp', x2l[b2][:], X2[ti * 128:(ti + 1) * 128, :], (), (f'x2l{b2}',))
            for k, yt, nm in ((0, y1[b2], 'y1'), (1, y2[b2], 'y2')):
                cx.dma('pool', lambda yt=yt, k=k: nc.gpsimd.indirect_dma_start(
                    out=yt[:, :], out_offset=None, in_=YS[:, :],
                    in_offset=bass.IndirectOffsetOnAxis(ap=DI[:, ti, k:k + 1], axis=0)), ('DI',), (f'{nm}{b2}',))
            stt(acc[b2][:], y1[b2][:], C12[:, ti, 0:1], x2l[b2][:], ALU.mult, ALU.add, (f'y1{b2}', f'x2l{b2}'), (f'acc{b2}',))
            stt(acc[b2][:], y2[b2][:], C12[:, ti, 1:2], acc[b2][:], ALU.mult, ALU.add, (f'y2{b2}', f'acc{b2}'), (f'acc{b2}',))
            ld('sp', out_d[ti * 128:(ti + 1) * 128, :], acc[b2][:], (f'acc{b2}',), ())
        cx.barrier()
    es.close()
    return nc


def _consts(S):
    NTILES = (2 * S) // TROWS + 32
    ident = np.eye(128, dtype=np.float32)
    ones = np.ones((128, 128), np.float32)
    blk = np.zeros((128, 128), np.float32)
    blk[:64, :64] = 1
    blk[64:, 64:] = 1
    swap = np.zeros((128, 128), np.float32)
    for h in range(2):
        for i in range(32):
            swap[h * 64 + 32 + i, h * 64 + i] = 1
            swap[h * 64 + i, h * 64 + 32 + i] = 1
    ust = np.triu(np.ones((128, 128), np.float32), 1)
    cm = np.stack([ident, ones, blk, swap, ust]).astype(np.float32)
    s_idx = np.arange(128)[:, None]
    c_idx = np.arange(128)[None, :]
    same = (s_idx // 32) == (c_idx // 32)
    mf = (same & (s_idx <= c_idx)).astype(np.float32)
    mb = (same & (s_idx >= c_idx)).astype(np.float32)
    masks = np.stack([mf, mb])
    seg = np.ones((128, 512), np.float32)
    seg[:, ::32] = 0
    misc = np.zeros((128, 192), np.float32)
    misc[:, 0:32] = np.arange(32)[None, :]
    misc[:, 32] = np.arange(128)
    misc[:, 64:64 + NTILES] = (np.arange(NTILES) * TROWS)[None, :]
    rows = S // 64
    row_ids = np.repeat(np.arange(rows), 64).astype(np.float32)
    col_ids = np.tile(np.arange(64), rows).astype(np.float32)
    inv_freq = (10000.0 ** (-np.arange(0, 32, 2, dtype=np.float32) / 32)).astype(np.float32)
    ang = np.concatenate([row_ids[:, None] * inv_freq, col_ids[:, None] * inv_freq], axis=-1).astype(np.float32)
    cos = np.cos(ang).T.astype(np.float32)
    sin = np.sin(ang).T.astype(np.float32)
    cosT = np.tile(cos, (4, 1))
    sinT = np.tile(np.concatenate([-sin, sin], axis=0), (2, 1))
    return dict(cmats=cm, masks=masks, segmask=seg, misc=misc, cosT=np.ascontiguousarray(cosT), sinT=np.ascontiguousarray(sinT))


def _fm(w, nchunk):
    return np.ascontiguousarray(w.reshape(nchunk, 128, -1).transpose(1, 0, 2))


def _prep_shared(inp):
    w_in = inp["w_in"][0]
    offs = np.cumsum([0, 512, 128, 128, 512, 512, 512, 512, 512, 1024, 1024])
    aq, ak, av, hq, hff, hfb, hi, hg, ga, gb = [w_in[:, offs[i]:offs[i + 1]] for i in range(10)]
    deint = np.concatenate([np.arange(0, 64, 2), np.arange(1, 64, 2)])
    qperm = np.concatenate([h * 64 + deint for h in range(8)])
    aqp = aq[:, qperm]
    k0 = ak[:, 0:64][:, deint]
    k1 = ak[:, 64:128][:, deint]
    w1 = np.concatenate([aqp, k0, k0, k1, k1, hq, hff, hfb, av, hi], axis=1)
    w3 = np.concatenate([hg, ga, gb], axis=1)
    vec = np.zeros((128, 64), np.float32)
    vec[:, 0:8] = inp["g_mix"][0].reshape(8, 128).T
    vec[:, 8:16] = inp["g_ffn"][0].reshape(8, 128).T
    vec[:, 16] = np.tile(inp["q_norm"][0][deint], 2)
    vec[:, 17] = np.tile(inp["k_norm"][0][deint], 2)
    vec[:, 18] = inp["hgrn_norm"][0]
    lbf = np.stack([inp["lb_fwd"].reshape(2, 4, 128), inp["lb_bwd"].reshape(2, 4, 128)])
    lbf = np.ascontiguousarray(lbf.transpose(3, 0, 1, 2)).astype(np.float32)
    wr = np.concatenate([inp["w_router_group"][0], inp["w_router_expert"][0]], axis=1)
    rb = np.concatenate([inp["b_router_group"][0], inp["b_router_expert"][0]])[None, :].repeat(128, 0)

    def exp_rows(w, nchunk):
        n = w.shape[-1]
        a = w.reshape(32, 2, nchunk // 2, 128, n).transpose(0, 3, 1, 2, 4)
        return np.ascontiguousarray(a.reshape(32 * 128 * 2, (nchunk // 2) * n))

    return dict(
        w1=_fm(w1, 8), w3=_fm(w3, 8), wa=_fm(inp["w_attn_branch"][0], 4), wb=_fm(inp["w_hgrn_branch"][0], 4),
        wo=_fm(inp["w_out"][0], 8), wr=_fm(wr, 8).astype(np.float32),
        wg=exp_rows(inp["w_exp_gate"][0], 8), wu=exp_rows(inp["w_exp_up"][0], 8), wd=exp_rows(inp["w_exp_down"][0], 4),
        vecs=vec, lbf=lbf, gffn_bc=np.ascontiguousarray(inp["g_ffn"][0][None, :].repeat(128, 0)).astype(np.float32),
        rbias=np.ascontiguousarray(rb).astype(np.float32),
    )


def run(inp, cores=None, debug=False):
    x = np.asarray(inp["x"], np.float32)
    B, S, _ = x.shape
    inp = {k: np.asarray(v, np.float32) for k, v in inp.items()}
    shared = _prep_shared(inp)
    shared.update(_consts(S))
    nc = build(S, debug=debug)
    in_maps = []
    for b in range(B):
        m = dict(shared)
        m["x"] = np.ascontiguousarray(x[b])
        m["xT"] = np.ascontiguousarray(x[b].T)
        in_maps.append(m)
    res = run_bass_kernel_spmd(nc, in_maps, core_ids=list(range(B)) if cores is None else cores)
    if debug:
        return res.results
    return np.stack([r["out"] for r in res.results]).astype(np.float32)


def kernel(**inputs):
    return run(inputs)
```

```python
import numpy as np
import ml_dtypes
from contextlib import ExitStack
import concourse.bass as bass
import concourse.mybir as mybir
from concourse.bass_utils import run_bass_kernel_spmd

F32 = mybir.dt.float32
BF16 = mybir.dt.bfloat16
I32 = mybir.dt.int32
ALU = mybir.AluOpType
AF = mybir.ActivationFunctionType
AX = mybir.AxisListType

D = 1024
EPS = 1e-6
HGRN_SCALE = 128 ** -0.5
EPOCH = 30000
BIG = 1.0e30
TROWS = 512
OPT = {'spb_act': True, 'hla': True}


class Ctx:
    def __init__(self, nc):
        self.nc = nc
        self.E = {'pe': nc.tensor, 'act': nc.scalar, 'dve': nc.vector, 'pool': nc.gpsimd, 'sp': nc.sync}
        self.cnt = {e: 0 for e in self.E}
        self.known = {e: {} for e in self.E}
        self.esem = {e: [] for e in self.E}
        self.st = {}
        self.dsem = {}
        self.dcount = {}
        self.free_dsems = []
        self.nsem = 0
        self.all_dsems = []

    def newsem(self):
        self.nsem += 1
        return self.nc.alloc_semaphore(f"sm{self.nsem}")

    def _need(self, eng, tok):
        _, sem, val = tok
        k = self.known[eng]
        if k.get(id(sem), 0) >= val:
            return
        self.E[eng].wait_ge(sem, val)
        k[id(sem)] = val

    def _deps(self, eng, r, w, selfsync):
        deps = []
        for key in r:
            s = self.st.get(key)
            if s and s['w']:
                deps.append(s['w'])
        for key in w:
            s = self.st.get(key)
            if s:
                if s['w']:
                    deps.append(s['w'])
                deps.extend(s['r'].values())
        for tok in deps:
            if tok[0] == eng and not selfsync:
                continue
            self._need(eng, tok)

    def _mark(self, tok, r, w):
        for key in r:
            self.st.setdefault(key, {'w': None, 'r': {}})['r'][id(tok[1])] = tok
        for key in w:
            self.st[key] = {'w': tok, 'r': {}}

    def op(self, eng, fn, r=(), w=(), selfsync=True):
        self._deps(eng, r, w, selfsync)
        ins = fn()
        self.cnt[eng] += 1
        n = self.cnt[eng]
        idx = (n - 1) // EPOCH
        while len(self.esem[eng]) <= idx:
            self.esem[eng].append(self.newsem())
        sem = self.esem[eng][idx]
        val = (n - 1) % EPOCH + 1
        ins.then_inc(sem, 1)
        tok = (eng, sem, val)
        self._mark(tok, r, w)
        return tok

    def _dsem_for(self, key):
        if key not in self.dsem:
            if self.free_dsems:
                sem = self.free_dsems.pop()
            else:
                sem = self.newsem()
                self.all_dsems.append(sem)
                self.dcount[id(sem)] = 0
            self.dsem[key] = sem
        return self.dsem[key]

    def dma(self, q, fn, r=(), w=(), semkey=None):
        self._deps(q, r, w, True)
        key = semkey or (w[0] if w else r[0])
        sem = self._dsem_for(key)
        ins = fn()
        ins.then_inc(sem, 16)
        self.dcount[id(sem)] += 1
        tok = ('dma', sem, 16 * self.dcount[id(sem)])
        self._mark(tok, r, w)
        return tok

    def finalize_group(self, semkey, keys):
        sem = self.dsem[semkey]
        tok = ('dma', sem, 16 * self.dcount[id(sem)])
        for key in keys:
            self.st[key] = {'w': tok, 'r': {}}

    def barrier(self):
        toks = []
        for e in self.E:
            n = self.cnt[e]
            if n > 0:
                idx = (n - 1) // EPOCH
                toks.append((e, self.esem[e][idx], (n - 1) % EPOCH + 1))
        for sem in self.all_dsems:
            c = self.dcount[id(sem)]
            if c > 0:
                toks.append(('dma', sem, 16 * c))
        for e in self.E:
            for tok in toks:
                if tok[0] == e:
                    continue
                self._need(e, tok)
        self.st = {}
        self.free_dsems = list(self.all_dsems)
        self.dsem = {}


def build(S, debug=False):
    NB = S // 512
    NT = S // 128
    NCH = S // 32
    NTILES = (2 * S) // TROWS + 32
    NROWS = NTILES * TROWS
    nc = bass.Bass("TRN2", target_bir_lowering=False)
    cx = Ctx(nc)
    es = ExitStack()

    def din(name, shape, dt=F32):
        return nc.dram_tensor(name, list(shape), dt, kind="ExternalInput").ap()

    def dscr(name, shape, dt):
        return nc.dram_tensor(name, list(shape), dt, kind="ExternalOutput" if debug else "Internal").ap()

    xT_d = din("xT", [D, S])
    x_d = din("x", [S, D])
    w1_d = din("w1", [128, 8, 2944])
    w3_d = din("w3", [128, 8, 2560])
    wa_d = din("wa", [128, 4, 1024])
    wb_d = din("wb", [128, 4, 1024])
    wo_d = din("wo", [128, 8, 1024])
    wr_d = din("wr", [128, 8, 36])
    wg_d = din("wg", [32 * 128 * 2, 2048])
    wu_d = din("wu", [32 * 128 * 2, 2048])
    wd_d = din("wd", [32 * 128 * 2, 2048])
    cos_d = din("cosT", [128, S])
    sin_d = din("sinT", [128, S])
    vec_d = din("vecs", [128, 64])
    lbf_d = din("lbf", [128, 2, 2, 4])
    gffn_bc_d = din("gffn_bc", [128, 1024])
    rbias_d = din("rbias", [128, 36])
    cm_d = din("cmats", [5, 128, 128])
    msk_d = din("masks", [2, 128, 128])
    seg_d = din("segmask", [128, 512])
    misc_d = din("misc", [128, 64 + 128])

    out_d = nc.dram_tensor("out", [S, D], F32, kind="ExternalOutput").ap()

    QT = dscr("QT", [4, 128, S], BF16)
    KT = dscr("KT", [2, 128, S], BF16)
    Vd = dscr("Vd", [S, 128], BF16)
    Vi = dscr("Vi", [S, 512], BF16)
    QD = dscr("QD", [2, 4, 128, S], BF16)
    KD = dscr("KD", [2, 4, 128, S], BF16)
    KS = dscr("KS", [2, 4, S, 128], BF16)
    COLS = dscr("COLS", [2, 4, 128, 3, NCH], F32)
    OD = dscr("OD", [2, 4, 128, S], BF16)
    AO = dscr("AO", [4, 128, S], BF16)
    X2 = dscr("X2", [S, D], F32)
    H2 = dscr("H2", [S, D], BF16)
    XS = dscr("XS", [NROWS, D], BF16)
    YS = dscr("YS", [NROWS, D], BF16)

    uniq = [0]

    def sbuf(stack, name, shape, dt):
        uniq[0] += 1
        return stack.enter_context(nc.sbuf_tensor(f"{name}_u{uniq[0]}", list(shape), dt))

    def psum(stack, name, shape, dt):
        uniq[0] += 1
        return stack.enter_context(nc.psum_tensor(f"{name}_u{uniq[0]}", list(shape), dt))

    def mm(out, lhsT, rhs, start, stop, r, w):
        return cx.op('pe', lambda: nc.tensor.matmul(out, lhsT=lhsT, rhs=rhs, start=start, stop=stop), r, w, selfsync=False)

    def tr(out, in_, ident, r, w):
        return cx.op('pe', lambda: nc.tensor.transpose(out, in_, ident), r, w, selfsync=False)

    def act(out, in_, func, r, w, scale=1.0, bias=0.0, accum_out=None):
        if accum_out is None:
            return cx.op('act', lambda: nc.scalar.activation(out=out, in_=in_, func=func, bias=bias, scale=scale), r, w)
        return cx.op('act', lambda: nc.scalar.activation(out=out, in_=in_, func=func, bias=bias, scale=scale, accum_out=accum_out), r, w)

    def tt(eng, out, in0, in1, op, r, w):
        e = nc.vector if eng == 'dve' else nc.gpsimd
        return cx.op(eng, lambda: e.tensor_tensor(out=out, in0=in0, in1=in1, op=op), r, w)

    def ts(eng, out, in0, s1, s2, op0, op1, r, w):
        e = nc.vector if eng == 'dve' else nc.gpsimd
        if s2 is None:
            return cx.op(eng, lambda: e.tensor_scalar(out=out, in0=in0, scalar1=s1, scalar2=None, op0=op0), r, w)
        return cx.op(eng, lambda: e.tensor_scalar(out=out, in0=in0, scalar1=s1, scalar2=s2, op0=op0, op1=op1), r, w)

    def stt(out, in0, scalar, in1, op0, op1, r, w):
        return cx.op('dve', lambda: nc.vector.scalar_tensor_tensor(out=out, in0=in0, scalar=scalar, in1=in1, op0=op0, op1=op1), r, w)

    def recip(out, in_, r, w):
        return cx.op('dve', lambda: nc.vector.reciprocal(out=out, in_=in_), r, w)

    def cp(eng, out, in_, r, w):
        if eng == 'act':
            return cx.op('act', lambda: nc.scalar.copy(out=out, in_=in_), r, w)
        e = nc.vector if eng == 'dve' else nc.gpsimd
        return cx.op(eng, lambda: e.tensor_copy(out=out, in_=in_), r, w)

    def red(out, in_, op, r, w):
        return cx.op('dve', lambda: nc.vector.tensor_reduce(out=out, in_=in_, axis=AX.X, op=op), r, w)

    def ld(q, out, in_, r, w, semkey=None):
        e = cx.E[q]
        return cx.dma(q, lambda: e.dma_start(out=out, in_=in_), r, w, semkey)

    def load_cast(dst, src, n, key):
        o = 0
        while o < n:
            m = min(2048, n - o)
            ld('pool', dst[:, o:o + m], src[:, o:o + m], (), (key,), semkey='const')
            o += m

    cm = sbuf(es, "cm", [128, 5, 128], BF16)
    cmf = sbuf(es, "cmf", [128, 5, 128], F32)
    vecs = sbuf(es, "vecs_sb", [128, 64], F32)
    misc = sbuf(es, "misc_sb", [128, 192], F32)
    M12 = sbuf(es, "M12", [128, NT, 2, 32], F32)
    RK = sbuf(es, "RK", [128, NT, 2], F32)
    C12 = sbuf(es, "C12", [128, NT, 2], F32)
    Macc = sbuf(es, "Macc", [128, 32], F32)
    WI = sbuf(es, "WI", [128, NTILES, 2], I32)
    DI = sbuf(es, "DI", [128, NT, 2], I32)
    lbt = sbuf(es, "lbt", [128, 2, 2, 4], F32)
    lbc = sbuf(es, "lbc", [128, 8], F32)
    oml = sbuf(es, "oml", [128, 8], F32)

    for i in range(5):
        ld('sp', cmf[:, i, :], cm_d[i], (), ('cst',), semkey='const')
    ld('sp', vecs[:], vec_d, (), ('cst',), semkey='const')
    ld('sp', misc[:], misc_d, (), ('cst',), semkey='const')
    ld('sp', lbt[:], lbf_d, (), ('cst',), semkey='const')
    cx.finalize_group('const', ['cst'])
    cp('dve', cm[:], cmf[:], ('cst',), ('cm',))
    cx.op('pool', lambda: nc.gpsimd.memset(Macc[:], 0.0), (), ('Macc',))
    tt('dve', lbc[:].rearrange("p (d h) -> p d h", d=2), lbt[:, :, 1, :], lbt[:, :, 0, :], ALU.subtract, ('cst',), ('lbc',))
    act(lbc[:], lbc[:], AF.Exp, ('lbc',), ('lbc',))
    ts('dve', lbc[:], lbc[:], 1.0, None, ALU.add, None, ('lbc',), ('lbc',))
    recip(lbc[:], lbc[:], ('lbc',), ('lbc',))
    ts('dve', oml[:], lbc[:], -1.0, 1.0, ALU.mult, ALU.add, ('lbc',), ('oml',))

    ident = cm[:, 0, :]
    ones_bf = cm[:, 1, :]
    blk64 = cm[:, 2, :]
    swapm = cm[:, 3, :]
    ones_f = cmf[:, 1, :]
    ustrict_f = cmf[:, 4, :]

    def emit_norm(blk, xTs, sq, hT, rt, rstd, ps_ss, gcol0, lq=None):
        t0 = blk * 512
        for c in range(8):
            ld(lq or ('sp' if c % 2 == 0 else 'act'), xTs[:, c, :], xT_d[c * 128:(c + 1) * 128, t0:t0 + 512], (), (f'xT{c}',))
        for c in range(8):
            act(sq[:, c % 2, :], xTs[:, c, :], AF.Square, (f'xT{c}',), (f'sq{c % 2}',))
            mm(ps_ss[:], ones_bf, sq[:, c % 2, :], c == 0, c == 7, (f'sq{c % 2}', 'cm'), ('ps_ss',))
        act(rt[:], ps_ss[:], AF.Sqrt, ('ps_ss',), ('rt',), scale=1.0 / D, bias=EPS)
        recip(rstd[:], rt[:], ('rt',), ('rstd',))
        for c in range(8):
            stt(hT[:, c, :], xTs[:, c, :], vecs[:, gcol0 + c:gcol0 + c + 1], rstd[:], ALU.mult, ALU.mult,
                (f'xT{c}', 'rstd', 'cst'), (f'hT{c}',))

    with ExitStack() as ps1:
        W1 = sbuf(ps1, "W1", [128, 8, 2944], BF16)
        for c in range(8):
            load_cast(W1[:, c, :], w1_d[:, c, :], 2944, 'W1')
        cx.finalize_group('const', ['W1'])
        xTs = sbuf(ps1, "xTs", [128, 8, 512], F32)
        sq = sbuf(ps1, "sq", [128, 2, 512], BF16)
        hT = sbuf(ps1, "hT", [128, 8, 512], BF16)
        rt = sbuf(ps1, "rt", [128, 512], F32)
        rstd = sbuf(ps1, "rstd", [128, 512], F32)
        cos_sb = sbuf(ps1, "cos_sb", [128, 512], F32)
        sin_sb = sbuf(ps1, "sin_sb", [128, 512], F32)
        seg = sbuf(ps1, "seg", [128, 512], F32)
        msk = None
        QTst = sbuf(ps1, "QTst", [128, 6, 512], BF16)
        QDst = sbuf(ps1, "QDst", [128, 8, 512], BF16)
        KDst = sbuf(ps1, "KDst", [128, 8, 512], BF16)
        KSst = sbuf(ps1, "KSst", [128, 8, 4, 128], BF16)
        VIst = sbuf(ps1, "VIst", [128, 4, 640], BF16)
        CLst = sbuf(ps1, "CLst", [128, 8, 3, 16], F32)
        sqh = sbuf(ps1, "sqh", [128, 4, 512], BF16)
        sqq = sbuf(ps1, "sqq", [128, 2, 512], BF16)
        tq = sbuf(ps1, "tq", [128, 512], F32)
        rq = sbuf(ps1, "rq", [128, 512], F32)
        qn = sbuf(ps1, "qn", [128, 2, 512], BF16)
        t1 = sbuf(ps1, "t1", [128, 512], F32)
        t2 = sbuf(ps1, "t2", [128, 512], F32)
        TA = [sbuf(ps1, f"TA{i}", [128, 512], F32) for i in range(2)]
        TB = [sbuf(ps1, f"TB{i}", [128, 512], F32) for i in range(2)]
        TC = [sbuf(ps1, f"TC{i}", [128, 512], F32) for i in range(2)]
        TD = [sbuf(ps1, f"TD{i}", [128, 512], F32) for i in range(2)]
        TE = [sbuf(ps1, f"TE{i}", [128, 512], F32) for i in range(2)]
        cl = [sbuf(ps1, f"cl{i}", [128, 16], F32) for i in range(2)]
        ksT = [sbuf(ps1, f"ksT{i}", [128, 512], BF16) for i in range(2)]
        ps_ss = psum(ps1, "ps_ss", [128, 512], F32)
        pp = [psum(ps1, f"pp{i}", [128, 512], F32) for i in range(3)]
        ps_sum = psum(ps1, "ps_sum", [128, 512], F32)
        ps_swap = psum(ps1, "ps_swap", [128, 512], F32)
        psT = [psum(ps1, f"psT{i}", [128, 1024], BF16) for i in range(2)]
        ld('sp', seg[:], seg_d, (), ('seg',))
        pi = 0
        it = 0
        dq = []

        def defer(n, fn):
            dq.append([n, fn])

        def tick():
            for e in dq:
                e[0] -= 1
            ready = [e for e in dq if e[0] <= 0]
            for e in ready:
                dq.remove(e)
            for e in ready:
                e[1]()

        def f_chain(j, P, pk, s, t0, blk):
            steps = []
            dh = j - 10
            d = dh // 4
            hh = dh % 4
            A, B, C, Dd, Eb = TA[s], TB[s], TC[s], TD[s], TE[s]
            kA, kB, kC, kD, kE = f'TA{s}', f'TB{s}', f'TC{s}', f'TD{s}', f'TE{s}'
            C3 = C[:].rearrange("p (n c) -> p n c", c=32)
            D3 = Dd[:].rearrange("p (n c) -> p n c", c=32)
            ref = 16 if d == 0 else 15
            last = 31 if d == 0 else 0
            clk = f'cl{s}'
            ck = f'CLst{dh}'
            kst = ksT[s]
            steps.append(lambda: act(A[:], P[:], AF.Exp, (pk,), (kA,), scale=-1.0))
            steps.append(lambda: ts('pool', A[:], A[:], 1.0, None, ALU.add, None, (kA,), (kA,)))
            steps.append(lambda: recip(A[:], A[:], (kA,), (kA,)))
            steps.append(lambda: ts('dve', A[:], A[:], oml[:, dh:dh + 1], lbc[:, dh:dh + 1], ALU.mult, ALU.add, (kA, 'oml', 'lbc'), (kA,)))
            steps.append(lambda: act(B[:], A[:], AF.Ln, (kA,), (kB,)))
            steps.append(lambda: cx.op('dve', lambda: nc.vector.tensor_tensor_scan(out=C[:], data0=seg[:], data1=B[:], initial=0.0,
                                                                                  op0=ALU.mult, op1=ALU.add), (kB, 'seg'), (kC,)))
            if d == 1:
                steps.append(lambda: tt('dve', D3, C3[:, :, 31:32].to_broadcast([128, 16, 32]), C3, ALU.subtract, (kC,), (kD,)))
                steps.append(lambda: tt('dve', C[:], Dd[:], B[:], ALU.add, (kD, kB), (kC,)))
            steps.append(lambda: tt('dve', D3, C3, C3[:, :, ref:ref + 1].to_broadcast([128, 16, 32]), ALU.subtract, (kC,), (kD,)))
            steps.append(lambda: tt('dve', cl[s][:].rearrange("p (n o) -> p n o", o=1), C3[:, :, last:last + 1], C3[:, :, ref:ref + 1], ALU.subtract, (kC,), (clk,)))
            steps.append(lambda: act(B[:], Dd[:], AF.Exp, (kD,), (kB,)))
            steps.append(lambda: act(Eb[:], Dd[:], AF.Exp, (kD,), (kE,), scale=-1.0))

            def cols_():
                act(CLst[:, dh, 0, :], cl[s][:], AF.Exp, (clk,), (ck,))
                act(CLst[:, dh, 1, :].rearrange("p (n o) -> p n o", o=1), C3[:, :, ref:ref + 1], AF.Exp, (kC,), (ck,))
                act(CLst[:, dh, 2, :].rearrange("p (n o) -> p n o", o=1), C3[:, :, last:last + 1], AF.Exp, (kC,), (ck,))
                ld('sp', COLS[d, hh][:, :, blk * 16:(blk + 1) * 16], CLst[:, dh, :, :], (ck,), ())
            steps.append(cols_)
            steps.append(lambda: stt(QDst[:, dh, :], sqh[:, hh, :], HGRN_SCALE, B[:], ALU.mult, ALU.mult, (f'sqh{hh}', kB), (f'QDst{dh}',)))
            steps.append(lambda: ts('pool', A[:], A[:], -1.0, 1.0, ALU.mult, ALU.add, (kA,), (kA,)))
            steps.append(lambda: tt('dve', KDst[:, dh, :], A[:], Eb[:], ALU.mult, (kA, kE), (f'KDst{dh}',)))

            def stores_():
                ld('sp', QD[d, hh][:, t0:t0 + 512], QDst[:, dh, :], (f'QDst{dh}',), ())
                ld('sp', KD[d, hh][:, t0:t0 + 512], KDst[:, dh, :], (f'KDst{dh}',), ())
            steps.append(stores_)
            steps.append(lambda: tt('dve', kst[:].rearrange("p (n c) -> p n c", c=32), KDst[:, dh, :].rearrange("p (n c) -> p n c", c=32),
                                    CLst[:, dh, 0, :].rearrange("p (n o) -> p n o", o=1).to_broadcast([128, 16, 32]), ALU.mult,
                                    (f'KDst{dh}', ck), (f'ksT{s}',)))

            def stage_t():
                pT = psT[s]
                for t4 in range(4):
                    tr(pT[:, t4 * 128:(t4 + 1) * 128], kst[:, t4 * 128:(t4 + 1) * 128], ident, (f'ksT{s}', 'cm'), (f'psT{s}',))
                cp('act', KSst[:, dh, :, :], pT[:, 0:512].rearrange("p (t k) -> p t k", k=128), (f'psT{s}',), (f'KSst{dh}',))
                ld('sp', KS[d, hh][t0:t0 + 512, :].rearrange("(t p) k -> p t k", p=128), KSst[:, dh, :, :], (f'KSst{dh}',), ())
            steps.append(lambda: defer(2, stage_t))
            return steps

        pend_chain = None
        for blk in range(NB):
            t0 = blk * 512
            emit_norm(blk, xTs, sq, hT, rt, rstd, ps_ss, 0, lq='pool')
            ld('pool', cos_sb[:], cos_d[:, t0:t0 + 512], (), ('cos',))
            ld('pool', sin_sb[:], sin_d[:, t0:t0 + 512], (), ('sin',))
            hr = tuple(f'hT{c}' for c in range(8))
            for t4 in range(4):
                P = pp[pi]; pk = f'pp{pi}'; pi = (pi + 1) % 3
                for c in range(8):
                    mm(P[:], hT[:, c, t4 * 128:(t4 + 1) * 128], W1[:, c, 2304 + 128:2304 + 640], c == 0, c == 7, hr + ('W1',), (pk,))
                for c in range(8):
                    mm(ps_sum[:, 0:128], hT[:, c, t4 * 128:(t4 + 1) * 128], W1[:, c, 2304:2304 + 128], c == 0, c == 7, hr + ('W1',), ('ps_sum',))
                tick()
                cp('act', VIst[:, t4, 128:640], P[:], (pk,), (f'VIst{t4}',))
                cp('dve', VIst[:, t4, 0:128], ps_sum[:, 0:128], ('ps_sum',), (f'VIst{t4}',))
                ld('sp', Vi[t0 + t4 * 128:t0 + (t4 + 1) * 128, :], VIst[:, t4, 128:640], (f'VIst{t4}',), ())
                ld('sp', Vd[t0 + t4 * 128:t0 + (t4 + 1) * 128, :], VIst[:, t4, 0:128], (f'VIst{t4}',), ())
            for j in range(18):
                P = pp[pi]; pk = f'pp{pi}'; pi = (pi + 1) % 3
                for c in range(8):
                    mm(P[:], W1[:, c, j * 128:(j + 1) * 128], hT[:, c, :], c == 0, c == 7, hr + ('W1',), (pk,))
                tick()
                if j < 6:
                    q2 = j % 2
                    act(sqq[:, q2, :], P[:], AF.Square, (pk,), (f'sqq{q2}',))

                    def stage_a(j=j, P=P, pk=pk, q2=q2):
                        mm(ps_sum[:], blk64, sqq[:, q2, :], True, True, (f'sqq{q2}', 'cm'), ('ps_sum',))
                        act(tq[:], ps_sum[:], AF.Sqrt, ('ps_sum',), ('tq',), scale=1.0 / 64, bias=EPS)
                        recip(rq[:], tq[:], ('tq',), ('rq',))
                        gc = 16 if j < 4 else 17
                        stt(qn[:, q2, :], P[:], vecs[:, gc:gc + 1], rq[:], ALU.mult, ALU.mult, (pk, 'rq', 'cst'), (f'qn{q2}',))

                    def stage_b(j=j, q2=q2, t0=t0):
                        mm(ps_swap[:], swapm, qn[:, q2, :], True, True, (f'qn{q2}', 'cm'), ('ps_swap',))
                        tt('pool', t1[:], qn[:, q2, :], cos_sb[:], ALU.mult, (f'qn{q2}', 'cos'), ('t1',))
                        tt('dve', t2[:], ps_swap[:], sin_sb[:], ALU.mult, ('ps_swap', 'sin'), ('t2',))
                        tt('pool', QTst[:, j, :], t1[:], t2[:], ALU.add, ('t1', 't2'), (f'QTst{j}',))
                        dst = QT[j][:, t0:t0 + 512] if j < 4 else KT[j - 4][:, t0:t0 + 512]
                        ld('sp', dst, QTst[:, j, :], (f'QTst{j}',), ())

                    defer(1, stage_a)
                    defer(2, stage_b)
                elif j < 10:
                    hh = j - 6
                    act(sqh[:, hh, :], P[:], AF.Silu, (pk,), (f'sqh{hh}',))
                else:
                    s_ = it % 2
                    it += 1
                    chain = f_chain(j, P, pk, s_, t0, blk)
                    if pend_chain is None:
                        pend_chain = chain
                    else:
                        a_, b_ = pend_chain, chain
                        pend_chain = None
                        for k in range(max(len(a_), len(b_))):
                            if k < len(a_):
                                a_[k]()
                            if k < len(b_):
                                b_[k]()
        while dq:
            tick()
        cx.barrier()

    with ExitStack() as ps2:
        KTs = sbuf(ps2, "KTs", [128, 2, 2, S], BF16)
        Vs = sbuf(ps2, "Vs", [128, NT, 128], BF16)
        Vp0 = sbuf(ps2, "Vp0", [128, NT, 2, 65], BF16)
        Vp1 = sbuf(ps2, "Vp1", [128, NT, 2, 128], BF16)
        Qs = [sbuf(ps2, f"Qs{i}", [128, 512], BF16) for i in range(2)]
        Pt = [sbuf(ps2, f"Pt{i}", [128, 512], BF16) for i in range(4)]
        rs = sbuf(ps2, "rs", [128, 512], F32)
        rb = sbuf(ps2, "rb", [128, 512], F32)
        AOst = [sbuf(ps2, f"AOst{i}", [128, 512], BF16) for i in range(2)]
        pss = [psum(ps2, f"pss{i}", [128, 512], F32) for i in range(3)]
        po = [psum(ps2, f"po{i}", [128, 512], F32) for i in range(2)]
        pb = psum(ps2, "pb", [128, 512], F32)
        cx.op('pool', lambda: nc.gpsimd.memset(KTs[64:128, :, 0, :], 0.0), (), ('KTz0',))
        cx.op('pool', lambda: nc.gpsimd.memset(KTs[0:64, :, 1, :], 0.0), (), ('KTz1',))
        for g in range(2):
            ld('sp', KTs[0:64, g, 0, :], KT[g][0:64, :], (), ('KTs',), semkey='const')
            ld('act', KTs[64:128, g, 1, :], KT[g][64:128, :], (), ('KTs',), semkey='const')
        ld('act', Vs[:], Vd.rearrange("(t p) f -> p t f", p=128), (), ('Vs',), semkey='const')
        cx.finalize_group('const', ['KTs', 'Vs'])
        cx.op('pool', lambda: nc.gpsimd.memset(Vp0[:], 1.0), (), ('Vp0',))
        cx.op('pool', lambda: nc.gpsimd.memset(Vp1[:], 0.0), (), ('Vp1',))
        cx.op('pool', lambda: nc.gpsimd.memset(Vp1[:, :, :, 0:1], 1.0), ('Vp1',), ('Vp1',))
        for g in range(2):
            cp('dve', Vp0[:, :, g, 0:64], Vs[:, :, g * 64:(g + 1) * 64], ('Vs', 'Vp0'), ('Vp0',))
            cp('dve', Vp1[:, :, g, 64:128], Vs[:, :, g * 64:(g + 1) * 64], ('Vs', 'Vp1'), ('Vp1',))
        heads = [(qb, c, hl) for qb in range(NB) for c in range(4) for hl in range(2)]
        NH = len(heads)
        NI = NH * NT
        LA = 2

        def qload(ci):
            qb, c = ci // 4, ci % 4
            ld('sp', Qs[ci % 2][:], QT[c][:, qb * 512:(qb + 1) * 512], (), (f'Qs{ci % 2}',))

        def emit_S(i):
            hi, kc = i // NT, i % NT
            qb, c, hl = heads[hi]
            ci = qb * 4 + c
            if kc == 0 and hl == 0 and ci + 1 < NB * 4:
                qload(ci + 1)
            hp = hl * 64
            g = c // 2
            mm(pss[i % 3][:], KTs[:, g, hl, kc * 128:(kc + 1) * 128], Qs[ci % 2][:, :], True, True,
               ('KTs', 'KTz0', 'KTz1', f'Qs{ci % 2}'), (f'pss{i % 3}',))

        def emit_exp(i):
            act(Pt[i % 4][:], pss[i % 3][:], AF.Exp, (f'pss{i % 3}',), (f'Pt{i % 4}',), scale=0.125)

        def emit_PV(i):
            hi, kc = i // NT, i % NT
            qb, c, hl = heads[hi]
            g = c // 2
            O = po[hi % 2]; ok = f'po{hi % 2}'
            if hl == 0:
                mm(O[0:65, :], Vp0[:, kc, g, :], Pt[i % 4][:], kc == 0, kc == NT - 1, ('Vp0', f'Pt{i % 4}'), (ok,))
            else:
                mm(O[:, :], Vp1[:, kc, g, :], Pt[i % 4][:], kc == 0, kc == NT - 1, ('Vp1', f'Pt{i % 4}'), (ok,))

        def epilogue(hi):
            qb, c, hl = heads[hi]
            O = po[hi % 2]; ok = f'po{hi % 2}'
            ao = AOst[c % 2]; aok = f'AOst{c % 2}'
            if hl == 0:
                recip(rs[64:65, :], O[64:65, :], (ok,), ('rs',))
                mm(pb[0:64, :], cmf[64:65, 1, 0:64], rs[64:65, :], True, True, ('rs', 'cst'), ('pb',))
                cp('act', rb[0:64, :], pb[0:64, :], ('pb',), ('rb',))
                tt('dve', ao[0:64, :], O[0:64, :], rb[0:64, :], ALU.mult, (ok, 'rb'), (aok,))
            else:
                recip(rs[0:1, :], O[0:1, :], (ok,), ('rs',))
                mm(pb[:, :], cmf[0:1, 1, :], rs[0:1, :], True, True, ('rs', 'cst'), ('pb',))
                cp('act', rb[64:128, :], pb[64:128, :], ('pb',), ('rb',))
                tt('dve', ao[64:128, :], O[64:128, :], rb[64:128, :], ALU.mult, (ok, 'rb'), (aok,))
                ld('act', AO[c][:, qb * 512:(qb + 1) * 512], ao[:], (aok,), ())

        qload(0)
        epi_at = min(6, NT - 1)
        for i in range(-LA, NI):
            if i + LA < NI:
                emit_S(i + LA)
            if i >= 0:
                emit_exp(i)
                emit_PV(i)
                hi, kc = i // NT, i % NT
                if kc == epi_at and hi > 0:
                    epilogue(hi - 1)
        epilogue(NH - 1)
        cx.barrier()

    with ExitStack() as ps3:
        msk = sbuf(ps3, "msk", [128, 2, 128], F32)
        ld('sp', msk[:, 0, :], msk_d[0], (), ('msk',), semkey='const')
        ld('sp', msk[:, 1, :], msk_d[1], (), ('msk',), semkey='const')
        cols = sbuf(ps3, "cols", [128, 8, 3, NCH], F32)
        for d in range(2):
            for hh in range(4):
                ld('act', cols[:, d * 4 + hh, :, :], COLS[d, hh], (), ('cols',), semkey='const')
        cx.finalize_group('const', ['msk', 'cols'])
        for d in range(2):
            with ExitStack() as psd:
                qd = [[sbuf(psd, f"qd{hh}_{i}", [128, 512], BF16) for i in range(2)] for hh in range(4)]
                kd = [[sbuf(psd, f"kd{hh}_{i}", [128, 512], BF16) for i in range(2)] for hh in range(4)]
                ks32 = [[sbuf(psd, f"ks128{hh}_{i}", [128, 4, 128], BF16) for i in range(2)] for hh in range(4)]
                v32 = [[sbuf(psd, f"v32z{hh}_{i}", [128, 16, 128], BF16) for i in range(2)] for hh in range(4)]
                for hh in range(4):
                    for i in range(2):
                        cx.op('pool', lambda: nc.gpsimd.memset(v32[hh][i][:], 0.0), (), (f'v32{hh}_{i}',))
                v128 = [[sbuf(psd, f"v128{hh}_{i}", [128, 4, 128], BF16) for i in range(2)] for hh in range(4)]
                state = [sbuf(psd, f"state{hh}", [128, 128], F32) for hh in range(4)]
                Spb = [sbuf(psd, f"Spb{hh}", [128, 128], BF16) for hh in range(4)]
                Am = [[sbuf(psd, f"Am{hh}_{i}", [128, 128], BF16) for i in range(2)] for hh in range(4)]
                ost = [[sbuf(psd, f"ost{hh}_{i}", [128, 512], BF16) for i in range(2)] for hh in range(4)]
                poh = [psum(psd, f"poh{hh}", [128, 512], F32) for hh in range(4)]
                pA = [psum(psd, f"pA{i}", [128, 512], F32) for i in range(2)]
                pP = [psum(psd, f"pP{i}", [128, 512], F32) for i in range(2)]
                for hh in range(4):
                    cx.op('pool', lambda: nc.gpsimd.memset(state[hh][:], 0.0), (), (f'state{hh}',))
                    cx.op('pool', lambda: nc.gpsimd.memset(Spb[hh][:], 0.0), (), (f'Spb{hh}',))
                blocks = list(range(NB)) if d == 0 else list(range(NB - 1, -1, -1))
                ai = 0
                ppi = 0
                for bi, blk in enumerate(blocks):
                    t0 = blk * 512
                    b2 = bi % 2
                    for hh in range(4):
                        sfx = f'{hh}_{b2}'
                        ld('sp', qd[hh][b2][:], QD[d, hh][:, t0:t0 + 512], (), ('qd' + sfx,))
                        ld('act', kd[hh][b2][:], KD[d, hh][:, t0:t0 + 512], (), ('kd' + sfx,))
                        ld('sp', ks32[hh][b2][:], KS[d, hh][t0:t0 + 512, :].rearrange("(t p) k -> p t k", p=128), (), ('ks32' + sfx,))
                        for cc in range(4):
                            ld('act' if cc % 2 == 0 else 'sp', v32[hh][b2][cc * 32:(cc + 1) * 32, cc::4, :],
                               Vi[t0:t0 + 512, hh * 128:(hh + 1) * 128].rearrange("(t cc c) k -> cc c t k", cc=4, c=32)[cc], (), ('v32' + sfx,))
                        ld('sp', v128[hh][b2][:], Vi[t0:t0 + 512, hh * 128:(hh + 1) * 128].rearrange("(t p) k -> p t k", p=128), (), ('v128' + sfx,))
                    tiles = list(range(4)) if d == 0 else list(range(3, -1, -1))
                    for ti, t4 in enumerate(tiles):
                        for hh in range(4):
                            sfx = f'{hh}_{b2}'
                            tc = slice(t4 * 128, (t4 + 1) * 128)
                            A = pA[ai]; ak = f'pA{ai}'; ai = (ai + 1) % 2
                            mm(A[:, 0:128], kd[hh][b2][:, tc], qd[hh][b2][:, tc], True, True, ('kd' + sfx, 'qd' + sfx), (ak,))
                            am = Am[hh][ti % 2]; amk = f'Am{hh}_{ti % 2}'
                            tt('dve', am[:], A[:, 0:128], msk[:, d, :], ALU.mult, (ak, 'msk'), (amk,))
                            ok = f'poh{hh}'
                            mm(poh[hh][:, tc], v128[hh][b2][:, t4, :], am[:], True, False, ('v128' + sfx, amk), (ok,))
                        chunks = list(range(4)) if d == 0 else list(range(3, -1, -1))
                        for ci, cc in enumerate(chunks):
                            for hh in range(4):
                                sfx = f'{hh}_{b2}'
                                ok = f'poh{hh}'
                                n_loc = t4 * 4 + cc
                                n_glob = blk * 16 + n_loc
                                cs = slice(n_loc * 32, (n_loc + 1) * 32)
                                mm(poh[hh][:, cs], Spb[hh][:], qd[hh][b2][:, cs], False, ci == 3, (f'Spb{hh}', 'qd' + sfx), (ok,))
                                Pp = pP[ppi]; pk = f'pP{ppi}'; ppi = (ppi + 1) % 2
                                mm(Pp[:, 0:128], ks32[hh][b2][:, t4, :], v32[hh][b2][:, n_loc, :], True, True, ('ks32' + sfx, 'v32' + sfx), (pk,))
                                stt(state[hh][:], state[hh][:], cols[:, d * 4 + hh, 2, n_glob:n_glob + 1], Pp[:, 0:128], ALU.mult, ALU.add,
                                    (f'state{hh}', 'cols', pk), (f'state{hh}',))
                                n_next = n_glob + 1 if d == 0 else n_glob - 1
                                if 0 <= n_next < NCH:
                                    ts('dve', Spb[hh][:], state[hh][:], cols[:, d * 4 + hh, 1, n_next:n_next + 1], None, ALU.mult, None,
                                       (f'state{hh}', 'cols'), (f'Spb{hh}',))
                    for hh in range(4):
                        osk = f'ost{hh}_{b2}'
                        cp('act', ost[hh][b2][:], poh[hh][:], (f'poh{hh}',), (osk,))
                        ld('sp', OD[d, hh][:, t0:t0 + 512], ost[hh][b2][:], (osk,), ())
            cx.barrier()

    with ExitStack() as ps4:
        W3 = sbuf(ps4, "W3", [128, 8, 2560], BF16)
        Wa = sbuf(ps4, "Wa", [128, 4, 1024], BF16)
        Wb = sbuf(ps4, "Wb", [128, 4, 1024], BF16)
        Wo = sbuf(ps4, "Wo", [128, 8, 1024], BF16)
        Wr = sbuf(ps4, "Wr", [128, 8, 36], F32)
        gbc = sbuf(ps4, "gbc", [128, 1024], F32)
        rbias = sbuf(ps4, "rbias", [128, 36], F32)
        for c in range(8):
            load_cast(W3[:, c, :], w3_d[:, c, :], 2560, 'W3')
            load_cast(Wo[:, c, :], wo_d[:, c, :], 1024, 'Wo')
        for c in range(4):
            load_cast(Wa[:, c, :], wa_d[:, c, :], 1024, 'Wa')
            load_cast(Wb[:, c, :], wb_d[:, c, :], 1024, 'Wb')
        ld('sp', Wr[:], wr_d, (), ('Wr',), semkey='const')
        ld('sp', gbc[:], gffn_bc_d, (), ('gbc',), semkey='const')
        ld('sp', rbias[:], rbias_d, (), ('rbias',), semkey='const')
        cx.finalize_group('const', ['W3', 'Wo', 'Wa', 'Wb', 'Wr', 'gbc', 'rbias'])
        for c in range(8):
            ts('pool', Wr[:, c, :], Wr[:, c, :], vecs[:, 8 + c:9 + c], None, ALU.mult, None, ('Wr', 'cst'), ('Wr',))
        xTs = sbuf(ps4, "xTs3", [128, 8, 512], F32)
        sq = sbuf(ps4, "sq3", [128, 2, 512], BF16)
        hT = sbuf(ps4, "hT3", [128, 8, 512], BF16)
        rt = sbuf(ps4, "rt3", [128, 512], F32)
        rstd = sbuf(ps4, "rstd3", [128, 512], F32)
        AOs = sbuf(ps4, "AOs", [128, 4, 512], BF16)
        ODs = sbuf(ps4, "ODs", [128, 2, 4, 512], BF16)
        osum = sbuf(ps4, "osum", [128, 1, 512], F32)
        osq = sbuf(ps4, "osq", [128, 512], BF16)
        ort = sbuf(ps4, "ort", [128, 512], F32)
        orr = sbuf(ps4, "orr", [128, 512], F32)
        ho = sbuf(ps4, "ho", [128, 512], F32)
        sog = sbuf(ps4, "sog", [128, 512], BF16)
        HO = sbuf(ps4, "HO", [128, 4, 512], BF16)
        sga = sbuf(ps4, "sga", [128, 512], BF16)
        sgb = sbuf(ps4, "sgb", [128, 512], BF16)
        m1 = sbuf(ps4, "m1", [128, 512], F32)
        m2 = sbuf(ps4, "m2", [128, 512], F32)
        mg = sbuf(ps4, "mg", [128, 8, 512], BF16)
        x2T = xTs
        xtok = [sbuf(ps4, f"xtok{i}", [128, 1024], F32) for i in range(2)]
        x2tok = [sbuf(ps4, "x2tok0", [128, 1024], F32)] * 2
        junk = sbuf(ps4, "junk", [128, 1024], BF16)
        h2 = [sbuf(ps4, f"h2{i}", [128, 1024], BF16) for i in range(2)]
        sm = sbuf(ps4, "sm", [128, 64], F32)
        L_all = sbuf(ps4, "L_all", [128, NT, 36], F32)
        smr = [sbuf(ps4, f"smr{i}", [128, 16], F32) for i in range(4)]
        G1r = [sbuf(ps4, f"G1r{i}", [128, 4], F32) for i in range(4)]
        G2r = [sbuf(ps4, f"G2r{i}", [128, 4], F32) for i in range(4)]
        R1r = [sbuf(ps4, f"R1r{i}", [128, 32], F32) for i in range(4)]
        R2r = [sbuf(ps4, f"R2r{i}", [128, 32], F32) for i in range(4)]
        R1 = sbuf(ps4, "R1", [128, 32], F32)
        R2 = sbuf(ps4, "R2", [128, 32], F32)
        R3 = sbuf(ps4, "R3", [128, 32], F32)
        G1 = sbuf(ps4, "G1", [128, 4], F32)
        G2 = sbuf(ps4, "G2", [128, 4], F32)
        cum = sbuf(ps4, "cum", [128, 32], F32)
        p_ss = psum(ps4, "p_ss", [128, 512], F32)
        p_a = psum(ps4, "p_a", [128, 512], F32)
        p_b = psum(ps4, "p_b", [128, 512], F32)
        p_g = [psum(ps4, f"p_g{i}", [128, 512], F32) for i in range(2)]
        p_x = [psum(ps4, f"p_x{i}", [128, 512], F32) for i in range(2)]
        p_l = psum(ps4, "p_l", [128, 512], F32)
        gi = 0
        xi = 0

        def load_ao(b):
            tb = b * 512
            for c in range(4):
                ld('sp', AOs[:, c, :], AO[c][:, tb:tb + 512], (), (f'AOs{c}',))
                for d in range(2):
                    ld('act', ODs[:, d, c, :], OD[d, c][:, tb:tb + 512], (), (f'ODs{d}{c}',))

        def load_xtok(ti_):
            ld('sp', xtok[ti_ % 2][:], x_d[ti_ * 128:(ti_ + 1) * 128, :], (), (f'xtok{ti_ % 2}',))

        load_xtok(0)
        for blk in range(NB):
            t0 = blk * 512
            emit_norm(blk, xTs, sq, hT, rt, rstd, p_ss, 0)
            hr = tuple(f'hT{c}' for c in range(8))
            if blk == 0:
                load_ao(0)
            for hh in range(4):
                tt('pool', osum[:, 0, :], ODs[:, 0, hh, :], ODs[:, 1, hh, :], ALU.add, (f'ODs0{hh}', f'ODs1{hh}'), ('osum',))
                act(osq[:], osum[:, 0, :], AF.Square, ('osum',), ('osq',))
                mm(p_ss[:], ones_bf, osq[:], True, True, ('osq', 'cm'), ('ps_ss',))
                act(ort[:], p_ss[:], AF.Sqrt, ('ps_ss',), ('ort',), scale=1.0 / 128, bias=EPS)
                recip(orr[:], ort[:], ('ort',), ('orr',))
                stt(ho[:], osum[:, 0, :], vecs[:, 18:19], orr[:], ALU.mult, ALU.mult, ('osum', 'orr', 'cst'), ('ho',))
                G = p_g[gi]; gk = f'p_g{gi}'; gi = (gi + 1) % 2
                for c in range(8):
                    mm(G[:], W3[:, c, hh * 128:(hh + 1) * 128], hT[:, c, :], c == 0, c == 7, hr + ('W3',), (gk,))
                act(sog[:], G[:], AF.Silu, (gk,), ('sog',))
                tt('dve', HO[:, hh, :], ho[:], sog[:], ALU.mult, ('ho', 'sog'), (f'HO{hh}',))
            for oc in range(8):
                for c in range(4):
                    mm(p_a[:], Wa[:, c, oc * 128:(oc + 1) * 128], AOs[:, c, :], c == 0, c == 3, (f'AOs{c}', 'Wa'), ('p_a',))
                G = p_g[gi]; gk = f'p_g{gi}'; gi = (gi + 1) % 2
                for c in range(8):
                    mm(G[:], W3[:, c, 512 + oc * 128:512 + (oc + 1) * 128], hT[:, c, :], c == 0, c == 7, hr + ('W3',), (gk,))
                act(sga[:], G[:], AF.Sigmoid, (gk,), ('sga',))
                tt('dve', m1[:], p_a[:], sga[:], ALU.mult, ('p_a', 'sga'), ('m1',))
                for c in range(4):
                    mm(p_b[:], Wb[:, c, oc * 128:(oc + 1) * 128], HO[:, c, :], c == 0, c == 3, (f'HO{c}', 'Wb'), ('p_b',))
                G = p_g[gi]; gk = f'p_g{gi}'; gi = (gi + 1) % 2
                for c in range(8):
                    mm(G[:], W3[:, c, 1536 + oc * 128:1536 + (oc + 1) * 128], hT[:, c, :], c == 0, c == 7, hr + ('W3',), (gk,))
                act(sgb[:], G[:], AF.Sigmoid, (gk,), ('sgb',))
                tt('dve', m2[:], p_b[:], sgb[:], ALU.mult, ('p_b', 'sgb'), ('m2',))
                tt('pool', mg[:, oc, :], m1[:], m2[:], ALU.add, ('m1', 'm2'), (f'mg{oc}',))
            if blk + 1 < NB:
                load_ao(blk + 1)
            mr = tuple(f'mg{c}' for c in range(8))
            for oc in range(8):
                X = p_x[xi]; xk = f'p_x{xi}'; xi = (xi + 1) % 2
                for c in range(8):
                    mm(X[:], Wo[:, c, oc * 128:(oc + 1) * 128], mg[:, c, :], c == 0, c == 7, mr + ('Wo',), (xk,))
                tt('dve', x2T[:, oc, :], X[:], xTs[:, oc, :], ALU.add, (xk, f'xT{oc}'), (f'xT{oc}',))
            x2r = tuple(f'xT{c}' for c in range(8))
            for t4 in range(4):
                tile_i = blk * 4 + t4
                b2 = 0
                r0 = t0 + t4 * 128
                xb = tile_i % 2
                if tile_i + 1 < NT:
                    load_xtok(tile_i + 1)
                for half in range(2):
                    X = p_x[xi]; xk = f'p_x{xi}'; xi = (xi + 1) % 2
                    for c in range(8):
                        mm(X[:], mg[:, c, t4 * 128:(t4 + 1) * 128], Wo[:, c, half * 512:(half + 1) * 512], c == 0, c == 7, mr + ('Wo',), (xk,))
                    tt('dve', x2tok[b2][:, half * 512:(half + 1) * 512], X[:], xtok[xb][:, half * 512:(half + 1) * 512], ALU.add,
                       (xk, f'xtok{xb}'), (f'x2tok{b2}',))
                ld('act', X2[r0:r0 + 128, :], x2tok[b2][:], (f'x2tok{b2}',), ())
                act(junk[:], x2tok[b2][:], AF.Square, (f'x2tok{b2}',), ('junk',))
                red(sm[:, 0:1], junk[:], ALU.add, ('junk',), ('ssq',))
                act(sm[:, 1:2], sm[:, 0:1], AF.Sqrt, ('ssq',), ('rt2',), scale=1.0 / D, bias=EPS)
                recip(sm[:, 2:3], sm[:, 1:2], ('rt2',), ('r2',))
                stt(h2[b2][:], x2tok[b2][:], sm[:, 2:3], gbc[:], ALU.mult, ALU.mult, (f'x2tok{b2}', 'r2', 'gbc'), (f'h2{b2}',))
                ld('sp', H2[r0:r0 + 128, :], h2[b2][:], (f'h2{b2}',), ())
                for c in range(8):
                    mm(p_l[:, 0:36], x2T[:, c, t4 * 128:(t4 + 1) * 128], Wr[:, c, :], c == 0, c == 7, x2r + ('Wr',), ('p_l',))
                stt(L_all[:, tile_i, :], p_l[:, 0:36], sm[:, 2:3], rbias[:], ALU.mult, ALU.add, ('p_l', 'r2', 'rbias'), (f'L{tile_i}',))
        cx.barrier()

        def route_chain(tile_i, sl):
            st_ = []
            L = L_all[:, tile_i, :]
            smx = smr[sl]
            g1, g2, r1, r2_ = G1r[sl], G2r[sl], R1r[sl], R2r[sl]
            p = f'_{sl}'
            k1 = M12[:, tile_i, 0, :]
            k2 = M12[:, tile_i, 1, :]
            st_.append(lambda: red(smx[:, 3:4], L[:, 0:4], ALU.max, (), ('gmax' + p,)))
            st_.append(lambda: ts('dve', g1[:], L[:, 0:4], smx[:, 3:4], None, ALU.is_ge, None, ('gmax' + p,), ('G1' + p,)))
            st_.append(lambda: ts('dve', smx[:, 4:5], smx[:, 3:4], -1.0, None, ALU.mult, None, ('gmax' + p,), ('ngmax' + p,)))
            st_.append(lambda: act(g2[:], L[:, 0:4], AF.Exp, ('ngmax' + p,), ('G2' + p,), bias=smx[:, 4:5]))
            st_.append(lambda: red(smx[:, 5:6], g2[:], ALU.add, ('G2' + p,), ('gsum' + p,)))
            st_.append(lambda: recip(smx[:, 6:7], smx[:, 5:6], ('gsum' + p,), ('pg' + p,)))
            st_.append(lambda: ts('dve', g1[:], g1[:], -1.0, BIG, ALU.add, ALU.mult, ('G1' + p,), ('G1' + p,)))
            st_.append(lambda: tt('dve', r1[:].rearrange("p (g e) -> p g e", e=8), L[:, 4:36].rearrange("p (g e) -> p g e", e=8),
                                  g1[:].rearrange("p (g o) -> p g o", o=1).to_broadcast([128, 4, 8]), ALU.add, ('G1' + p,), ('R1' + p,)))
            st_.append(lambda: red(smx[:, 7:8], r1[:], ALU.max, ('R1' + p,), ('mx1' + p,)))
            st_.append(lambda: ts('dve', k1, r1[:], smx[:, 7:8], None, ALU.is_ge, None, ('R1' + p, 'mx1' + p), ('k1' + p,)))
            st_.append(lambda: stt(r2_[:], k1, -BIG, r1[:], ALU.mult, ALU.add, ('k1' + p, 'R1' + p), ('R2' + p,)))
            st_.append(lambda: red(smx[:, 8:9], r2_[:], ALU.max, ('R2' + p,), ('mx2' + p,)))
            st_.append(lambda: ts('dve', k2, r2_[:], smx[:, 8:9], None, ALU.is_ge, None, ('R2' + p, 'mx2' + p), ('k2' + p,)))
            st_.append(lambda: tt('dve', smx[:, 9:10], smx[:, 8:9], smx[:, 7:8], ALU.subtract, ('mx1' + p, 'mx2' + p), ('dm' + p,)))
            st_.append(lambda: act(smx[:, 10:11], smx[:, 9:10], AF.Exp, ('dm' + p,), ('edm' + p,)))
            st_.append(lambda: ts('dve', smx[:, 10:11], smx[:, 10:11], 1.0, None, ALU.add, None, ('edm' + p,), ('edm' + p,)))
            st_.append(lambda: recip(smx[:, 11:12], smx[:, 10:11], ('edm' + p,), ('p1' + p,)))
            st_.append(lambda: tt('dve', C12[:, tile_i, 0:1], smx[:, 11:12], smx[:, 6:7], ALU.mult, ('p1' + p, 'pg' + p), ('c1' + p,)))
            st_.append(lambda: tt('dve', C12[:, tile_i, 1:2], smx[:, 6:7], C12[:, tile_i, 0:1], ALU.subtract, ('pg' + p, 'c1' + p), ('c2' + p,)))
            return st_

        NSL = 4
        for t0_ in range(0, NT, NSL):
            chains = [route_chain(t0_ + sl, sl) for sl in range(min(NSL, NT - t0_))]
            for k in range(max(len(c) for c in chains)):
                for c in chains:
                    if k < len(c):
                        c[k]()
        cx.barrier()
        for tile_i in range(NT):
            k1 = M12[:, tile_i, 0, :]
            k2 = M12[:, tile_i, 1, :]
            tt('dve', R3[:], k1, k2, ALU.add, (), ('R3',))
            mm(p_l[:, 64:96], ustrict_f, R3[:], True, False, ('R3', 'cst'), ('p_l2',))
            mm(p_l[:, 64:96], ones_f, Macc[:], False, True, ('Macc', 'cst'), ('p_l2',))
            cp('act', cum[:], p_l[:, 64:96], ('p_l2',), ('cum',))
            tt('pool', Macc[:], Macc[:], R3[:], ALU.add, ('Macc', 'R3'), ('Macc',))
            rkk = f'RK_{tile_i}'
            tt('dve', R1[:], k1, cum[:], ALU.mult, ('cum',), ('R1',))
            red(RK[:, tile_i, 0:1], R1[:], ALU.add, ('R1',), (rkk + 'a',))
            tt('dve', R2[:], k2, cum[:], ALU.mult, ('cum',), ('R2',))
            red(RK[:, tile_i, 1:2], R2[:], ALU.add, ('R2',), (rkk + 'b',))
        cx.barrier()

    with ExitStack() as ps5:
        nf = sbuf(ps5, "nf", [128, 32], F32)
        ni = sbuf(ps5, "ni", [128, 32], I32)
        npad = sbuf(ps5, "npad", [128, 32], F32)
        endc = sbuf(ps5, "endc", [128, 32], F32)
        base = sbuf(ps5, "base", [128, 32], F32)
        onesr = sbuf(ps5, "onesr", [128, 32], F32)
        cmpb = sbuf(ps5, "cmpb", [128, NTILES, 32], F32)
        te = sbuf(ps5, "te", [128, NTILES], F32)
        wif = sbuf(ps5, "wif", [128, NTILES, 2], F32)
        Dtmp = sbuf(ps5, "Dtmp", [128, 32], F32)
        Df = sbuf(ps5, "Df", [128, NT, 2], F32)
        h2l = [sbuf(ps5, f"h2l{i}", [128, 1024], BF16) for i in range(2)]
        p_t = psum(ps5, "p_t", [128, 512], F32)
        mm(p_t[:, 0:32], ones_f, Macc[:], True, True, ('Macc', 'cst'), ('p_t',))
        ts('dve', nf[:], p_t[:, 0:32], 511.0, 1.0 / 512, ALU.add, ALU.mult, ('p_t',), ('nf',))
        ts('dve', nf[:], nf[:], -0.499, None, ALU.add, None, ('nf',), ('nf',))
        cp('dve', ni[:], nf[:], ('nf',), ('ni',))
        cp('dve', npad[:], ni[:], ('ni',), ('npad',))
        ts('dve', npad[:], npad[:], 512.0, None, ALU.mult, None, ('npad',), ('npad',))
        cx.op('pool', lambda: nc.gpsimd.memset(onesr[:], 1.0), (), ('onesr',))
        cx.op('dve', lambda: nc.vector.tensor_tensor_scan(out=endc[:], data0=onesr[:], data1=npad[:], initial=0.0,
                                                         op0=ALU.mult, op1=ALU.add), ('onesr', 'npad'), ('endc',))
        tt('dve', base[:], endc[:], npad[:], ALU.subtract, ('endc', 'npad'), ('base',))
        tt('dve', cmpb[:], endc[:].rearrange("p (o e) -> p o e", o=1).to_broadcast([128, NTILES, 32]),
           misc[:, 64:64 + NTILES].rearrange("p (t o) -> p t o", o=1).to_broadcast([128, NTILES, 32]), ALU.is_le, ('endc', 'cst'), ('cmpb',))
        red(te[:].rearrange("p (t o) -> p t o", o=1), cmpb[:], ALU.add, ('cmpb',), ('te',))
        ts('dve', te[:], te[:], 31.0, None, ALU.min, None, ('te',), ('te',))
        for half in range(2):
            ts('dve', wif[:, :, half], te[:], 256.0, float(half), ALU.mult, ALU.add, ('te',), ('wif',))
        sm5 = sbuf(ps5, "sm5", [128, 1], F32)
        ts('dve', sm5[:], misc[:, 32:33], 2.0, None, ALU.mult, None, ('cst',), ('sm5',))
        ts('dve', wif[:], wif[:], sm5[:, 0:1], None, ALU.add, None, ('wif', 'sm5'), ('wif',))
        cp('dve', WI[:], wif[:], ('wif',), ('WI',))
        for ti in range(NT):
            for k in range(2):
                tt('dve', Dtmp[:], M12[:, ti, k, :], base[:], ALU.mult, ('base',), ('Dtmp',))
                red(Df[:, ti, k:k + 1], Dtmp[:], ALU.add, ('Dtmp',), ('Df',))
        tt('dve', Df[:], Df[:], RK[:], ALU.add, ('Df',), ('Df',))
        cp('dve', DI[:], Df[:], ('Df',), ('DI',))
        for ti in range(NT):
            b2 = ti % 2
            ld('sp', h2l[b2][:], H2[ti * 128:(ti + 1) * 128, :], (), (f'h2l{b2}',))
            for k in range(2):
                cx.dma('pool', lambda: nc.gpsimd.indirect_dma_start(
                    out=XS[:, :], out_offset=bass.IndirectOffsetOnAxis(ap=DI[:, ti, k:k + 1], axis=0),
                    in_=h2l[b2][:, :], in_offset=None), (f'h2l{b2}', 'DI'), (), semkey=f'sc{b2}')
        cx.barrier()

    with ExitStack() as ps6:
        Wg = [sbuf(ps6, f"Wg{i}", [128, 8, 512], BF16) for i in range(2)]
        Wu = [sbuf(ps6, f"Wu{i}", [128, 8, 512], BF16) for i in range(2)]
        Wd = [sbuf(ps6, f"Wd{i}", [128, 4, 1024], BF16) for i in range(2)]
        Xr = [sbuf(ps6, f"Xr{i}", [128, 4, 1024], BF16) for i in range(2)]
        XT = sbuf(ps6, "XTm", [128, 8, 512], BF16)
        sg = [sbuf(ps6, f"sg{i}", [128, 512], F32) for i in range(2)]
        hid = sbuf(ps6, "hid", [128, 4, 512], BF16)
        Yst = [sbuf(ps6, f"Yst{i}", [128, 1024], BF16) for i in range(2)]
        pTm = [psum(ps6, f"pTm{i}", [128, 1024], BF16) for i in range(2)]
        pg_ = [psum(ps6, f"pgm{i}", [128, 512], F32) for i in range(2)]
        pu_ = [psum(ps6, f"pum{i}", [128, 512], F32) for i in range(2)]
        py_ = [psum(ps6, f"pym{i}", [128, 512], F32) for i in range(2)]
        ti2 = 0
        gi = 0
        yi = 0
        ysi = 0
        for t in range(NTILES):
            b2 = t % 2
            for half in range(2):
                for (Wt, src, nm, ncc) in ((Wg[b2], wg_d, 'Wg', 4), (Wu[b2], wu_d, 'Wu', 4), (Wd[b2], wd_d, 'Wd', 2)):
                    dst = Wt[:, half * ncc:(half + 1) * ncc, :].rearrange("p c n -> p (c n)")
                    cx.dma('pool', lambda dst=dst, src=src: nc.gpsimd.indirect_dma_start(
                        out=dst, out_offset=None, in_=src[:, :],
                        in_offset=bass.IndirectOffsetOnAxis(ap=WI[:, t, half:half + 1], axis=0)), ('WI',), (f'{nm}{b2}',), semkey=f'{nm}{b2}')
            ld('sp', Xr[b2][:], XS[t * 512:(t + 1) * 512, :].rearrange("(s p) d -> p s d", p=128), (), (f'Xr{b2}',))
            for s in range(4):
                T = pTm[ti2]; tk = f'pTm{ti2}'; ti2 = (ti2 + 1) % 2
                for c in range(8):
                    tr(T[:, c * 128:(c + 1) * 128], Xr[b2][:, s, c * 128:(c + 1) * 128], ident, (f'Xr{b2}', 'cm'), (tk,))
                cp('dve' if s % 2 == 0 else 'act', XT[:, :, s * 128:(s + 1) * 128], T[:].rearrange("p (c k) -> p c k", k=128), (tk,), (f'XT{s}',))
            xr = tuple(f'XT{s}' for s in range(4))
            for hc in range(4):
                Gp = pg_[gi]; gk = f'pgm{gi}'
                Up = pu_[gi]; uk = f'pum{gi}'
                sgt = sg[gi]; sk = f'sg{gi}'
                gi = (gi + 1) % 2
                for c in range(8):
                    mm(Gp[:], Wg[b2][:, c, hc * 128:(hc + 1) * 128], XT[:, c, :], c == 0, c == 7, xr + (f'Wg{b2}',), (gk,))
                for c in range(8):
                    mm(Up[:], Wu[b2][:, c, hc * 128:(hc + 1) * 128], XT[:, c, :], c == 0, c == 7, xr + (f'Wu{b2}',), (uk,))
                act(sgt[:], Gp[:], AF.Silu, (gk,), (sk,))
                tt('dve', hid[:, hc, :], Up[:], sgt[:], ALU.mult, (uk, sk), (f'hid{hc}',))
            hr4 = tuple(f'hid{c}' for c in range(4))
            for s in range(4):
                ys = Yst[ysi]; ysk = f'Yst{ysi}'; ysi = (ysi + 1) % 2
                for half in range(2):
                    Y = py_[yi]; yk = f'pym{yi}'; yi = (yi + 1) % 2
                    for c in range(4):
                        mm(Y[:], hid[:, c, s * 128:(s + 1) * 128], Wd[b2][:, c, half * 512:(half + 1) * 512], c == 0, c == 3, hr4 + (f'Wd{b2}',), (yk,))
                    cp('act' if half == 0 else 'dve', ys[:, half * 512:(half + 1) * 512], Y[:], (yk,), (ysk,))
                ld('act', YS[t * 512 + s * 128:t * 512 + (s + 1) * 128, :], ys[:], (ysk,), ())
        cx.barrier()

    with ExitStack() as ps7:
        x2l = [sbuf(ps7, f"x2l{i}", [128, 1024], F32) for i in range(2)]
        y1 = [sbuf(ps7, f"y1{i}", [128, 1024], BF16) for i in range(2)]
        y2 = [sbuf(ps7, f"y2{i}", [128, 1024], BF16) for i in range(2)]
        acc = [sbuf(ps7, f"acc{i}", [128, 1024], F32) for i in range(2)]
        for ti in range(NT):
            b2 = ti % 2
            ld('sp', x2l[b2][:], X2[ti * 128:(ti + 1) * 128, :], (), (f'x2l{b2}',))
            for k, yt, nm in ((0, y1[b2], 'y1'), (1, y2[b2], 'y2')):
                cx.dma('pool', lambda yt=yt, k=k: nc.gpsimd.indirect_dma_start(
                    out=yt[:, :], out_offset=None, in_=YS[:, :],
                    in_offset=bass.IndirectOffsetOnAxis(ap=DI[:, ti, k:k + 1], axis=0)), ('DI',), (f'{nm}{b2}',))
            stt(acc[b2][:], y1[b2][:], C12[:, ti, 0:1], x2l[b2][:], ALU.mult, ALU.add, (f'y1{b2}', f'x2l{b2}'), (f'acc{b2}',))
            stt(acc[b2][:], y2[b2][:], C12[:, ti, 1:2], acc[b2][:], ALU.mult, ALU.add, (f'y2{b2}', f'acc{b2}'), (f'acc{b2}',))
            ld('sp', out_d[ti * 128:(ti + 1) * 128, :], acc[b2][:], (f'acc{b2}',), ())
        cx.barrier()
    es.close()
    return nc


def _consts(S):
    NTILES = (2 * S) // TROWS + 32
    ident = np.eye(128, dtype=np.float32)
    ones = np.ones((128, 128), np.float32)
    blk = np.zeros((128, 128), np.float32)
    blk[:64, :64] = 1
    blk[64:, 64:] = 1
    swap = np.zeros((128, 128), np.float32)
    for h in range(2):
        for i in range(32):
            swap[h * 64 + 32 + i, h * 64 + i] = 1
            swap[h * 64 + i, h * 64 + 32 + i] = 1
    ust = np.triu(np.ones((128, 128), np.float32), 1)
    cm = np.stack([ident, ones, blk, swap, ust]).astype(np.float32)
    s_idx = np.arange(128)[:, None]
    c_idx = np.arange(128)[None, :]
    same = (s_idx // 32) == (c_idx // 32)
    mf = (same & (s_idx <= c_idx)).astype(np.float32)
    mb = (same & (s_idx >= c_idx)).astype(np.float32)
    masks = np.stack([mf, mb])
    seg = np.ones((128, 512), np.float32)
    seg[:, ::32] = 0
    misc = np.zeros((128, 192), np.float32)
    misc[:, 0:32] = np.arange(32)[None, :]
    misc[:, 32] = np.arange(128)
    misc[:, 64:64 + NTILES] = (np.arange(NTILES) * TROWS)[None, :]
    rows = S // 64
    row_ids = np.repeat(np.arange(rows), 64).astype(np.float32)
    col_ids = np.tile(np.arange(64), rows).astype(np.float32)
    inv_freq = (10000.0 ** (-np.arange(0, 32, 2, dtype=np.float32) / 32)).astype(np.float32)
    ang = np.concatenate([row_ids[:, None] * inv_freq, col_ids[:, None] * inv_freq], axis=-1).astype(np.float32)
    cos = np.cos(ang).T.astype(np.float32)
    sin = np.sin(ang).T.astype(np.float32)
    cosT = np.tile(cos, (4, 1))
    sinT = np.tile(np.concatenate([-sin, sin], axis=0), (2, 1))
    return dict(cmats=cm, masks=masks, segmask=seg, misc=misc, cosT=np.ascontiguousarray(cosT), sinT=np.ascontiguousarray(sinT))


def _fm(w, nchunk):
    return np.ascontiguousarray(w.reshape(nchunk, 128, -1).transpose(1, 0, 2))


def _prep_shared(inp):
    w_in = inp["w_in"][0]
    offs = np.cumsum([0, 512, 128, 128, 512, 512, 512, 512, 512, 1024, 1024])
    aq, ak, av, hq, hff, hfb, hi, hg, ga, gb = [w_in[:, offs[i]:offs[i + 1]] for i in range(10)]
    deint = np.concatenate([np.arange(0, 64, 2), np.arange(1, 64, 2)])
    qperm = np.concatenate([h * 64 + deint for h in range(8)])
    aqp = aq[:, qperm]
    k0 = ak[:, 0:64][:, deint]
    k1 = ak[:, 64:128][:, deint]
    w1 = np.concatenate([aqp, k0, k0, k1, k1, hq, hff, hfb, av, hi], axis=1)
    w3 = np.concatenate([hg, ga, gb], axis=1)
    vec = np.zeros((128, 64), np.float32)
    vec[:, 0:8] = inp["g_mix"][0].reshape(8, 128).T
    vec[:, 8:16] = inp["g_ffn"][0].reshape(8, 128).T
    vec[:, 16] = np.tile(inp["q_norm"][0][deint], 2)
    vec[:, 17] = np.tile(inp["k_norm"][0][deint], 2)
    vec[:, 18] = inp["hgrn_norm"][0]
    lbf = np.stack([inp["lb_fwd"].reshape(2, 4, 128), inp["lb_bwd"].reshape(2, 4, 128)])
    lbf = np.ascontiguousarray(lbf.transpose(3, 0, 1, 2)).astype(np.float32)
    wr = np.concatenate([inp["w_router_group"][0], inp["w_router_expert"][0]], axis=1)
    rb = np.concatenate([inp["b_router_group"][0], inp["b_router_expert"][0]])[None, :].repeat(128, 0)

    def exp_rows(w, nchunk):
        n = w.shape[-1]
        a = w.reshape(32, 2, nchunk // 2, 128, n).transpose(0, 3, 1, 2, 4)
        return np.ascontiguousarray(a.reshape(32 * 128 * 2, (nchunk // 2) * n))

    return dict(
        w1=_fm(w1, 8), w3=_fm(w3, 8), wa=_fm(inp["w_attn_branch"][0], 4), wb=_fm(inp["w_hgrn_branch"][0], 4),
        wo=_fm(inp["w_out"][0], 8), wr=_fm(wr, 8).astype(np.float32),
        wg=exp_rows(inp["w_exp_gate"][0], 8), wu=exp_rows(inp["w_exp_up"][0], 8), wd=exp_rows(inp["w_exp_down"][0], 4),
        vecs=vec, lbf=lbf, gffn_bc=np.ascontiguousarray(inp["g_ffn"][0][None, :].repeat(128, 0)).astype(np.float32),
        rbias=np.ascontiguousarray(rb).astype(np.float32),
    )


def run(inp, cores=None, debug=False):
    x = np.asarray(inp["x"], np.float32)
    B, S, _ = x.shape
    inp = {k: np.asarray(v, np.float32) for k, v in inp.items()}
    shared = _prep_shared(inp)
    shared.update(_consts(S))
    nc = build(S, debug=debug)
    in_maps = []
    for b in range(B):
        m = dict(shared)
        m["x"] = np.ascontiguousarray(x[b])
        m["xT"] = np.ascontiguousarray(x[b].T)
        in_maps.append(m)
    res = run_bass_kernel_spmd(nc, in_maps, core_ids=list(range(B)) if cores is None else cores)
    if debug:
        return res.results
    return np.stack([r["out"] for r in res.results]).astype(np.float32)


def kernel(**inputs):
    return run(inputs)
```
